# Optimizing a Trainium2 kernel written in Bass

```python
import jax
import jax.numpy as jnp
from jax import lax
import numpy as np

D_MODEL = 4096
BATCH = 8
SEQ = 2048
DEPTH = 1

GRID_W = 64
CTX_LEN = 256
EPS = 1e-6
N_MOD = 6
N_BRANCHES = 2

MLA_HEADS = 16
Q_LORA = 1024
KV_LORA = 512
QK_NOPE = 128
QK_ROPE = 64
V_HEAD = 128
Q_BLOCK = 128
ROPE_THETA = 10000.0

DN_HEADS = 16
DN_DK = 128
DN_DV = 128
DN_QK = DN_HEADS * DN_DK
DN_V = DN_HEADS * DN_DV
CONV_W = 5
CHUNK = 64

N_GROUPS = 4
EXPERTS_PER_GROUP = 8
N_EXPERTS = N_GROUPS * EXPERTS_PER_GROUP
TOP_K = 2
EXPERT_HIDDEN = 1024
MOE_BLOCK = 256

OFF_CKV = 0
OFF_KR = OFF_CKV + KV_LORA
OFF_AB = OFF_KR + QK_ROPE
OFF_DK = OFF_AB + 4 * DN_HEADS
OFF_DV = OFF_DK + DN_QK
OFF_DQ = OFF_DV + DN_V
OFF_CQ = OFF_DQ + DN_QK
OFF_Z = OFF_CQ + Q_LORA
OFF_GATE = OFF_Z + DN_V
IN_COLS = OFF_GATE + N_BRANCHES * D_MODEL
CTX_COLS = OFF_DQ

kernel_name = "hybrid_mla_gdn_hmoe_dit_block"


def rmsnorm(x, g):
    xf = x.astype(jnp.float32)
    y = xf * lax.rsqrt(jnp.mean(xf * xf, axis=-1, keepdims=True) + EPS)
    return (y * g).astype(x.dtype)


def l2norm(x):
    xf = x.astype(jnp.float32)
    return (xf * lax.rsqrt(jnp.sum(xf * xf, axis=-1, keepdims=True) + EPS)).astype(x.dtype)


def modulate(h, shift, scale):
    return h * (1.0 + scale) + shift


def axial_rope(n):
    rows = n // GRID_W
    row = jnp.repeat(jnp.arange(rows), GRID_W).astype(jnp.float32)
    col = jnp.tile(jnp.arange(GRID_W), rows).astype(jnp.float32)
    half = QK_ROPE // 2
    inv_freq = ROPE_THETA ** (-jnp.arange(0, half, 2, dtype=jnp.float32) / half)
    ang = jnp.concatenate([row[:, None] * inv_freq, col[:, None] * inv_freq], axis=-1)
    return jnp.cos(ang), jnp.sin(ang)


def apply_rope(x, cos, sin):
    x0, x1 = x[..., 0::2], x[..., 1::2]
    r0 = x0 * cos - x1 * sin
    r1 = x0 * sin + x1 * cos
    return jnp.stack([r0, r1], axis=-1).reshape(x.shape).astype(x.dtype)


def mla_keys(proj, p, rope):
    b, t, _ = proj.shape
    ckv = rmsnorm(proj[..., OFF_CKV:OFF_KR], p["kv_norm_g"])
    kv = (ckv @ p["w_ukv"]).reshape(b, t, MLA_HEADS, QK_NOPE + V_HEAD)
    k_nope = rmsnorm(kv[..., :QK_NOPE], p["k_norm_g"])
    v = kv[..., QK_NOPE:]
    k_rope = rmsnorm(proj[..., OFF_KR:OFF_AB], p["k_rope_norm_g"])
    if rope is not None:
        k_rope = apply_rope(k_rope, rope[0], rope[1])
    return k_nope, k_rope, v


def mla_queries(proj, p, rope):
    b, t, _ = proj.shape
    cq = rmsnorm(proj[..., OFF_CQ:OFF_Z], p["q_a_norm_g"])
    q = (cq @ p["w_uq"]).reshape(b, t, MLA_HEADS, QK_NOPE + QK_ROPE)
    q_nope = rmsnorm(q[..., :QK_NOPE], p["q_norm_g"])
    q_rope = rmsnorm(q[..., QK_NOPE:], p["q_rope_norm_g"])
    if rope is not None:
        q_rope = apply_rope(q_rope, rope[0][:, None, :], rope[1][:, None, :])
    return q_nope, q_rope


def mla_attend(q_nope, q_rope, k_nope, k_rope, v):
    b, t, h, _ = q_nope.shape
    nb = t // Q_BLOCK
    scale = (QK_NOPE + QK_ROPE) ** -0.5

    def blocks(z):
        return z.reshape(b, nb, Q_BLOCK, *z.shape[2:]).swapaxes(0, 1)

    def one_block(args):
        qn, qr = args
        s = jnp.einsum("bqhd,bkhd->bhqk", qn, k_nope) + jnp.einsum("bqhr,bkr->bhqk", qr, k_rope)
        prob = jax.nn.softmax(s.astype(jnp.float32) * scale, axis=-1).astype(v.dtype)
        return jnp.einsum("bhqk,bkhd->bqhd", prob, v)

    o = lax.map(one_block, (blocks(q_nope), blocks(q_rope)))
    return o.swapaxes(0, 1).reshape(b, t, h * V_HEAD)


def short_conv(x, w):
    y = lax.conv_general_dilated(
        x, w[:, None, :], window_strides=(1,), padding=[(CONV_W // 2, CONV_W // 2)],
        dimension_numbers=("NWC", "WIO", "NWC"), feature_group_count=x.shape[-1])
    return jax.nn.silu(y)


def delta_inputs(proj, p, with_q):
    b, t, _ = proj.shape
    width = DN_QK + DN_V + (DN_QK if with_q else 0)
    kvq = short_conv(proj[..., OFF_DK:OFF_DK + width], p["conv_w"][:, :width])
    k = l2norm(kvq[..., :DN_QK].reshape(b, t, DN_HEADS, DN_DK))
    v = kvq[..., DN_QK:DN_QK + DN_V].reshape(b, t, DN_HEADS, DN_DV)
    q = l2norm(kvq[..., DN_QK + DN_V:].reshape(b, t, DN_HEADS, DN_DK)) if with_q else None
    ab = proj[..., OFF_AB:OFF_DK].astype(jnp.float32).reshape(b, t, 2, 2, DN_HEADS)
    log_alpha = -jnp.exp(p["a_log"].astype(jnp.float32)) * jax.nn.softplus(ab[:, :, 0] + p["dt_bias"])
    beta = jax.nn.sigmoid(ab[:, :, 1])
    return q, k, v, log_alpha, beta


def gated_delta_chunked(q, k, v, log_alpha, beta, s0):
    b, t, h, dk = k.shape
    dv = v.shape[-1]
    n = t // CHUNK

    def to_chunks(z):
        z = z.astype(jnp.float32).reshape(b, n, CHUNK, h, *z.shape[3:])
        return jnp.moveaxis(z, (1, 3), (0, 2))

    kc, vc, bc = to_chunks(k), to_chunks(v), to_chunks(beta)
    gc = jnp.cumsum(to_chunks(log_alpha), axis=-1)
    idx = jnp.arange(CHUNK)
    incl = idx[:, None] >= idx[None, :]
    strict = idx[:, None] > idx[None, :]
    diff = gc[..., :, None] - gc[..., None, :]
    decay = jnp.where(incl, jnp.exp(jnp.where(incl, diff, 0.0)), 0.0)
    kb = kc * bc[..., None]
    lmat = jnp.where(strict, jnp.einsum("nbhcd,nbhsd->nbhcs", kb, kc) * decay, 0.0)
    rhs = jnp.concatenate([vc * bc[..., None], kb * jnp.exp(gc)[..., None]], axis=-1)
    sol = lax.linalg.triangular_solve(jnp.eye(CHUNK, dtype=jnp.float32) + lmat, rhs,
                                      left_side=True, lower=True, unit_diagonal=True)
    u, w = sol[..., :dv], sol[..., dv:]
    k_tail = kc * jnp.exp(gc[..., -1:] - gc)[..., None]
    g_last = jnp.exp(gc[..., -1])
    if q is None:
        xs = (u, w, k_tail, g_last)
    else:
        qc = to_chunks(q) * (dk ** -0.5)
        qk = jnp.einsum("nbhcd,nbhsd->nbhcs", qc, kc) * decay
        xs = (u, w, k_tail, g_last, qk, qc * jnp.exp(gc)[..., None])

    def step(state, xs_c):
        u_c, w_c, kt_c, gl_c = xs_c[:4]
        v_new = u_c - jnp.einsum("bhcd,bhde->bhce", w_c, state)
        new_state = state * gl_c[..., None, None] + jnp.einsum("bhcd,bhce->bhde", kt_c, v_new)
        if len(xs_c) == 4:
            return new_state, None
        qk_c, qg_c = xs_c[4:]
        o_c = jnp.einsum("bhcd,bhde->bhce", qg_c, state) + jnp.einsum("bhcs,bhse->bhce", qk_c, v_new)
        return new_state, o_c

    s_final, o = lax.scan(step, s0.astype(jnp.float32), xs)
    if q is None:
        return None, s_final
    o = jnp.moveaxis(o, (0, 2), (1, 3)).reshape(b, t, h, dv)
    return o.astype(v.dtype), s_final


def bidir_delta(q, k, v, log_alpha, beta, s0):
    def flip(z):
        return None if z is None else jnp.flip(z, axis=1)
    o_f, s_f = gated_delta_chunked(q, k, v, log_alpha[:, :, 0], beta[:, :, 0], s0[0])
    o_b, s_b = gated_delta_chunked(flip(q), flip(k), flip(v), flip(log_alpha[:, :, 1]),
                                   flip(beta[:, :, 1]), s0[1])
    o = None if q is None else o_f + flip(o_b)
    return o, (s_f, s_b)


def merge_branches(proj, att, od, p):
    b, t, _ = proj.shape
    o_a = att @ p["w_oa"]
    z = jax.nn.silu(proj[..., OFF_Z:OFF_GATE]).reshape(b, t, DN_HEADS, DN_DV)
    o_b = (rmsnorm(od, p["dn_norm_g"]) * z).reshape(b, t, DN_V) @ p["w_ob"]
    g = jax.nn.sigmoid(proj[..., OFF_GATE:].reshape(b, t, N_BRANCHES, D_MODEL))
    return (g[:, :, 0] * o_a + g[:, :, 1] * o_b) @ p["w_out"]


def moe_ffn(h, p):
    n, d = h.shape
    hf = h.astype(jnp.float32)
    pg = jax.nn.softmax(hf @ p["w_rg"] + p["b_rg"], axis=-1)
    pg_top, g_top = lax.top_k(pg, 1)
    le = (hf @ p["w_re"] + p["b_re"]).reshape(n, N_GROUPS, EXPERTS_PER_GROUP)
    le_g = le[jnp.arange(n), g_top[:, 0]]
    pe_top, e_top = lax.top_k(jax.nn.softmax(le_g, axis=-1), TOP_K)
    gate = pe_top / jnp.sum(pe_top, axis=-1, keepdims=True) * pg_top
    expert = g_top * EXPERTS_PER_GROUP + e_top

    a = n * TOP_K
    e_flat = expert.reshape(a)
    order = jnp.argsort(e_flat)
    e_s = e_flat[order]
    t_s = order // TOP_K
    p_s = gate.reshape(a)[order]
    counts = jax.ops.segment_sum(jnp.ones((a,), jnp.int32), e_flat, num_segments=N_EXPERTS)
    padded = (counts + MOE_BLOCK - 1) // MOE_BLOCK * MOE_BLOCK
    pad_end = jnp.cumsum(padded)
    pad_start = pad_end - padded
    seg_start = jnp.cumsum(counts) - counts
    dest = pad_start[e_s] + jnp.arange(a) - seg_start[e_s]
    n_blocks = -(-a // MOE_BLOCK) + N_EXPERTS
    x_pad = jnp.zeros((n_blocks * MOE_BLOCK, d), h.dtype).at[dest].set(h[t_s])
    blk_expert = jnp.minimum(
        jnp.searchsorted(pad_end, jnp.arange(n_blocks) * MOE_BLOCK, side="right"), N_EXPERTS - 1)

    def expert_block(args):
        xb, e = args
        hb = jax.nn.silu(xb @ p["w1"][e]) * (xb @ p["w3"][e])
        return hb @ p["w2"][e]

    y_pad = lax.map(expert_block, (x_pad.reshape(n_blocks, MOE_BLOCK, d), blk_expert)).reshape(-1, d)
    y = y_pad[dest] * p_s[:, None].astype(h.dtype)
    return jax.ops.segment_sum(y, t_s, num_segments=n)


def trunk_layer(xl, xc, mod_l, mod_c, rope, p, update_ctx):
    b, t, d = xl.shape
    s1, sc1, g1, s2, sc2, g2 = (mod_l[:, i, None, :] for i in range(N_MOD))
    cs1, csc1, cg1, cs2, csc2, cg2 = (mod_c[i] for i in range(N_MOD))
    hl = modulate(rmsnorm(xl, p["norm1_g"]), s1, sc1)
    hc = modulate(rmsnorm(xc, p["norm1_g"]), cs1, csc1)
    pl = hl @ p["w_in"]
    pc = hc @ (p["w_in"] if update_ctx else p["w_in"][:, :CTX_COLS])

    kn_c, kr_c, v_c = mla_keys(pc, p, None)
    qd_c, kd_c, vd_c, la_c, be_c = delta_inputs(pc, p, with_q=update_ctx)
    s_zero = jnp.zeros((xc.shape[0], DN_HEADS, DN_DK, DN_DV), jnp.float32)
    od_c, s_ctx = bidir_delta(qd_c, kd_c, vd_c, la_c, be_c, (s_zero, s_zero))

    kn_l, kr_l, v_l = mla_keys(pl, p, rope)
    qn_l, qr_l = mla_queries(pl, p, rope)
    att_l = mla_attend(qn_l, qr_l, jnp.concatenate([kn_l, kn_c], axis=1),
                       jnp.concatenate([kr_l, kr_c], axis=1), jnp.concatenate([v_l, v_c], axis=1))
    qd_l, kd_l, vd_l, la_l, be_l = delta_inputs(pl, p, with_q=True)
    od_l, _ = bidir_delta(qd_l, kd_l, vd_l, la_l, be_l, s_ctx)
    xl = xl + g1 * merge_branches(pl, att_l, od_l, p)
    hl2 = modulate(rmsnorm(xl, p["norm2_g"]), s2, sc2)

    if update_ctx:
        qn_c, qr_c = mla_queries(pc, p, None)
        att_c = mla_attend(qn_c, qr_c, kn_c, kr_c, v_c)
        xc = xc + cg1 * merge_branches(pc, att_c, od_c, p)
        hc2 = modulate(rmsnorm(xc, p["norm2_g"]), cs2, csc2)
        ffn = moe_ffn(jnp.concatenate([hl2.reshape(-1, d), hc2.reshape(-1, d)], axis=0), p)
        xl = xl + g2 * ffn[:b * t].reshape(b, t, d)
        xc = xc + cg2 * ffn[b * t:].reshape(xc.shape)
    else:
        xl = xl + g2 * moe_ffn(hl2.reshape(-1, d), p).reshape(b, t, d)
    return xl, xc


def setup_inputs(seed: int = 0) -> dict:
    key = jax.random.key(seed)
    ks = jax.random.split(key, 32)
    f32 = jnp.float32
    L, D = DEPTH, D_MODEL

    def nrm(k, shape, scale):
        return jax.random.normal(k, shape, f32) * scale

    def gain(k, shape):
        return 1.0 + 0.05 * jax.random.normal(k, shape, f32)

    dt = jnp.exp(jax.random.uniform(ks[19], (L, 2, DN_HEADS), f32, np.log(1e-3), np.log(1e-1)))
    return {
        "x": nrm(ks[0], (BATCH, SEQ, D), 1.0),
        "c": nrm(ks[1], (BATCH, D), 1.0),
        "ctx": nrm(ks[2], (BATCH, CTX_LEN, D), 1.0),
        "c_ctx": nrm(ks[3], (D,), 1.0),
        "norm1_g": gain(ks[4], (L, D)),
        "norm2_g": gain(ks[5], (L, D)),
        "w_mod": nrm(ks[6], (L, D, N_MOD * D), 0.5 * D ** -0.5),
        "b_mod": nrm(ks[7], (L, N_MOD * D), 0.02),
        "w_in": nrm(ks[8], (L, D, IN_COLS), D ** -0.5),
        "q_a_norm_g": gain(ks[9], (L, Q_LORA)),
        "w_uq": nrm(ks[10], (L, Q_LORA, MLA_HEADS * (QK_NOPE + QK_ROPE)), Q_LORA ** -0.5),
        "kv_norm_g": gain(ks[11], (L, KV_LORA)),
        "w_ukv": nrm(ks[12], (L, KV_LORA, MLA_HEADS * (QK_NOPE + V_HEAD)), KV_LORA ** -0.5),
        "q_norm_g": gain(ks[13], (L, QK_NOPE)),
        "q_rope_norm_g": gain(ks[14], (L, QK_ROPE)),
        "k_norm_g": gain(ks[15], (L, QK_NOPE)),
        "k_rope_norm_g": gain(ks[16], (L, QK_ROPE)),
        "conv_w": nrm(ks[17], (L, CONV_W, DN_QK + DN_V + DN_QK), CONV_W ** -0.5),
        "a_log": jnp.log(jax.random.uniform(ks[18], (L, 2, DN_HEADS), f32, 1.0, 16.0)),
        "dt_bias": dt + jnp.log(-jnp.expm1(-dt)),
        "dn_norm_g": gain(ks[20], (L, DN_DV)),
        "w_oa": nrm(ks[21], (L, MLA_HEADS * V_HEAD, D), (MLA_HEADS * V_HEAD) ** -0.5),
        "w_ob": nrm(ks[22], (L, DN_V, D), DN_V ** -0.5),
        "w_out": nrm(ks[23], (L, D, D), D ** -0.5),
        "w_rg": nrm(ks[24], (L, D, N_GROUPS), D ** -0.5),
        "b_rg": nrm(ks[25], (L, N_GROUPS), 0.01),
        "w_re": nrm(ks[26], (L, D, N_EXPERTS), D ** -0.5),
        "b_re": nrm(ks[27], (L, N_EXPERTS), 0.01),
        "w1": nrm(ks[28], (L, N_EXPERTS, D, EXPERT_HIDDEN), D ** -0.5),
        "w3": nrm(ks[29], (L, N_EXPERTS, D, EXPERT_HIDDEN), D ** -0.5),
        "w2": nrm(ks[30], (L, N_EXPERTS, EXPERT_HIDDEN, D), EXPERT_HIDDEN ** -0.5),
    }


def reference(x, c, ctx, c_ctx, norm1_g, norm2_g, w_mod, b_mod, w_in, q_a_norm_g, w_uq,
              kv_norm_g, w_ukv, q_norm_g, q_rope_norm_g, k_norm_g, k_rope_norm_g, conv_w,
              a_log, dt_bias, dn_norm_g, w_oa, w_ob, w_out, w_rg, b_rg, w_re, b_re, w1, w3, w2):
    rope = axial_rope(x.shape[1])
    xl, xc = x, ctx
    for layer in range(DEPTH):
        p = {
            "norm1_g": norm1_g[layer], "norm2_g": norm2_g[layer], "w_in": w_in[layer],
            "q_a_norm_g": q_a_norm_g[layer], "w_uq": w_uq[layer],
            "kv_norm_g": kv_norm_g[layer], "w_ukv": w_ukv[layer],
            "q_norm_g": q_norm_g[layer], "q_rope_norm_g": q_rope_norm_g[layer],
            "k_norm_g": k_norm_g[layer], "k_rope_norm_g": k_rope_norm_g[layer],
            "conv_w": conv_w[layer], "a_log": a_log[layer], "dt_bias": dt_bias[layer],
            "dn_norm_g": dn_norm_g[layer], "w_oa": w_oa[layer], "w_ob": w_ob[layer],
            "w_out": w_out[layer], "w_rg": w_rg[layer], "b_rg": b_rg[layer],
            "w_re": w_re[layer], "b_re": b_re[layer],
            "w1": w1[layer], "w3": w3[layer], "w2": w2[layer],
        }
        mod_l = (jax.nn.silu(c) @ w_mod[layer] + b_mod[layer]).reshape(x.shape[0], N_MOD, D_MODEL)
        mod_c = (jax.nn.silu(c_ctx) @ w_mod[layer] + b_mod[layer]).reshape(N_MOD, D_MODEL)
        xl, xc = trunk_layer(xl, xc, mod_l, mod_c, rope, p, update_ctx=layer < DEPTH - 1)
    return xl
```

```python
import contextlib
import numpy as np
import concourse.bass as bass
import concourse.mybir as mybir

F32 = mybir.dt.float32
BF16 = mybir.dt.bfloat16
I32 = mybir.dt.int32
AF = mybir.ActivationFunctionType
ALU = mybir.AluOpType
AX = mybir.AxisListType

EPOCH = 30000
NDMASEM = 48


class Res:
    __slots__ = ("t", "w", "r", "name", "excl")

    def __init__(self, t, name="", excl=False):
        self.excl = excl
        self.t = t
        self.w = None
        self.r = []
        self.name = name

    def __getitem__(self, idx):
        return self.t[idx]


class _Proxy:
    def __getattr__(self, name):
        def f(*a, **kw):
            return (name, a, kw)
        return f


PROXY = _Proxy()


class Sched:
    ENGS = ("pe", "act", "dve", "pool", "sp")

    def __init__(self, nc, stack):
        self.nc = nc
        self.stack = stack
        self.ops = {e: [] for e in self.ENGS}
        self.count = {e: 0 for e in self.ENGS}
        self.sems = {e: [] for e in self.ENGS}
        self.waited = {e: {} for e in self.ENGS}
        self.dsem = [stack.enter_context(nc.semaphore(f"dma{i}")) for i in range(NDMASEM)]
        self.dcount = 0
        self.dlast = [0] * NDMASEM
        self.nsb = 0

    def sb(self, name, shape, dt=F32):
        t = self.tstack.enter_context(self.nc.sbuf_tensor("sb_" + name, list(shape), dt))
        return Res(t, name)

    def ps(self, name, shape, dt=F32):
        t = self.tstack.enter_context(self.nc.psum_tensor("ps_" + name, list(shape), dt))
        return Res(t, name, excl=True)

    def dram(self, name, shape, dt=F32):
        t = self.nc.dram_tensor("d_" + name, list(shape), dt, kind="Internal")
        return Res(t, name)

    def _tok(self, eng):
        self.count[eng] += 1
        k = self.count[eng]
        ep = (k - 1) // EPOCH
        while len(self.sems[eng]) <= ep:
            self.sems[eng].append(self.stack.enter_context(
                self.nc.semaphore(f"s_{eng}_{len(self.sems[eng])}")))
        return (self.sems[eng][ep], (k - 1) % EPOCH + 1, (eng, ep))

    def _need(self, eng, toks):
        waits = []
        best = {}
        for tk in toks:
            if tk is None:
                continue
            sem, val, key = tk
            if eng == "pe" and key[0] == "pe":
                continue
            if best.get(key, (None, 0))[1] < val:
                best[key] = (sem, val)
        for key, (sem, val) in best.items():
            if self.waited[eng].get(key, 0) >= val:
                continue
            self.waited[eng][key] = val
            waits.append((sem, val))
        return waits

    def _deps(self, reads, writes):
        toks = []
        for r in reads:
            toks.append(r.w)
            if r.excl:
                toks.extend(r.r)
        for w in writes:
            toks.append(w.w)
            toks.extend(w.r)
        return toks

    def _commit(self, tok, reads, writes):
        for r in reads:
            r.r.append(tok)
            if len(r.r) > 24:
                best = {}
                for t in r.r:
                    if best.get(t[2], (None, 0, None))[1] < t[1]:
                        best[t[2]] = t
                r.r = list(best.values())
        for w in writes:
            w.w = tok
            w.r = []

    def op(self, eng, fn, reads=(), writes=()):
        waits = self._need(eng, self._deps(reads, writes))
        tok = self._tok(eng)
        self.ops[eng].append((waits, fn(PROXY), (tok[0], 1)))
        self._commit(tok, reads, writes)
        return tok

    def group(self, eng, fns, reads=(), writes=()):
        waits = self._need(eng, self._deps(reads, writes))
        tok = self._tok(eng)
        n = len(fns)
        for i, fn in enumerate(fns):
            self.ops[eng].append((waits if i == 0 else [], fn(PROXY),
                                  (tok[0], 1) if i == n - 1 else None))
        self._commit(tok, reads, writes)
        return tok

    def dma(self, q, out, in_, reads=(), writes=(), **kw):
        i = self.dcount
        self.dcount += 1
        s = i % NDMASEM
        sem = self.dsem[s]
        prev = self.dlast[s]
        tgt = prev + 16
        if tgt > EPOCH:
            raise RuntimeError("dma sem overflow")
        self.dlast[s] = tgt
        toks = self._deps(reads, writes)
        if prev:
            toks.append((sem, prev, ("d", s)))
        waits = self._need(q, toks)
        tok = (sem, tgt, ("d", s))

        self.ops[q].append((waits, ("dma_start", (), dict(out=out, in_=in_, **kw)), (sem, 16)))
        self._commit(tok, reads, writes)
        return tok

    def barrier(self):
        toks = []
        for e in self.ENGS:
            k = self.count[e]
            if k:
                ep = (k - 1) // EPOCH
                toks.append((self.sems[e][ep], (k - 1) % EPOCH + 1, (e, ep)))
        for s in range(NDMASEM):
            if self.dlast[s]:
                toks.append((self.dsem[s], self.dlast[s], ("d", s)))
        for e in self.ENGS:
            waits = self._need(e, toks)
            if waits:
                self.ops[e].append((waits, None, None))

    def wait_all(self, eng, toks):
        waits = self._need(eng, toks)
        if waits:
            self.ops[eng].append((waits, None, None))

    def emit(self):
        nc = self.nc
        ops = self.ops

        def run(e, lst):
            for waits, fn, inc in lst:
                for sem, val in waits:
                    e.wait_ge(sem, val)
                if fn is None:
                    continue
                name, a, kw = fn
                ins = getattr(e, name)(*a, **kw)
                if inc is not None:
                    ins.then_inc(inc[0], inc[1])

        with nc.Block() as block:
            @block.tensor
            def _(e):
                run(e, ops["pe"])

            @block.scalar
            def _(e):
                run(e, ops["act"])

            @block.vector
            def _(e):
                run(e, ops["dve"])

            @block.gpsimd
            def _(e):
                run(e, ops["pool"])

            @block.sync
            def _(e):
                run(e, ops["sp"])

D = 4096; T = 2048; TC = 256; NT = 2304
NTILE = 18
IN_COLS = 18048
EPS = 1e-6
SCALE = 192 ** -0.5
TB = [(0, 512), (512, 512), (1024, 512), (1536, 512), (2048, 256)]
C_ML, C_MC, C_N1, C_N2, C_CW, C_G = 0, 192, 384, 416, 448, 688


def _consts():
    tri = np.triu(np.ones((128, 128), np.float32))
    i = np.arange(128)
    c = {
        "ident": np.eye(128, dtype=np.float32),
        "triF": tri, "triB": tri.T.copy(),
        "pmF": np.where(i[:, None] > i[None, :], 0.0, 1e4).astype(np.float32),
        "pmB": np.where(i[:, None] < i[None, :], 0.0, 1e4).astype(np.float32),
        "nmF": np.where(i[None, :] >= i[:, None], 0.0, -1e4).astype(np.float32),
        "nmB": np.where(i[None, :] <= i[:, None], 0.0, -1e4).astype(np.float32),
    }
    rows = T // 64
    row = np.repeat(np.arange(rows), 64).astype(np.float32)
    col = np.tile(np.arange(64), rows).astype(np.float32)
    inv = (10000.0 ** (-np.arange(0, 32, 2, dtype=np.float32) / 32)).astype(np.float32)
    ang = np.concatenate([row[:, None] * inv, col[:, None] * inv], axis=-1)
    c["cosT"] = np.cos(ang).T.astype(np.float32).copy()
    c["sinT"] = np.sin(ang).T.astype(np.float32).copy()
    return c


class K:
    pass


IN_SHAPES = {
    "x": [T, D], "ctx": [TC, D], "c2": [2, D], "norm1_g": [D], "norm2_g": [D], "w_mod": [D, 6 * D], "b_mod": [6 * D],
    "w_in": [D, IN_COLS], "q_a_norm_g": [1024], "w_uq": [1024, 3072], "kv_norm_g": [512], "w_ukv": [512, 4096],
    "q_norm_g": [128], "q_rope_norm_g": [64], "k_norm_g": [128], "k_rope_norm_g": [64], "conv_w": [5, 6144],
    "a_log": [32], "dt_bias": [32], "dn_norm_g": [128], "w_oa": [2048, D], "w_ob": [2048, D], "w_out": [D, D],
    "w_rg": [D, 4], "b_rg": [4], "w_re": [D, 32], "b_re": [32], "w1": [32, D, 1024], "w3": [32, D, 1024], "w2": [32, 1024, D],
    "ident": [128, 128], "triF": [128, 128], "triB": [128, 128], "pmF": [128, 128], "pmB": [128, 128],
    "nmF": [128, 128], "nmB": [128, 128], "cosT": [32, T], "sinT": [32, T],
}


class LazyIn(dict):
    def __init__(self, nc):
        super().__init__()
        self.nc = nc

    def __missing__(self, name):
        ap = self.nc.dram_tensor(name, list(IN_SHAPES[name]), F32, kind="ExternalInput").ap()
        self[name] = ap
        return ap


def declare(nc):
    return LazyIn(nc)


def sl(c, n=128):
    return slice(c * n, (c + 1) * n)


def phase0(k):
    S, I, P, ident, colT = k.S, k.I, k.P, k.ident, k.colT
    csrc = S.sb("csrc", [64, 128]); S.dma("sp", csrc[:], I["c2"].rearrange("j (k p) -> (j k) p", p=128), writes=[csrc])
    cT = S.sb("cT", [128, 64])
    S.group("pe", [lambda e: e.transpose(P[0][:, 0:64], csrc[:], ident[0:64, 0:64])], reads=[csrc, ident], writes=[P[0]])
    S.op("act", lambda e: e.activation(out=cT[:], in_=P[0][:, 0:64], func=AF.Silu), reads=[P[0]], writes=[cT])
    mrows = [S.sb(f"mrow{i}", [2, 512]) for i in range(2)]
    brows = [S.sb(f"brow{i}", [2, 512]) for i in range(2)]
    wm = [S.sb(f"wm{i}", [128, 32, 512]) for i in range(2)]
    wv = I["w_mod"].rearrange("(k p) n -> p k n", p=128)
    bv = I["b_mod"].rearrange("(o n) -> o n", o=1)
    for nb in range(48):
        w = wm[nb % 2]; mrow = mrows[nb % 2]; brow = brows[nb % 2]
        S.dma("sp", w[:], wv[:, :, sl(nb, 512)], writes=[w])
        S.dma("sp", brow[0:1, :], bv[:, sl(nb, 512)], writes=[brow])
        S.dma("sp", brow[1:2, :], bv[:, sl(nb, 512)], reads=[brow], writes=[brow])
        pb = P[nb % 2]
        S.group("pe", [lambda e, kk=kk, w=w, pb=pb: e.matmul(pb[0:2, :], lhsT=cT[:, kk:64:32], rhs=w[:, kk, :], start=(kk == 0), stop=(kk == 31)) for kk in range(32)],
                reads=[cT, w], writes=[pb])
        S.op("dve", lambda e, pb=pb, mrow=mrow, brow=brow: e.tensor_tensor(out=mrow[:], in0=pb[0:2, :], in1=brow[:], op=ALU.add),
             reads=[pb, brow], writes=[mrow])
        S.dma("sp", k.modrow[:, sl(nb, 512)], mrow[:], reads=[mrow], writes=[k.modrow])
    rows = S.sb("rows", [128, 6, 128])
    S.op("pool", lambda e: e.memset(rows[:], 0.0), writes=[rows])
    mv = k.modrow.t.ap().rearrange("j (r p) -> (j r) p", p=128)
    for i in range(3):
        S.dma("sp", rows[:, i, :], mv[sl(i), :], reads=[k.modrow, rows], writes=[rows])

    def rowsrc(name):
        return I[name].rearrange("(r p) -> r p", p=128)
    S.dma("sp", rows[0:32, 3, :], rowsrc("norm1_g"), reads=[rows], writes=[rows])
    S.dma("sp", rows[32:64, 3, :], rowsrc("norm2_g"), reads=[rows], writes=[rows])
    cwv = I["conv_w"].rearrange("j (r p) -> (j r) p", p=128)
    S.dma("sp", rows[0:128, 4, :], cwv[0:128, :], reads=[rows], writes=[rows])
    S.dma("sp", rows[0:112, 5, :], cwv[128:240, :], reads=[rows], writes=[rows])
    S.dma("sp", rows[112:116, 5, :], rowsrc("kv_norm_g"), reads=[rows], writes=[rows])
    S.dma("sp", rows[116:124, 5, :], rowsrc("q_a_norm_g"), reads=[rows], writes=[rows])
    S.dma("sp", rows[124:125, 5, :], rowsrc("q_norm_g"), reads=[rows], writes=[rows])
    S.dma("sp", rows[125:126, 5, :], rowsrc("k_norm_g"), reads=[rows], writes=[rows])
    S.dma("sp", rows[126:127, 5, :], rowsrc("dn_norm_g"), reads=[rows], writes=[rows])
    for i, (off, n) in enumerate([(0, 128), (128, 128), (256, 128), (384, 64), (448, 128), (576, 128)]):
        pb = P[2 + i % 2]
        S.group("pe", [lambda e, i=i, n=n, pb=pb: e.transpose(pb[:, 0:n], rows[0:n, i, :], ident[0:n, 0:n])], reads=[rows, ident], writes=[pb])
        S.op("dve", lambda e, off=off, n=n, pb=pb: e.tensor_copy(out=colT[:, off:off + n], in_=pb[:, 0:n]), reads=[pb], writes=[colT])


def derived(k):
    S, colT = k.S, k.colT
    k.A1l = S.sb("A1l", [128, 32]); k.A1c = S.sb("A1c", [128, 32]); k.A2l = S.sb("A2l", [128, 32])
    for A, sc in ((k.A1l, C_ML + 32), (k.A1c, C_MC + 32)):
        S.op("dve", lambda e, A=A, sc=sc: e.scalar_tensor_tensor(out=A[:], in0=colT[:, sc:sc + 32], scalar=1.0, in1=colT[:, C_N1:C_N1 + 32], op0=ALU.add, op1=ALU.mult), reads=[colT], writes=[A])
    S.op("dve", lambda e: e.scalar_tensor_tensor(out=k.A2l[:], in0=colT[:, C_ML + 128:C_ML + 160], scalar=1.0, in1=colT[:, C_N2:C_N2 + 32], op0=ALU.add, op1=ALU.mult), reads=[colT], writes=[k.A2l])


def norm_transpose(k, xt, xs, junk, ss, rs, A, shoff, dstT, f32copy=None):
    S, P, ident, colT = k.S, k.P, k.ident, k.colT
    S.op("act", lambda e: e.activation(out=junk[:], in_=xt[:], func=AF.Square, accum_out=ss[:]), reads=[xt], writes=[junk, ss])
    S.op("act", lambda e: e.activation(out=rs[:], in_=ss[:], func=AF.Sqrt, bias=k.epsc[:], scale=1.0 / D), reads=[ss, k.epsc], writes=[rs])
    S.op("dve", lambda e: e.reciprocal(out=rs[:], in_=rs[:]), reads=[rs], writes=[rs])
    S.op("dve", lambda e: e.tensor_scalar(out=xs[:], in0=xt[:], scalar1=rs[:, 0:1], scalar2=None, op0=ALU.mult), reads=[xt, rs], writes=[xs])
    for g in range(8):
        pb = P[4 + g % 4]
        S.group("pe", [lambda e, c=c, pb=pb, g=g: e.transpose(pb[:, sl(c - 4 * g)], xs[:, sl(c)], ident[:]) for c in range(4 * g, 4 * g + 4)],
                reads=[xs, ident], writes=[pb])
        for c in range(4 * g, 4 * g + 4):
            if c % 2:
                S.op("act", lambda e, c=c, pb=pb, g=g: e.activation(out=dstT[:, c, :], in_=pb[:, sl(c - 4 * g)], func=AF.Identity, scale=A[:, c:c + 1], bias=colT[:, shoff + c:shoff + c + 1]),
                     reads=[pb, A, colT], writes=[dstT])
            else:
                S.op("dve", lambda e, c=c, pb=pb, g=g: e.tensor_scalar(out=dstT[:, c, :], in0=pb[:, sl(c - 4 * g)], scalar1=A[:, c:c + 1], scalar2=colT[:, shoff + c:shoff + c + 1], op0=ALU.mult, op1=ALU.add),
                     reads=[pb, A, colT], writes=[dstT])
            if f32copy is not None:
                S.op("dve", lambda e, c=c, pb=pb, g=g: e.tensor_scalar(out=f32copy[:, c, :], in0=pb[:, sl(c - 4 * g)], scalar1=A[:, c:c + 1], scalar2=colT[:, shoff + c:shoff + c + 1], op0=ALU.mult, op1=ALU.add),
                     reads=[pb, A, colT], writes=[f32copy])


def phase1(k):
    S, I = k.S, k.I
    xts = [S.sb(f"xt{i}", [128, D]) for i in range(2)]
    xss = [S.sb(f"xs{i}", [128, D]) for i in range(2)]
    hts = [S.sb(f"ht{i}", [128, 32, 128], BF16) for i in range(2)]
    junk = S.sb("junk", [128, D], BF16)
    sss = [S.sb(f"ss{i}", [128, 1]) for i in range(2)]
    rss = [S.sb(f"rs{i}", [128, 1]) for i in range(2)]
    for ti in range(NTILE):
        xt = xts[ti % 2]; ht = hts[ti % 2]
        src = I["x"][sl(ti), :] if ti < 16 else I["ctx"][sl(ti - 16), :]
        S.dma("sp", xt[:], src, writes=[xt])
        norm_transpose(k, xt, xss[ti % 2], junk, sss[ti % 2], rss[ti % 2], k.A1l if ti < 16 else k.A1c, C_ML if ti < 16 else C_MC, ht)
        S.dma("sp", k.hT.t.ap()[:, :, sl(ti)].rearrange("c p t -> p c t"), ht[:], reads=[ht], writes=[k.hT])


def phase2(k):
    S, I, P = k.S, k.I, k.P
    wb = [S.sb(f"wb{i}", [128, 32, 1024], BF16) for i in range(2)]
    hb = [S.sb(f"hb{i}", [128, 32, 512], BF16) for i in range(2)]
    ob = [S.sb(f"ob{i}", [128, 512], BF16) for i in range(2)]
    of = [S.sb(f"of{i}", [64, 512]) for i in range(3)]
    wv = I["w_in"].rearrange("(k p) n -> p k n", p=128)
    hv = k.hT.t.ap().rearrange("c p t -> p c t")
    nblk = [(n0, min(1024, IN_COLS - n0)) for n0 in range(0, IN_COLS, 1024)]
    cnt = 0
    hcnt = 0
    for bi, (n0, nw) in enumerate(nblk):
        w = wb[bi % 2]
        S.dma("pool", w[:, :, 0:nw], wv[:, :, n0:n0 + nw], writes=[w])
        for (t0, tw) in TB:
            if t0 >= T and n0 >= 4736:
                continue
            h = hb[hcnt % 2]; hcnt += 1
            S.dma("sp", h[:, :, 0:tw], hv[:, :, t0:t0 + tw], reads=[k.hT], writes=[h])
            for ci in range(nw // 128):
                ch = n0 // 128 + ci
                pb = P[cnt % 4]; o = ob[cnt % 2]; cnt += 1
                S.group("pe", [lambda e, kk=kk, w=w, h=h, pb=pb, ci=ci, tw=tw: e.matmul(pb[:, 0:tw], lhsT=w[:, kk, sl(ci)], rhs=h[:, kk, 0:tw], start=(kk == 0), stop=(kk == 31)) for kk in range(32)],
                        reads=[w, h], writes=[pb])
                if cnt % 2:
                    S.op("act", lambda e, o=o, pb=pb, tw=tw: e.copy(out=o[:, 0:tw], in_=pb[:, 0:tw]), reads=[pb], writes=[o])
                else:
                    S.op("dve", lambda e, o=o, pb=pb, tw=tw: e.tensor_copy(out=o[:, 0:tw], in_=pb[:, 0:tw]), reads=[pb], writes=[o])
                S.dma("sp", k.plT[ch][:, t0:t0 + tw], o[:, 0:tw], reads=[o], writes=[k.plT[ch]])
                if ch == 4:
                    for j, (dst, lsl, m) in enumerate(((k.kreD, slice(512 - n0, 576 - n0, 2), 32), (k.kroD, slice(513 - n0, 576 - n0, 2), 32), (k.abD, slice(576 - n0, 640 - n0), 64))):
                        pb2 = P[4 + j]; o2 = of[j]
                        S.group("pe", [lambda e, kk=kk, w=w, h=h, pb2=pb2, lsl=lsl, m=m, tw=tw: e.matmul(pb2[0:m, 0:tw], lhsT=w[:, kk, lsl], rhs=h[:, kk, 0:tw], start=(kk == 0), stop=(kk == 31)) for kk in range(32)],
                                reads=[w, h], writes=[pb2])
                        S.op("dve", lambda e, o2=o2, pb2=pb2, m=m, tw=tw: e.tensor_copy(out=o2[0:m, 0:tw], in_=pb2[0:m, 0:tw]), reads=[pb2], writes=[o2])
                        S.dma("sp", dst[:, t0:t0 + tw], o2[0:m, 0:tw], reads=[o2], writes=[dst])


def blocks(W):
    return [(t0, min(512, W - t0)) for t0 in range(0, W, 512)]


def phase3(k):
    import os
    lvl = int(os.environ.get("K3", "9"))
    S, I, P, colT = k.S, k.I, k.P, k.colT
    ones_b, ones_f, epsc = k.ones_b, k.ones_f, k.epsc
    wukvs = [S.sb(f"wukv{i}", [128, 4, 256], BF16) for i in range(2)]
    wuqs = [S.sb(f"wuq{i}", [128, 8, 192], BF16) for i in range(2)]
    wukv_v = I["w_ukv"].rearrange("(c p) n -> p c n", p=128); wuq_v = I["w_uq"].rearrange("(c p) n -> p c n", p=128)
    ckvn = S.sb("ckvn", [128, 4, NT], BF16)
    cqn = S.sb("cqn", [128, 8, T], BF16)
    kr0 = S.sb("kr0", [32, NT], BF16); kr1 = S.sb("kr1", [32, NT], BF16)
    rbc = S.sb("rbc", [128, NT])
    cosT = S.sb("cosT", [32, T]); sinT = S.sb("sinT", [32, T])
    S.dma("sp", cosT[:], I["cosT"], writes=[cosT]); S.dma("sp", sinT[:], I["sinT"], writes=[sinT])
    sqs = [S.sb(f"sq{i}", [128, 512], BF16) for i in range(3)]
    rsq = [S.sb(f"rsq{i}", [128, 512]) for i in range(2)]
    grope = S.sb("grope", [32, 4])
    for j, (nm, par) in enumerate((("q_rope_norm_g", 0), ("q_rope_norm_g", 1), ("k_rope_norm_g", 0), ("k_rope_norm_g", 1))):
        S.dma("sp", grope[:, j:j + 1], I[nm].rearrange("(i two) -> i two", two=2)[:, par:par + 1], reads=[grope], writes=[grope], allow_slow_non_contiguous=True)
    grow = S.sb("grow", [1, 384]); gm = S.sb("gm", [1, 8]); negb = S.sb("negb", [128, 1])
    for j, (nm, o, n) in enumerate((("q_norm_g", 0, 128), ("k_norm_g", 128, 128), ("q_rope_norm_g", 256, 64), ("k_rope_norm_g", 320, 64))):
        S.dma("sp", grow[:, o:o + n], I[nm].rearrange("(o n) -> o n", o=1), reads=[grow], writes=[grow])
        S.op("dve", lambda e, j=j, o=o, n=n: e.tensor_reduce(out=gm[:, j:j + 1], in_=grow[:, o:o + n], axis=AX.X, op=ALU.max, apply_absolute_value=True), reads=[grow, gm], writes=[gm])
    S.op("dve", lambda e: e.tensor_tensor(out=gm[:, 4:5], in0=gm[:, 0:1], in1=gm[:, 1:2], op=ALU.mult), reads=[gm], writes=[gm])
    S.op("dve", lambda e: e.tensor_tensor(out=gm[:, 5:6], in0=gm[:, 2:3], in1=gm[:, 3:4], op=ALU.mult), reads=[gm], writes=[gm])
    S.op("dve", lambda e: e.tensor_scalar(out=gm[:, 4:5], in0=gm[:, 4:5], scalar1=-128.0 * SCALE, scalar2=None, op0=ALU.mult), reads=[gm], writes=[gm])
    S.op("dve", lambda e: e.scalar_tensor_tensor(out=gm[:, 6:7], in0=gm[:, 5:6], scalar=-64.0 * SCALE, in1=gm[:, 4:5], op0=ALU.mult, op1=ALU.add), reads=[gm], writes=[gm])
    S.group("pe", [lambda e: e.matmul(P[7][:, 0:1], lhsT=ones_f[0:1, :], rhs=gm[:, 6:7], start=True, stop=True)], reads=[ones_f, gm], writes=[P[7]])
    S.op("dve", lambda e: e.tensor_copy(out=negb[:], in_=P[7][:, 0:1]), reads=[P[7]], writes=[negb])

    if lvl < 1:
        return
    cnt = [0]

    def sumsq_rstd(srcs, parts, nfeat, t0, tw, dst_ap, dst_res):
        pb = P[7]
        n = len(srcs)
        for ci, (res, ap) in enumerate(srcs):
            sq = sqs[cnt[0] % 3]; cnt[0] += 1
            S.op("act", lambda e, sq=sq, ap=ap: e.activation(out=sq[0:parts, 0:tw], in_=ap, func=AF.Square), reads=[res], writes=[sq])
            S.group("pe", [lambda e, sq=sq, ci=ci: e.matmul(pb[0:parts, 0:tw], lhsT=ones_b[0:parts, 0:parts], rhs=sq[0:parts, 0:tw], start=(ci == 0), stop=(ci == n - 1))],
                    reads=[sq, ones_b], writes=[pb])
        S.op("act", lambda e: e.activation(out=dst_ap, in_=pb[0:parts, 0:tw], func=AF.Sqrt, bias=epsc[0:parts, :], scale=1.0 / nfeat), reads=[pb, epsc], writes=[dst_res])
        S.op("dve", lambda e: e.reciprocal(out=dst_ap, in_=dst_ap), reads=[dst_res], writes=[dst_res])

    ld = [S.sb(f"ld{i}", [128, NT], BF16) for i in range(8)]
    for c in range(4):
        S.dma("sp", ld[c][:], k.plT[c][:], reads=[k.plT[c]], writes=[ld[c]])
    for (t0, tw) in blocks(NT):
        sumsq_rstd([(ld[c], ld[c][:, t0:t0 + tw]) for c in range(4)], 128, 512, t0, tw, rbc[:, t0:t0 + tw], rbc)
    for c in range(4):
        S.op("dve", lambda e, c=c: e.scalar_tensor_tensor(out=ckvn[:, c, :], in0=ld[c][:], scalar=colT[:, C_G + c:C_G + c + 1], in1=rbc[:], op0=ALU.mult, op1=ALU.mult), reads=[ld[c], colT, rbc], writes=[ckvn])
    for c in range(8):
        S.dma("sp", ld[c][:, 0:T], k.plT[53 + c][:, 0:T], reads=[k.plT[53 + c]], writes=[ld[c]])
    for (t0, tw) in blocks(T):
        sumsq_rstd([(ld[c], ld[c][:, t0:t0 + tw]) for c in range(8)], 128, 1024, t0, tw, rbc[:, t0:t0 + tw], rbc)
    for c in range(8):
        S.op("dve", lambda e, c=c: e.scalar_tensor_tensor(out=cqn[:, c, :], in0=ld[c][:, 0:T], scalar=colT[:, C_G + 4 + c:C_G + 5 + c], in1=rbc[:, 0:T], op0=ALU.mult, op1=ALU.mult), reads=[ld[c], colT, rbc], writes=[cqn])

    if lvl < 2:
        return
    t1 = S.sb("t1", [32, 512]); t2 = S.sb("t2", [32, 512])

    def rope(ne, no, nres, t0, tw, d0, d1, dres0, dres1, pos0):
        cs = cosT[:, pos0:pos0 + tw]; sn = sinT[:, pos0:pos0 + tw]
        S.op("dve", lambda e: e.tensor_tensor(out=t1[:, 0:tw], in0=ne, in1=cs, op=ALU.mult), reads=[nres, cosT], writes=[t1])
        S.op("pool", lambda e: e.tensor_tensor(out=t2[:, 0:tw], in0=no, in1=sn, op=ALU.mult), reads=[nres, sinT], writes=[t2])
        S.op("dve", lambda e: e.tensor_tensor(out=d0, in0=t1[:, 0:tw], in1=t2[:, 0:tw], op=ALU.subtract), reads=[t1, t2], writes=[dres0])
        S.op("dve", lambda e: e.tensor_tensor(out=t1[:, 0:tw], in0=ne, in1=sn, op=ALU.mult), reads=[nres, sinT], writes=[t1])
        S.op("pool", lambda e: e.tensor_tensor(out=t2[:, 0:tw], in0=no, in1=cs, op=ALU.mult), reads=[nres, cosT], writes=[t2])
        S.op("dve", lambda e: e.tensor_tensor(out=d1, in0=t1[:, 0:tw], in1=t2[:, 0:tw], op=ALU.add), reads=[t1, t2], writes=[dres1])

    kre = S.sb("kre", [32, NT]); kro = S.sb("kro", [32, NT]); nn = S.sb("nn", [32, 2, 512])
    S.dma("sp", kre[:], k.kreD[:], reads=[k.kreD], writes=[kre]); S.dma("sp", kro[:], k.kroD[:], reads=[k.kroD], writes=[kro])
    for (t0, tw) in blocks(NT):
        rs = rsq[0]
        sumsq_rstd([(kre, kre[:, t0:t0 + tw]), (kro, kro[:, t0:t0 + tw])], 32, 64, t0, tw, rs[0:32, 0:tw], rs)
        S.op("dve", lambda e, t0=t0, tw=tw, rs=rs: e.scalar_tensor_tensor(out=nn[:, 0, 0:tw], in0=kre[:, t0:t0 + tw], scalar=grope[:, 2:3], in1=rs[0:32, 0:tw], op0=ALU.mult, op1=ALU.mult), reads=[kre, grope, rs], writes=[nn])
        S.op("dve", lambda e, t0=t0, tw=tw, rs=rs: e.scalar_tensor_tensor(out=nn[:, 1, 0:tw], in0=kro[:, t0:t0 + tw], scalar=grope[:, 3:4], in1=rs[0:32, 0:tw], op0=ALU.mult, op1=ALU.mult), reads=[kro, grope, rs, nn], writes=[nn])
        if t0 < T:
            rope(nn[:, 0, 0:tw], nn[:, 1, 0:tw], nn, t0, tw, kr0[:, t0:t0 + tw], kr1[:, t0:t0 + tw], kr0, kr1, t0)
        else:
            S.op("dve", lambda e, t0=t0, tw=tw: e.tensor_copy(out=kr0[:, t0:t0 + tw], in_=nn[:, 0, 0:tw]), reads=[nn], writes=[kr0])
            S.op("dve", lambda e, t0=t0, tw=tw: e.tensor_copy(out=kr1[:, t0:t0 + tw], in_=nn[:, 1, 0:tw]), reads=[nn], writes=[kr1])

    if lvl < 3:
        return
    knT = S.sb("knT", [128, NT], BF16); vsb = S.sb("vsb", [128, 18, 128], BF16)
    qnT = S.sb("qnT", [128, T], BF16); qr0 = S.sb("qr0", [32, T], BF16); qr1 = S.sb("qr1", [32, T], BF16)
    pTs = [S.sb(f"pT{i}", [128, 512], BF16) for i in range(3)]
    rec = S.sb("rec", [128, 512]); ao = [S.sb(f"ao{i}", [128, 512], BF16) for i in range(2)]
    k.d3 = dict(negb=negb, knT=knT, qnT=qnT, kr0=kr0, kr1=kr1, qr0=qr0, qr1=qr1, vsb=vsb, rec=rec, pT=pTs[0], ao1=ao[1], ao0=ao[0], ckvn=ckvn, cqn=cqn, rbc=rbc, gm=gm)
    pc = 0
    for h in range(16 if lvl >= 9 else 1):
        wukv = wukvs[h % 2]; wuq = wuqs[h % 2]
        S.dma("pool", wukv[:], wukv_v[:, :, h * 256:(h + 1) * 256], writes=[wukv])
        S.dma("pool", wuq[:], wuq_v[:, :, h * 192:(h + 1) * 192], writes=[wuq])
        for (t0, tw) in blocks(NT):
            pa = P[6]
            S.group("pe", [lambda e, c=c, t0=t0, tw=tw, wukv=wukv: e.matmul(pa[:, 0:tw], lhsT=wukv[:, c, 0:128], rhs=ckvn[:, c, t0:t0 + tw], start=(c == 0), stop=(c == 3)) for c in range(4)],
                    reads=[wukv, ckvn], writes=[pa])
            rs = rsq[1]
            sumsq_rstd([(pa, pa[:, 0:tw])], 128, 128, t0, tw, rs[:, 0:tw], rs)
            S.op("dve", lambda e, t0=t0, tw=tw, rs=rs, pa=pa: e.scalar_tensor_tensor(out=knT[:, t0:t0 + tw], in0=pa[:, 0:tw], scalar=colT[:, C_G + 13:C_G + 14], in1=rs[:, 0:tw], op0=ALU.mult, op1=ALU.mult), reads=[pa, colT, rs], writes=[knT])
        for g in range(5):
            pa = P[6]
            tiles = list(range(4 * g, min(4 * g + 4, 18)))
            for ti in tiles:
                S.group("pe", [lambda e, c=c, ti=ti, g=g, wukv=wukv: e.matmul(pa[:, sl(ti - 4 * g)], lhsT=ckvn[:, c, sl(ti)], rhs=wukv[:, c, 128:256], start=(c == 0), stop=(c == 3)) for c in range(4)],
                        reads=[wukv, ckvn], writes=[pa])
            S.op("act", lambda e, g=g, n=len(tiles): e.copy(out=vsb[:, 4 * g:4 * g + n, :], in_=pa[:, 0:n * 128].rearrange("p (a b) -> p a b", b=128)), reads=[pa], writes=[vsb])
        for (t0, tw) in blocks(T):
            pa = P[6]
            S.group("pe", [lambda e, c=c, t0=t0, tw=tw, wuq=wuq: e.matmul(pa[:, 0:tw], lhsT=wuq[:, c, 0:128], rhs=cqn[:, c, t0:t0 + tw], start=(c == 0), stop=(c == 7)) for c in range(8)],
                    reads=[wuq, cqn], writes=[pa])
            rs = rsq[1]
            sumsq_rstd([(pa, pa[:, 0:tw])], 128, 128, t0, tw, rs[:, 0:tw], rs)
            S.op("dve", lambda e, t0=t0, tw=tw, rs=rs, pa=pa: e.scalar_tensor_tensor(out=qnT[:, t0:t0 + tw], in0=pa[:, 0:tw], scalar=colT[:, C_G + 12:C_G + 13], in1=rs[:, 0:tw], op0=ALU.mult, op1=ALU.mult), reads=[pa, colT, rs], writes=[qnT])
            pe_, po_ = P[4], P[5]
            for par, pp in ((0, pe_), (1, po_)):
                S.group("pe", [lambda e, c=c, t0=t0, tw=tw, par=par, pp=pp, wuq=wuq: e.matmul(pp[0:32, 0:tw], lhsT=wuq[:, c, 128 + par:192:2], rhs=cqn[:, c, t0:t0 + tw], start=(c == 0), stop=(c == 7)) for c in range(8)],
                        reads=[wuq, cqn], writes=[pp])
            rs = rsq[0]
            sumsq_rstd([(pe_, pe_[0:32, 0:tw]), (po_, po_[0:32, 0:tw])], 32, 64, t0, tw, rs[0:32, 0:tw], rs)
            S.op("dve", lambda e, tw=tw, rs=rs: e.scalar_tensor_tensor(out=nn[:, 0, 0:tw], in0=pe_[0:32, 0:tw], scalar=grope[:, 0:1], in1=rs[0:32, 0:tw], op0=ALU.mult, op1=ALU.mult), reads=[pe_, grope, rs], writes=[nn])
            S.op("dve", lambda e, tw=tw, rs=rs: e.scalar_tensor_tensor(out=nn[:, 1, 0:tw], in0=po_[0:32, 0:tw], scalar=grope[:, 1:2], in1=rs[0:32, 0:tw], op0=ALU.mult, op1=ALU.mult), reads=[po_, grope, rs, nn], writes=[nn])
            rope(nn[:, 0, 0:tw], nn[:, 1, 0:tw], nn, t0, tw, qr0[:, t0:t0 + tw], qr1[:, t0:t0 + tw], qr0, qr1, t0)
        if lvl < 4:
            break
        for qb in range(4):
            q0 = qb * 512
            po = P[2 + qb % 2]; pd = P[4 + qb % 2]
            prev = None
            for kc in range(19):
                if kc < 18:
                    ps_ = P[kc % 2]
                    S.group("pe", [lambda e, ps_=ps_, kc=kc: e.matmul(ps_[:, :], lhsT=knT[:, sl(kc)], rhs=qnT[:, q0:q0 + 512], start=True, stop=False),
                                   lambda e, ps_=ps_, kc=kc: e.matmul(ps_[:, :], lhsT=kr0[:, sl(kc)], rhs=qr0[:, q0:q0 + 512], start=False, stop=False),
                                   lambda e, ps_=ps_, kc=kc: e.matmul(ps_[:, :], lhsT=kr1[:, sl(kc)], rhs=qr1[:, q0:q0 + 512], start=False, stop=True)],
                            reads=[knT, qnT, kr0, kr1, qr0, qr1], writes=[ps_])
                    pT = pTs[pc % 3]; pc += 1
                    S.op("act", lambda e, pT=pT, ps_=ps_: e.activation(out=pT[:], in_=ps_[:], func=AF.Exp, bias=negb[:, 0:1], scale=SCALE), reads=[ps_, negb], writes=[pT])
                if prev is not None and lvl >= 5:
                    pkc, ppT = prev
                    S.group("pe", [lambda e, pkc=pkc, ppT=ppT: e.matmul(pd[:, :], lhsT=ones_b[:], rhs=ppT[:], start=(pkc == 0), stop=(pkc == 17)),
                                   lambda e, pkc=pkc, ppT=ppT: e.matmul(po[:, :], lhsT=vsb[:, pkc, :], rhs=ppT[:], start=(pkc == 0), stop=(pkc == 17))],
                            reads=[ones_b, vsb, ppT], writes=[pd, po])
                prev = (kc, pT) if kc < 18 else None
            if lvl < 6:
                continue
            S.op("act", lambda e, pd=pd: e.copy(out=rec[:], in_=pd[:]), reads=[pd], writes=[rec])
            S.op("dve", lambda e: e.reciprocal(out=rec[:], in_=rec[:]), reads=[rec], writes=[rec])
            a = ao[qb % 2]
            if lvl < 7:
                continue
            S.op("dve", lambda e, a=a, po=po: e.tensor_tensor(out=a[:], in0=po[:], in1=rec[:], op=ALU.mult), reads=[po, rec], writes=[a])
            if lvl < 8:
                continue
            S.dma("sp", k.attD[h][:, q0:q0 + 512], a[:], reads=[a], writes=[k.attD[h]])
            if os.environ.get("K3BAR"):
                S.barrier()


def phase4(k):
    S, I, P, colT = k.S, k.I, k.P, k.colT
    ident, ones_f, ones_b, epsc = k.ident, k.ones_f, k.ones_b, k.epsc
    NG = 18 * 32
    cons = {}
    for nm in ("triF", "triB", "pmF", "pmB", "nmF", "nmB"):
        cons[nm] = S.sb("c_" + nm, [128, 128]); S.dma("sp", cons[nm][:], I[nm], writes=[cons[nm]])
    tri = (cons["triF"], cons["triB"]); pm = (cons["pmF"], cons["pmB"]); nm_ = (cons["nmF"], cons["nmB"])
    abT = S.sb("abT", [64, NT]); S.dma("sp", abT[:], k.abD[:], reads=[k.abD], writes=[abT])
    ab = S.sb("ab", [128, 18, 64])
    for g in range(5):
        tiles = list(range(4 * g, min(4 * g + 4, 18)))
        pa = P[g % 2]
        S.group("pe", [lambda e, ti=ti, g=g: e.transpose(pa[:, (ti - 4 * g) * 64:(ti - 4 * g + 1) * 64], abT[:, sl(ti)], ident[0:64, 0:64]) for ti in tiles], reads=[abT, ident], writes=[pa])
        S.op("dve", lambda e, g=g, n=len(tiles), pa=pa: e.tensor_copy(out=ab[:, 4 * g:4 * g + n, :], in_=pa[:, 0:n * 64].rearrange("p (a b) -> p a b", b=64)), reads=[pa], writes=[ab])
    dtb = S.sb("dtb", [128, 32]); negA = S.sb("negA", [128, 32])
    S.dma("sp", dtb[:], I["dt_bias"].partition_broadcast(128), writes=[dtb])
    S.dma("sp", negA[:], I["a_log"].partition_broadcast(128), writes=[negA])
    S.op("act", lambda e: e.activation(out=negA[:], in_=negA[:], func=AF.Exp), reads=[negA], writes=[negA])
    S.op("dve", lambda e: e.tensor_scalar(out=negA[:], in0=negA[:], scalar1=-1.0, scalar2=None, op0=ALU.mult), reads=[negA], writes=[negA])
    names = ("la", "lb", "beta", "g", "glast", "a", "eg", "c1", "kts", "egl")
    G_ = {n: S.sb("g_" + n, [128, 18, 32]) for n in names}
    la, lb, beta, gg, glast, aa, eg, c1, kts, egl = (G_[n] for n in names)
    one_c = ones_f[:, 0:1]
    bc = lambda t: t[:].unsqueeze(1).to_broadcast([128, 18, 32])
    S.op("dve", lambda e: e.tensor_tensor(out=la[:], in0=ab[:, :, 0:32], in1=bc(dtb), op=ALU.add), reads=[ab, dtb], writes=[la])
    S.op("act", lambda e: e.activation(out=la[:], in_=la[:], func=AF.Exp), reads=[la], writes=[la])
    S.op("act", lambda e: e.activation(out=la[:], in_=la[:], func=AF.Ln, bias=one_c), reads=[la, ones_f], writes=[la])
    S.op("dve", lambda e: e.tensor_tensor(out=la[:], in0=la[:], in1=bc(negA), op=ALU.mult), reads=[la, negA], writes=[la])
    S.op("act", lambda e: e.activation(out=lb[:], in_=ab[:, :, 32:64], func=AF.Exp, scale=-1.0), reads=[ab], writes=[lb])
    S.op("act", lambda e: e.activation(out=lb[:], in_=lb[:], func=AF.Ln, bias=one_c), reads=[lb, ones_f], writes=[lb])
    S.op("act", lambda e: e.activation(out=beta[:], in_=lb[:], func=AF.Exp, scale=-1.0), reads=[lb], writes=[beta])
    S.op("dve", lambda e: e.tensor_scalar(out=lb[:], in0=lb[:], scalar1=-1.0, scalar2=None, op0=ALU.mult), reads=[lb], writes=[lb])
    for half in range(2):
        tiles = list(range(9 * half, 9 * half + 9))
        pa, pb = P[2], P[3]
        for ti in tiles:
            j = ti - 9 * half
            S.group("pe", [lambda e, ti=ti, j=j: e.matmul(pa[:, j * 32:j * 32 + 16], lhsT=tri[0][:], rhs=la[:, ti, 0:16], start=True, stop=True),
                           lambda e, ti=ti, j=j: e.matmul(pa[:, j * 32 + 16:j * 32 + 32], lhsT=tri[1][:], rhs=la[:, ti, 16:32], start=True, stop=True),
                           lambda e, ti=ti, j=j: e.matmul(pb[:, j * 32:j * 32 + 32], lhsT=ones_f[:], rhs=la[:, ti, :], start=True, stop=True)],
                    reads=[tri[0], tri[1], la, ones_f], writes=[pa, pb])
        S.op("dve", lambda e, half=half, pa=pa: e.tensor_copy(out=gg[:, 9 * half:9 * half + 9, :], in_=pa[:, 0:288].rearrange("p (a b) -> p a b", b=32)), reads=[pa], writes=[gg])
        S.op("dve", lambda e, half=half, pb=pb: e.tensor_copy(out=glast[:, 9 * half:9 * half + 9, :], in_=pb[:, 0:288].rearrange("p (a b) -> p a b", b=32)), reads=[pb], writes=[glast])
    S.op("dve", lambda e: e.tensor_tensor(out=aa[:], in0=gg[:], in1=lb[:], op=ALU.add), reads=[gg, lb], writes=[aa])
    S.op("act", lambda e: e.activation(out=eg[:], in_=gg[:], func=AF.Exp), reads=[gg], writes=[eg])
    S.op("dve", lambda e: e.scalar_tensor_tensor(out=c1[:], in0=beta[:], scalar=-1.0, in1=eg[:], op0=ALU.mult, op1=ALU.mult), reads=[beta, eg], writes=[c1])
    S.op("dve", lambda e: e.tensor_tensor(out=kts[:], in0=glast[:], in1=gg[:], op=ALU.subtract), reads=[glast, gg], writes=[kts])
    S.op("act", lambda e: e.activation(out=kts[:], in_=kts[:], func=AF.Exp), reads=[kts], writes=[kts])
    S.op("act", lambda e: e.activation(out=egl[:], in_=glast[:], func=AF.Exp), reads=[glast], writes=[egl])

    import os
    lvl4 = int(os.environ.get("K4", "9"))
    if lvl4 < 1:
        return
    xin = S.sb("xin", [128, NT], BF16)
    kTf = S.sb("kTf", [128, NT]); vTf = S.sb("vTf", [128, NT]); qTf = S.sb("qTf", [128, T])
    kTb = S.sb("kTb", [128, NT], BF16); qTb = S.sb("qTb", [128, T], BF16)
    ktok = S.sb("ktok", [128, 18, 128]); vtok = S.sb("vtok", [128, 18, 128])
    Qall = S.sb("Qall", [128, 36, 128]); QKall = S.sb("QKall", [128, 32, 128], BF16)
    oall = S.sb("oall", [128, 16, 128])
    sqb = S.sb("sqb", [128, 512], BF16); rsb = S.sb("rsb", [128, 512])
    zs = S.sb("zs", [128, T]); odo = S.sb("odo", [128, T], BF16)
    tmp = {}
    for c4 in range(4):
        for nm in ("LT", "u", "F", "A", "B", "A2", "B2", "Q", "v2"):
            tmp[(nm, c4)] = S.sb(f"t_{nm}{c4}", [128, 128])
    Gs2 = [S.sb(f"Gs2_{i}", [128, 128]) for i in range(2)]; GqT2 = [S.sb(f"GqT2_{i}", [128, 128]) for i in range(2)]
    for d in range(2):
        for nm in ("vb", "X"):
            tmp[(nm, d)] = S.sb(f"t_{nm}{d}", [128, 128])
        for nm in ("ktl", "vn", "Sb"):
            tmp[(nm, d)] = S.sb(f"t_{nm}{d}", [128, 128], BF16)
        tmp[("Sf", d)] = S.sb(f"t_Sf{d}", [128, 128])
    ssn = S.sb("ssn", [128, 16]); jk = S.sb("jk", [128, 128], BF16)

    def conv_silu(src_ch, conv_ch, dst, W):
        S.dma("sp", xin[:, 0:W], k.plT[src_ch][:, 0:W], reads=[k.plT[src_ch]], writes=[xin])
        segs = [(0, T)] + ([(T, TC)] if W > T else [])
        wcol = lambda j: colT[:, C_CW + j * 48 + conv_ch:C_CW + j * 48 + conv_ch + 1]
        for si, (s0, L) in enumerate(segs):
            eng = "dve"
            S.op(eng, lambda e, s0=s0, L=L: e.tensor_scalar(out=dst[:, s0:s0 + L], in0=xin[:, s0:s0 + L], scalar1=wcol(2), scalar2=None, op0=ALU.mult), reads=[xin, colT], writes=[dst])
            for j in (0, 1, 3, 4):
                sft = j - 2
                a0 = max(0, -sft); a1 = L - max(0, sft)
                S.op(eng, lambda e, s0=s0, a0=a0, a1=a1, sft=sft, j=j: e.scalar_tensor_tensor(out=dst[:, s0 + a0:s0 + a1], in0=xin[:, s0 + a0 + sft:s0 + a1 + sft], scalar=wcol(j), in1=dst[:, s0 + a0:s0 + a1], op0=ALU.mult, op1=ALU.add),
                     reads=[xin, colT, dst], writes=[dst])
        S.op("act", lambda e: e.activation(out=dst[:, 0:W], in_=dst[:, 0:W], func=AF.Silu), reads=[dst], writes=[dst])

    def l2n(src, W, dstb, dstf, scale):
        for (t0, tw) in blocks(W):
            pa = P[7]
            S.op("act", lambda e, t0=t0, tw=tw: e.activation(out=sqb[:, 0:tw], in_=src[:, t0:t0 + tw], func=AF.Square), reads=[src], writes=[sqb])
            S.group("pe", [lambda e, tw=tw: e.matmul(pa[:, 0:tw], lhsT=ones_b[:], rhs=sqb[:, 0:tw], start=True, stop=True)], reads=[sqb, ones_b], writes=[pa])
            S.op("act", lambda e, tw=tw: e.activation(out=rsb[:, 0:tw], in_=pa[:, 0:tw], func=AF.Sqrt, bias=epsc[:], scale=1.0), reads=[pa, epsc], writes=[rsb])
            S.op("dve", lambda e, tw=tw: e.reciprocal(out=rsb[:, 0:tw], in_=rsb[:, 0:tw]), reads=[rsb], writes=[rsb])
            S.op("dve", lambda e, t0=t0, tw=tw: e.scalar_tensor_tensor(out=dstb[:, t0:t0 + tw], in0=src[:, t0:t0 + tw], scalar=scale, in1=rsb[:, 0:tw], op0=ALU.mult, op1=ALU.mult), reads=[src, rsb], writes=[dstb])
            if dstf is not None:
                S.op("pool", lambda e, t0=t0, tw=tw: e.tensor_tensor(out=dstf[:, t0:t0 + tw], in0=src[:, t0:t0 + tw], in1=rsb[:, 0:tw], op=ALU.mult), reads=[src, rsb], writes=[dstf])

    import os
    k.d4 = dict(oall=oall, Qall=Qall, QKall=QKall, kTb=kTb, qTb=qTb, vtok=vtok, ktok=ktok, gg=gg, la=la, beta=beta, lb=lb, glast=glast)
    for h in range(int(os.environ.get("K4H", "16"))):
        conv_silu(5 + h, h, kTf, NT)
        conv_silu(21 + h, 16 + h, vTf, NT)
        conv_silu(37 + h, 32 + h, qTf, T)
        l2n(kTf, NT, kTb, kTf, 1.0)
        l2n(qTf, T, qTb, None, 128 ** -0.5)
        for src, dst in ((kTf, ktok), (vTf, vtok)):
            for g in range(5):
                tiles = list(range(4 * g, min(4 * g + 4, 18)))
                pa = P[g % 2]
                S.group("pe", [lambda e, ti=ti, g=g, src=src, pa=pa: e.transpose(pa[:, sl(ti - 4 * g)], src[:, sl(ti)], ident[:]) for ti in tiles], reads=[src, ident], writes=[pa])
                S.op("act", lambda e, g=g, n=len(tiles), dst=dst, pa=pa: e.copy(out=dst[:, 4 * g:4 * g + n, :], in_=pa[:, 0:n * 128].rearrange("p (a b) -> p a b", b=128)), reads=[pa], writes=[dst])
        if lvl4 < 2:
            continue
        X = lambda nm, c: tmp[(nm, c)]
        PO = [(16, 17), (0, 1), (14, 15), (2, 3), (12, 13), (4, 5), (10, 11), (6, 7), (8, 9)]
        order = ([16, 17] + list(range(16)), [17, 16] + list(range(15, -1, -1)))
        ptr = [0, 0]; done = set()
        S.op("pool", lambda e: e.memset(oall[:], 0.0), writes=[oall])
        for d in range(2):
            S.op("pool", lambda e, d=d: e.memset(tmp[("Sf", d)][:], 0.0), writes=[tmp[("Sf", d)]])
            S.op("pool", lambda e, d=d: e.memset(tmp[("Sb", d)][:], 0.0), writes=[tmp[("Sb", d)]])
        for tp in range(9):
            pair = PO[tp]
            chains = [(sub * 2 + d, pair[sub], d) for sub in range(2) for d in range(2)]
            for sub in range(2):
                ti = pair[sub]
                lat = ti < 16
                pg = P[4 * sub]
                S.group("pe", [lambda e, ti=ti, pg=pg: e.matmul(pg[:, 256:384], lhsT=kTb[:, sl(ti)], rhs=kTb[:, sl(ti)], start=True, stop=True)], reads=[kTb], writes=[pg])
                S.op("act", lambda e, sub=sub, pg=pg: e.copy(out=Gs2[sub][:], in_=pg[:, 256:384]), reads=[pg], writes=[Gs2[sub]])
                if lat:
                    S.group("pe", [lambda e, ti=ti, pg=pg: e.matmul(pg[:, 384:512], lhsT=kTb[:, sl(ti)], rhs=qTb[:, sl(ti)], start=True, stop=True)], reads=[kTb, qTb], writes=[pg])
                    S.op("act", lambda e, sub=sub, pg=pg: e.copy(out=GqT2[sub][:], in_=pg[:, 384:512]), reads=[pg], writes=[GqT2[sub]])
            for (c, ti, d) in chains:
                lat = ti < 16; sub = c // 2
                cd = d * 16 + h
                px = P[2 * c]
                S.op("dve", lambda e, c=c, d=d, cd=cd, ti=ti: e.tensor_scalar(out=X("LT", c)[:], in0=tri[d][:], scalar1=la[:, ti, cd:cd + 1], scalar2=None, op0=ALU.mult), reads=[tri[d], la], writes=[X("LT", c)])
                S.group("pe", [lambda e, c=c, px=px: e.matmul(px[:, 0:128], lhsT=ones_f[:], rhs=X("LT", c)[:], start=True, stop=True)], reads=[ones_f, X("LT", c)], writes=[px])
                S.op("dve", lambda e, c=c, d=d, cd=cd, ti=ti, px=px: e.scalar_tensor_tensor(out=X("u", c)[:], in0=px[:, 0:128], scalar=aa[:, ti, cd:cd + 1], in1=pm[d][:], op0=ALU.subtract, op1=ALU.max), reads=[px, aa, pm[d]], writes=[X("u", c)])
                if lat:
                    S.op("dve", lambda e, c=c, d=d, cd=cd, ti=ti, px=px: e.scalar_tensor_tensor(out=X("v2", c)[:], in0=px[:, 0:128], scalar=gg[:, ti, cd:cd + 1], in1=nm_[d][:], op0=ALU.subtract, op1=ALU.min), reads=[px, gg, nm_[d]], writes=[X("v2", c)])
                S.op("act", lambda e, c=c: e.activation(out=X("F", c)[:], in_=X("u", c)[:], func=AF.Exp, scale=-1.0), reads=[X("u", c)], writes=[X("F", c)])
                S.op("dve", lambda e, c=c, sub=sub: e.scalar_tensor_tensor(out=X("A", c)[:], in0=Gs2[sub][:], scalar=-1.0, in1=X("F", c)[:], op0=ALU.mult, op1=ALU.mult), reads=[Gs2[sub], X("F", c)], writes=[X("A", c)])
                if lat:
                    S.op("act", lambda e, c=c: e.activation(out=X("v2", c)[:], in_=X("v2", c)[:], func=AF.Exp), reads=[X("v2", c)], writes=[X("v2", c)])
                    S.op("pool", lambda e, c=c, d=d, ti=ti, sub=sub: e.tensor_tensor(out=QKall[:, d * 16 + ti, :], in0=GqT2[sub][:], in1=X("v2", c)[:], op=ALU.mult), reads=[GqT2[sub], X("v2", c)], writes=[QKall])
            for (c, ti, d) in chains:
                px = P[2 * c]
                S.group("pe", [lambda e, c=c, px=px: e.transpose(px[:, 128:256], X("A", c)[:], ident[:])], reads=[X("A", c), ident], writes=[px])
                S.op("act", lambda e, c=c, px=px: e.copy(out=X("B", c)[:], in_=px[:, 128:256]), reads=[px], writes=[X("B", c)])
                S.op("dve", lambda e, c=c, px=px: e.tensor_tensor(out=X("Q", c)[:], in0=px[:, 128:256], in1=ident[:], op=ALU.add), reads=[px, ident], writes=[X("Q", c)])
            cur = {c: ("A", "B") for c in range(4)}
            for lvl in range(6):
                last = lvl == 5
                for (c, ti, d) in chains:
                    An, Bn = cur[c]
                    A2n, B2n = ("A2", "B2") if An == "A" else ("A", "B")
                    px, py = P[2 * c], P[2 * c + 1]
                    S.group("pe", [lambda e, c=c, An=An, Bn=Bn, px=px: e.matmul(px[:, 0:128], lhsT=X(Bn, c)[:], rhs=X(An, c)[:], start=True, stop=True)], reads=[X(An, c), X(Bn, c)], writes=[px])
                    S.op("act", lambda e, c=c, A2n=A2n, px=px: e.copy(out=X(A2n, c)[:], in_=px[:, 0:128]), reads=[px], writes=[X(A2n, c)])
                    if not last:
                        S.group("pe", [lambda e, c=c, An=An, Bn=Bn, py=py: e.matmul(py[:, 0:128], lhsT=X(An, c)[:], rhs=X(Bn, c)[:], start=True, stop=True)], reads=[X(An, c), X(Bn, c)], writes=[py])
                        S.op("dve", lambda e, c=c, B2n=B2n, py=py: e.tensor_copy(out=X(B2n, c)[:], in_=py[:, 0:128]), reads=[py], writes=[X(B2n, c)])
                    cur[c] = (A2n, B2n)
                for (c, ti, d) in chains:
                    A2n = cur[c][0]
                    py = P[2 * c + 1]
                    S.group("pe", [lambda e, c=c, A2n=A2n, py=py: e.matmul(py[:, 256:384], lhsT=X(A2n, c)[:], rhs=X("Q", c)[:], start=True, stop=True)], reads=[X(A2n, c), X("Q", c)], writes=[py])
                    dstQ = Qall[:, d * 18 + ti, :] if last else X("Q", c)[:]
                    dres = Qall if last else X("Q", c)
                    S.op("dve", lambda e, c=c, py=py, dstQ=dstQ: e.tensor_tensor(out=dstQ, in0=py[:, 256:384], in1=X("Q", c)[:], op=ALU.add), reads=[py, X("Q", c)], writes=[dres])
            done.update(pair)
            progressed = True
            while progressed:
                progressed = False
                for d in range(2):
                    if ptr[d] < 18 and order[d][ptr[d]] in done:
                        ti = order[d][ptr[d]]; ptr[d] += 1; progressed = True
                        lat = ti < 16; cd = d * 16 + h
                        Sf, Sb, vb, Xx, vn, ktl = (tmp[(n, d)] for n in ("Sf", "Sb", "vb", "X", "vn", "ktl"))
                        pks, pvn, pqs, po2, pds = P[0 + d], P[2 + d], P[4 + d], P[6 + d], P[6 + d]
                        S.op("pool", lambda e, ti=ti, cd=cd, vb=vb: e.tensor_scalar(out=vb[:], in0=vtok[:, ti, :], scalar1=beta[:, ti, cd:cd + 1], scalar2=None, op0=ALU.mult), reads=[vtok, beta], writes=[vb])
                        S.op("pool", lambda e, ti=ti, cd=cd, ktl=ktl: e.tensor_scalar(out=ktl[:], in0=ktok[:, ti, :], scalar1=kts[:, ti, cd:cd + 1], scalar2=None, op0=ALU.mult), reads=[ktok, kts], writes=[ktl])
                        S.group("pe", [lambda e, ti=ti, Sb=Sb, pks=pks: e.matmul(pks[:, 0:128], lhsT=kTb[:, sl(ti)], rhs=Sb[:], start=True, stop=True)], reads=[kTb, Sb], writes=[pks])
                        if lat:
                            S.group("pe", [lambda e, ti=ti, Sb=Sb, pqs=pqs: e.matmul(pqs[:, 0:128], lhsT=qTb[:, sl(ti)], rhs=Sb[:], start=True, stop=True)], reads=[qTb, Sb], writes=[pqs])
                        S.op("dve", lambda e, ti=ti, cd=cd, pks=pks, vb=vb, Xx=Xx: e.scalar_tensor_tensor(out=Xx[:], in0=pks[:, 0:128], scalar=c1[:, ti, cd:cd + 1], in1=vb[:], op0=ALU.mult, op1=ALU.add), reads=[pks, c1, vb], writes=[Xx])
                        S.group("pe", [lambda e, ti=ti, d=d, Xx=Xx, pvn=pvn: e.matmul(pvn[:, 0:128], lhsT=Qall[:, d * 18 + ti, :], rhs=Xx[:], start=True, stop=True)], reads=[Qall, Xx], writes=[pvn])
                        S.op("act", lambda e, vn=vn, pvn=pvn: e.copy(out=vn[:], in_=pvn[:, 0:128]), reads=[pvn], writes=[vn])
                        if lat:
                            S.group("pe", [lambda e, ti=ti, d=d, vn=vn, po2=po2: e.matmul(po2[:, 128:256], lhsT=QKall[:, d * 16 + ti, :], rhs=vn[:], start=True, stop=True)], reads=[QKall, vn], writes=[po2])
                            S.op("dve", lambda e, ti=ti, cd=cd, pqs=pqs: e.scalar_tensor_tensor(out=oall[:, ti, :], in0=pqs[:, 0:128], scalar=eg[:, ti, cd:cd + 1], in1=oall[:, ti, :], op0=ALU.mult, op1=ALU.add), reads=[pqs, eg, oall], writes=[oall])
                            S.op("dve", lambda e, ti=ti, po2=po2: e.tensor_tensor(out=oall[:, ti, :], in0=po2[:, 128:256], in1=oall[:, ti, :], op=ALU.add), reads=[po2, oall], writes=[oall])
                        S.group("pe", [lambda e, ktl=ktl, vn=vn, pds=pds: e.matmul(pds[:, 0:128], lhsT=ktl[:], rhs=vn[:], start=True, stop=True)], reads=[ktl, vn], writes=[pds])
                        S.op("dve", lambda e, ti=ti, cd=cd, Sf=Sf, pds=pds: e.scalar_tensor_tensor(out=Sf[:], in0=Sf[:], scalar=egl[:, ti, cd:cd + 1], in1=pds[:, 0:128], op0=ALU.mult, op1=ALU.add), reads=[Sf, egl, pds], writes=[Sf])
                        S.op("act", lambda e, Sf=Sf, Sb=Sb: e.copy(out=Sb[:], in_=Sf[:]), reads=[Sf], writes=[Sb])
        if lvl4 < 4:
            continue
        for ti in range(16):
            S.op("act", lambda e, ti=ti: e.activation(out=jk[:], in_=oall[:, ti, :], func=AF.Square, accum_out=ssn[:, ti:ti + 1]), reads=[oall], writes=[jk, ssn])
        S.op("act", lambda e: e.activation(out=ssn[:], in_=ssn[:], func=AF.Sqrt, bias=epsc[:], scale=1.0 / 128), reads=[ssn, epsc], writes=[ssn])
        S.op("dve", lambda e: e.reciprocal(out=ssn[:], in_=ssn[:]), reads=[ssn], writes=[ssn])
        S.op("dve", lambda e: e.tensor_tensor(out=oall[:], in0=oall[:], in1=ssn[:].unsqueeze(2).to_broadcast([128, 16, 128]), op=ALU.mult), reads=[oall, ssn], writes=[oall])
        S.dma("sp", xin[:, 0:T], k.plT[61 + h][:, 0:T], reads=[k.plT[61 + h]], writes=[xin])
        S.op("act", lambda e: e.activation(out=zs[:], in_=xin[:, 0:T], func=AF.Silu), reads=[xin], writes=[zs])
        for g in range(4):
            pa = P[g % 2]
            S.group("pe", [lambda e, ti=ti, g=g, pa=pa: e.transpose(pa[:, sl(ti - 4 * g)], oall[:, ti, :], ident[:]) for ti in range(4 * g, 4 * g + 4)], reads=[oall, ident], writes=[pa])
            S.op("dve", lambda e, g=g, pa=pa: e.scalar_tensor_tensor(out=odo[:, sl(g, 512)], in0=pa[:], scalar=colT[:, C_G + 14:C_G + 15], in1=zs[:, sl(g, 512)], op0=ALU.mult, op1=ALU.mult), reads=[pa, colT, zs], writes=[odo])
        S.dma("sp", k.odD[h][:], odo[:], reads=[odo], writes=[k.odD[h]])


def phase5(k):
    S, I, P, colT = k.S, k.I, k.P, k.colT
    g1bc = S.sb("g1bc", [128, D])
    S.dma("sp", g1bc[:], k.modrow.t.ap()[0, 2 * D:3 * D].partition_broadcast(128), reads=[k.modrow], writes=[g1bc])
    wr = S.sb("wr", [128, 32, 36])
    S.dma("sp", wr[:, :, 0:4], I["w_rg"].rearrange("(c p) n -> p c n", p=128), writes=[wr])
    S.dma("sp", wr[:, :, 4:36], I["w_re"].rearrange("(c p) n -> p c n", p=128), reads=[wr], writes=[wr])
    brb = S.sb("brb", [128, 36])
    S.dma("sp", brb[:, 0:4], I["b_rg"].partition_broadcast(128), writes=[brb])
    S.dma("sp", brb[:, 4:36], I["b_re"].partition_broadcast(128), reads=[brb], writes=[brb])
    minT = S.sb("minT", [128, 32, 512], BF16)
    big = S.sb("big5", [128, 16384])
    bv = big.t
    attb = Res(bv[:, 0:4096].bitcast(BF16).rearrange("p (h t) -> p h t", t=512), "attb")
    odb = Res(bv[:, 4096:8192].bitcast(BF16).rearrange("p (h t) -> p h t", t=512), "odb")
    wab = [Res(bv[:, 8192 + 2048 * i:10240 + 2048 * i].bitcast(BF16).rearrange("p (h t) -> p h t", t=256), f"wab{i}") for i in range(2)]
    rest5 = Res(bv[:, 12288:16384], "rest5")
    xs4 = bv[:].rearrange("p (a b) -> p a b", b=D)
    allbig = [attb, odb, wab[0], wab[1], rest5]
    gsb = [S.sb(f"gsb{i}", [128, 512], BF16) for i in range(2)]
    gsg = [S.sb(f"gsg{i}", [128, 512]) for i in range(2)]
    tmpa = S.sb("tmpa", [128, 512])
    wo = [S.sb(f"wo{i}", [128, 32, 128], BF16) for i in range(2)]
    xt = S.sb("xt5", [128, D]); xs = S.sb("xs5", [128, D]); junk = xs
    ss = S.sb("ss5", [128, 1]); rs = S.sb("rs5", [128, 1])
    ht = S.sb("ht5", [128, 32, 128], BF16); hf = S.sb("hf5", [128, 32, 128])
    lg = S.sb("lg", [128, 36]); sm = S.sb("sm", [128, 64])
    woav = I["w_oa"].rearrange("(h p) n -> p h n", p=128); wobv = I["w_ob"].rearrange("(h p) n -> p h n", p=128)
    woutv = I["w_out"].rearrange("(c p) n -> p c n", p=128)
    wi = 0
    for tb in range(4):
        t0 = tb * 512
        for h in range(16):
            S.dma("sp", attb[:, h, :], k.attD[h][:, t0:t0 + 512], reads=[k.attD[h], attb], writes=[attb])
            S.dma("sp", odb[:, h, :], k.odD[h][:, t0:t0 + 512], reads=[k.odD[h], odb], writes=[odb])
        for nb in range(16):
            wa = wab[0]; wbb = wab[1]
            S.dma("pool", wa[:], woav[:, :, sl(nb, 256)], writes=[wa])
            S.dma("pool", wbb[:], wobv[:, :, sl(nb, 256)], writes=[wbb])
            for ci in range(2):
                ch = nb * 2 + ci
                pa, pb = P[0], P[1]
                S.group("pe", [lambda e, h=h, ci=ci: e.matmul(pa[:, :], lhsT=wa[:, h, sl(ci)], rhs=attb[:, h, :], start=(h == 0), stop=(h == 15)) for h in range(16)], reads=[wa, attb], writes=[pa])
                S.group("pe", [lambda e, h=h, ci=ci: e.matmul(pb[:, :], lhsT=wbb[:, h, sl(ci)], rhs=odb[:, h, :], start=(h == 0), stop=(h == 15)) for h in range(16)], reads=[wbb, odb], writes=[pb])
                for j, pp in ((0, pa), (1, pb)):
                    gch = 77 + j * 32 + ch
                    S.dma("sp", gsb[j][:], k.plT[gch][:, t0:t0 + 512], reads=[k.plT[gch]], writes=[gsb[j]])
                    S.op("act", lambda e, j=j: e.activation(out=gsg[j][:], in_=gsb[j][:], func=AF.Sigmoid), reads=[gsb[j]], writes=[gsg[j]])
                S.op("dve", lambda e: e.tensor_tensor(out=tmpa[:], in0=pa[:], in1=gsg[0][:], op=ALU.mult), reads=[pa, gsg[0]], writes=[tmpa])
                S.op("dve", lambda e: e.tensor_tensor(out=gsg[1][:], in0=pb[:], in1=gsg[1][:], op=ALU.mult), reads=[pb, gsg[1]], writes=[gsg[1]])
                S.op("dve", lambda e, ch=ch: e.tensor_tensor(out=minT[:, ch, :], in0=tmpa[:], in1=gsg[1][:], op=ALU.add), reads=[tmpa, gsg[1]], writes=[minT])
        for nb in range(32):
            w = wo[wi % 2]; wi += 1
            S.dma("pool", w[:], woutv[:, :, sl(nb, 128)], writes=[w])
            pp = P[2 + nb % 2]
            for tt in range(4):
                S.group("pe", [lambda e, c=c, w=w, pp=pp, tt=tt: e.matmul(pp[:, sl(tt)], lhsT=minT[:, c, sl(tt)], rhs=w[:, c, :], start=(c == 0), stop=(c == 31)) for c in range(32)], reads=[minT, w], writes=[pp])
            S.op("dve", lambda e, pp=pp, nb=nb: e.tensor_tensor(out=xs4[:, :, sl(nb, 128)], in0=pp[:, :].rearrange("p (a b) -> p a b", b=128), in1=g1bc[:, sl(nb, 128)].unsqueeze(1).to_broadcast([128, 4, 128]), op=ALU.mult), reads=[pp, g1bc] + allbig, writes=allbig)
        for tt in range(4):
            ti = tb * 4 + tt
            S.dma("sp", xt[:], I["x"][sl(ti), :], writes=[xt])
            S.op("pool", lambda e, tt=tt: e.tensor_tensor(out=xt[:], in0=xt[:], in1=xs4[:, tt, :], op=ALU.add), reads=[xt] + allbig, writes=[xt])
            S.dma("sp", k.xl1D[sl(ti), :], xt[:], reads=[xt], writes=[k.xl1D])
            norm_transpose(k, xt, xs, junk, ss, rs, k.A2l, C_ML + 96, ht, f32copy=hf)
            S.dma("sp", k.hl2D.t.ap()[:, :, sl(ti)].rearrange("c p t -> p c t"), ht[:], reads=[ht], writes=[k.hl2D])
            pr = P[4]
            S.group("pe", [lambda e, c=c: e.matmul(pr[:, 0:36], lhsT=hf[:, c, :], rhs=wr[:, c, :], start=(c == 0), stop=(c == 31)) for c in range(32)], reads=[hf, wr], writes=[pr])
            S.op("dve", lambda e: e.tensor_tensor(out=lg[:], in0=pr[:, 0:36], in1=brb[:], op=ALU.add), reads=[pr, brb], writes=[lg])
            router(k, lg, sm, ti)


def router(k, lg, sm, ti):
    S = k.S
    g = k.gates

    def dv(fn):
        S.op("dve", fn, reads=[lg, sm, g], writes=[sm, g])
    dv(lambda e: e.tensor_reduce(out=sm[:, 0:1], in_=lg[:, 0:4], axis=AX.X, op=ALU.max))
    dv(lambda e: e.tensor_scalar(out=sm[:, 4:8], in0=lg[:, 0:4], scalar1=sm[:, 0:1], scalar2=None, op0=ALU.subtract))
    S.op("act", lambda e: e.activation(out=sm[:, 8:12], in_=sm[:, 4:8], func=AF.Exp, accum_out=sm[:, 1:2]), reads=[sm], writes=[sm])
    dv(lambda e: e.reciprocal(out=sm[:, 2:3], in_=sm[:, 1:2]))
    dv(lambda e: e.tensor_scalar(out=sm[:, 12:16], in0=lg[:, 0:4], scalar1=sm[:, 0:1], scalar2=None, op0=ALU.is_equal))
    dv(lambda e: e.tensor_scalar(out=sm[:, 12:16], in0=sm[:, 12:16], scalar1=1e4, scalar2=-1e4, op0=ALU.mult, op1=ALU.add))
    dv(lambda e: e.tensor_tensor(out=g[:, ti, :].rearrange("p (a b) -> p a b", b=8), in0=lg[:, 4:36].rearrange("p (a b) -> p a b", b=8),
                                  in1=sm[:, 12:16].unsqueeze(2).to_broadcast([128, 4, 8]), op=ALU.add))
    dv(lambda e: e.tensor_reduce(out=sm[:, 16:17], in_=g[:, ti, :], axis=AX.X, op=ALU.max))
    dv(lambda e: e.tensor_scalar(out=sm[:, 32:64], in0=g[:, ti, :], scalar1=sm[:, 16:17], scalar2=None, op0=ALU.is_equal))
    dv(lambda e: e.scalar_tensor_tensor(out=sm[:, 20:21].to_broadcast([128, 32]) if False else g[:, ti, :], in0=sm[:, 32:64], scalar=-1e4, in1=g[:, ti, :], op0=ALU.mult, op1=ALU.add))
    dv(lambda e: e.tensor_reduce(out=sm[:, 17:18], in_=g[:, ti, :], axis=AX.X, op=ALU.max))
    dv(lambda e: e.tensor_tensor(out=sm[:, 18:19], in0=sm[:, 17:18], in1=sm[:, 16:17], op=ALU.subtract))
    S.op("act", lambda e: e.activation(out=sm[:, 19:20], in_=sm[:, 18:19], func=AF.Exp), reads=[sm], writes=[sm])
    dv(lambda e: e.tensor_scalar(out=sm[:, 20:21], in0=sm[:, 19:20], scalar1=1.0, scalar2=None, op0=ALU.add))
    dv(lambda e: e.reciprocal(out=sm[:, 20:21], in_=sm[:, 20:21]))
    dv(lambda e: e.tensor_tensor(out=sm[:, 20:21], in0=sm[:, 20:21], in1=sm[:, 2:3], op=ALU.mult))
    dv(lambda e: e.tensor_tensor(out=sm[:, 21:22], in0=sm[:, 20:21], in1=sm[:, 19:20], op=ALU.mult))
    dv(lambda e: e.tensor_scalar(out=g[:, ti, :], in0=g[:, ti, :], scalar1=sm[:, 17:18], scalar2=sm[:, 21:22], op0=ALU.is_equal, op1=ALU.mult))
    dv(lambda e: e.scalar_tensor_tensor(out=g[:, ti, :], in0=sm[:, 32:64], scalar=sm[:, 20:21], in1=g[:, ti, :], op0=ALU.mult, op1=ALU.add))


def phase6(k):
    S, I, P = k.S, k.I, k.P
    g2bc = S.sb("g2bc", [128, D])
    S.dma("sp", g2bc[:], k.modrow.t.ap()[0, 5 * D:6 * D].partition_broadcast(128), reads=[k.modrow], writes=[g2bc])
    hb = S.sb("hb6", [128, 32, 512], BF16)
    yacc = S.sb("yacc", [128, 4, D])
    hid = S.sb("hid", [128, 8, 512], BF16)
    wq = [S.sb(f"wq{i}", [128, 8192], BF16) for i in range(4)]
    h1s = S.sb("h1s", [128, 512])
    xo = S.sb("xo", [128, 2048])
    hv = k.hl2D.t.ap().rearrange("c p t -> p c t")
    wc = 0
    for tb in range(4):
        t0 = tb * 512
        S.dma("sp", hb[:], hv[:, :, t0:t0 + 512], reads=[k.hl2D], writes=[hb])
        S.op("pool", lambda e: e.memset(yacc[:], 0.0), writes=[yacc])
        for ex in range(32):
            w1v = I["w1"][ex].rearrange("(c p) n -> p c n", p=128); w3v = I["w3"][ex].rearrange("(c p) n -> p c n", p=128)
            w2v = I["w2"][ex].rearrange("(c p) n -> p c n", p=128)
            for hq in range(4):
                wa = wq[wc % 4]; wc += 1
                wbq = wq[wc % 4]; wc += 1
                wa3 = wa[:].rearrange("p (c n) -> p c n", n=256); wb3 = wbq[:].rearrange("p (c n) -> p c n", n=256)
                S.dma("pool", wa3, w1v[:, :, sl(hq, 256)], writes=[wa])
                S.dma("pool", wb3, w3v[:, :, sl(hq, 256)], writes=[wbq])
                for hc2 in range(2):
                    hc = hq * 2 + hc2
                    p1, p3 = P[0 + hc % 2], P[2 + hc % 2]
                    S.group("pe", [lambda e, c=c, wa3=wa3, p1=p1, hc2=hc2: e.matmul(p1[:, :], lhsT=wa3[:, c, sl(hc2)], rhs=hb[:, c, :], start=(c == 0), stop=(c == 31)) for c in range(32)], reads=[wa, hb], writes=[p1])
                    S.group("pe", [lambda e, c=c, wb3=wb3, p3=p3, hc2=hc2: e.matmul(p3[:, :], lhsT=wb3[:, c, sl(hc2)], rhs=hb[:, c, :], start=(c == 0), stop=(c == 31)) for c in range(32)], reads=[wbq, hb], writes=[p3])
                    S.op("act", lambda e, p1=p1: e.activation(out=h1s[:], in_=p1[:], func=AF.Silu), reads=[p1], writes=[h1s])
                    S.op("dve", lambda e, p3=p3, hc=hc: e.tensor_tensor(out=hid[:, hc, :], in0=p3[:], in1=h1s[:], op=ALU.mult), reads=[p3, h1s], writes=[hid])
            for nq in range(4):
                w2 = wq[wc % 4]; wc += 1
                w23 = w2[:].rearrange("p (c n) -> p c n", n=1024)
                S.dma("pool", w23, w2v[:, :, sl(nq, 1024)], writes=[w2])
                for tt in range(4):
                    for nh in range(2):
                        nb = nq * 2 + nh
                        py = P[4 + (tt * 2 + nh) % 4]
                        S.group("pe", [lambda e, c=c, w23=w23, py=py, tt=tt, nh=nh: e.matmul(py[:, :], lhsT=hid[:, c, sl(tt)], rhs=w23[:, c, sl(nh, 512)], start=(c == 0), stop=(c == 7)) for c in range(8)], reads=[hid, w2], writes=[py])
                        S.op("dve", lambda e, py=py, tt=tt, nb=nb, ex=ex, tb=tb: e.scalar_tensor_tensor(out=yacc[:, tt, sl(nb, 512)], in0=py[:], scalar=k.gates[:, tb * 4 + tt, ex:ex + 1], in1=yacc[:, tt, sl(nb, 512)], op0=ALU.mult, op1=ALU.add),
                             reads=[py, k.gates, yacc], writes=[yacc])
        for tt in range(4):
            ti = tb * 4 + tt
            S.op("dve", lambda e, tt=tt: e.tensor_tensor(out=yacc[:, tt, :], in0=yacc[:, tt, :], in1=g2bc[:], op=ALU.mult), reads=[yacc, g2bc], writes=[yacc])
            for hf_ in range(2):
                S.dma("sp", xo[:], k.xl1D[sl(ti), sl(hf_, 2048)], reads=[k.xl1D], writes=[xo])
                S.op("pool", lambda e, tt=tt, hf_=hf_: e.tensor_tensor(out=xo[:], in0=xo[:], in1=yacc[:, tt, sl(hf_, 2048)], op=ALU.add), reads=[xo, yacc], writes=[xo])
                S.dma("sp", k.out[sl(ti), sl(hf_, 2048)], xo[:], reads=[xo], writes=[])


def build(stop_after=99, dbg=None, only=None, iso=()):
    nc = bass.Bass("TRN2", target_bir_lowering=False)
    I = declare(nc)
    out = nc.dram_tensor("out", [T, D], F32, kind="ExternalOutput").ap()
    dbg_out = None
    if dbg and not isinstance(dbg, list):
        dbg = [("dbg",) + tuple(dbg)]
    dbg_outs = []
    for (nm, fn, shp, dt_) in (dbg or []):
        dbg_outs.append(nc.dram_tensor(nm, list(shp), dt_, kind="ExternalOutput").ap())
    with contextlib.ExitStack() as gst:
        S = Sched(nc, gst)
        k = K(); k.S = S; k.I = I; k.nc = nc; k.out = out

        def scr(name, shape, dt):
            if name in iso:
                return Res(nc.dram_tensor("d_" + name, list(shape), dt, kind="ExternalInput"), name)
            return S.dram(name, shape, dt)
        k.hT = scr("hT", [32, 128, NT], BF16)
        k.plT = [scr(f"plT{c}", [128, NT], BF16) for c in range(141)]
        k.kreD = scr("kreD", [32, NT], F32); k.kroD = scr("kroD", [32, NT], F32); k.abD = scr("abD", [64, NT], F32)
        k.modrow = scr("modrow", [2, 6 * D], F32)
        k.attD = [scr(f"attD{h}", [128, T], BF16) for h in range(16)]
        k.odD = [scr(f"odD{h}", [128, T], BF16) for h in range(16)]
        k.xl1D = scr("xl1D", [T, D], F32)
        k.hl2D = scr("hl2D", [32, 128, T], BF16)
        S.tstack = gst
        k.ident = S.sb("ident", [128, 128]); S.dma("sp", k.ident[:], I["ident"], writes=[k.ident])
        k.ones_f = S.sb("ones_f", [128, 128]); S.op("pool", lambda e: e.memset(k.ones_f[:], 1.0), writes=[k.ones_f])
        k.ones_b = S.sb("ones_b", [128, 128], BF16); S.op("pool", lambda e: e.memset(k.ones_b[:], 1.0), writes=[k.ones_b])
        k.epsc = S.sb("epsc", [128, 1]); S.op("pool", lambda e: e.memset(k.epsc[:], EPS), writes=[k.epsc])
        k.colT = S.sb("colT", [128, 704])
        k.gates = S.sb("gates", [128, 16, 32])
        k.P = [S.ps(f"P{i}", [128, 512]) for i in range(8)]
        if "colT" in iso:
            cin = nc.dram_tensor("d_colT", [128, 704], F32, kind="ExternalInput").ap()
            S.dma("sp", k.colT[:], cin, writes=[k.colT])
            derived(k)
        if "gates" in iso:
            gin = nc.dram_tensor("d_gates", [128, 16, 32], F32, kind="ExternalInput").ap()
            S.dma("sp", k.gates[:], gin, writes=[k.gates])
        phases = [phase0, phase1, phase2, phase3, phase4, phase5, phase6]
        for pi, ph in enumerate(phases):
            if pi > stop_after:
                break
            if only is not None and pi not in only:
                continue
            st = contextlib.ExitStack()
            S.tstack = st
            ph(k)
            S.barrier()
            st.close()
            S.tstack = gst
            if pi == 0:
                derived(k)
        for (nm, fn, shp, dt_), do in zip(dbg or [], dbg_outs):
            S.dma("sp", do, fn(k), reads=[], writes=[])
        S.barrier()
        S.emit()
    LAST["inputs"] = set(I.keys())
    return nc


def make_inputs(inputs):
    sq = {n: np.ascontiguousarray(np.asarray(inputs[n])[0]) for n in
          ("norm1_g", "norm2_g", "w_mod", "b_mod", "w_in", "q_a_norm_g", "w_uq", "kv_norm_g", "w_ukv", "q_norm_g",
           "q_rope_norm_g", "k_norm_g", "k_rope_norm_g", "conv_w", "dn_norm_g", "w_oa", "w_ob", "w_out", "w_rg",
           "b_rg", "w_re", "b_re", "w1", "w3", "w2")}
    sq["a_log"] = np.ascontiguousarray(np.asarray(inputs["a_log"])[0].reshape(32))
    sq["dt_bias"] = np.ascontiguousarray(np.asarray(inputs["dt_bias"])[0].reshape(32))
    sq.update(_consts())
    x = np.asarray(inputs["x"]); ctx = np.asarray(inputs["ctx"]); c = np.asarray(inputs["c"]); cc = np.asarray(inputs["c_ctx"])
    maps = []
    for b in range(8):
        m = dict(sq)
        m["x"] = np.ascontiguousarray(x[b]); m["ctx"] = np.ascontiguousarray(ctx[b])
        m["c2"] = np.ascontiguousarray(np.stack([c[b], cc], axis=0))
        maps.append(m)
    return maps


LAST = {}


def nc_inputs(nc, m):
    return [n for n in m if n in LAST["inputs"]]


def kernel(**inputs):
    from concourse.bass_utils import run_bass_kernel_spmd
    nc = build()
    maps = make_inputs(inputs)
    maps = [{n: m[n] for n in nc_inputs(nc, m)} for m in maps]
    res = run_bass_kernel_spmd(nc, maps, core_ids=list(range(8)))
    return np.stack([np.asarray(r["out"]) for r in res.results], axis=0).astype(np.float32)
```

```python
import contextlib
import numpy as np
import concourse.bass as bass
import concourse.mybir as mybir

F32 = mybir.dt.float32
BF16 = mybir.dt.bfloat16
I32 = mybir.dt.int32
AF = mybir.ActivationFunctionType
ALU = mybir.AluOpType
AX = mybir.AxisListType

EPOCH = 30000
NDMASEM = 48


class Res:
    __slots__ = ("t", "w", "r", "name", "excl")

    def __init__(self, t, name="", excl=False):
        self.excl = excl
        self.t = t
        self.w = None
        self.r = []
        self.name = name

    def __getitem__(self, idx):
        return self.t[idx]


class _Proxy:
    def __getattr__(self, name):
        def f(*a, **kw):
            return (name, a, kw)
        return f


PROXY = _Proxy()


class Sched:
    ENGS = ("pe", "act", "dve", "pool", "sp")

    def __init__(self, nc, stack):
        self.nc = nc
        self.stack = stack
        self.ops = {e: [] for e in self.ENGS}
        self.count = {e: 0 for e in self.ENGS}
        self.sems = {e: [] for e in self.ENGS}
        self.waited = {e: {} for e in self.ENGS}
        self.dsem = [stack.enter_context(nc.semaphore(f"dma{i}")) for i in range(NDMASEM)]
        self.dcount = 0
        self.dlast = [0] * NDMASEM
        self.nsb = 0

    def sb(self, name, shape, dt=F32):
        t = self.tstack.enter_context(self.nc.sbuf_tensor("sb_" + name, list(shape), dt))
        return Res(t, name)

    def ps(self, name, shape, dt=F32):
        t = self.tstack.enter_context(self.nc.psum_tensor("ps_" + name, list(shape), dt))
        return Res(t, name, excl=True)

    def dram(self, name, shape, dt=F32):
        t = self.nc.dram_tensor("d_" + name, list(shape), dt, kind="Internal")
        return Res(t, name)

    def _tok(self, eng):
        self.count[eng] += 1
        k = self.count[eng]
        ep = (k - 1) // EPOCH
        while len(self.sems[eng]) <= ep:
            self.sems[eng].append(self.stack.enter_context(
                self.nc.semaphore(f"s_{eng}_{len(self.sems[eng])}")))
        return (self.sems[eng][ep], (k - 1) % EPOCH + 1, (eng, ep))

    def _need(self, eng, toks):
        waits = []
        best = {}
        for tk in toks:
            if tk is None:
                continue
            sem, val, key = tk
            if eng == "pe" and key[0] == "pe":
                continue
            if best.get(key, (None, 0))[1] < val:
                best[key] = (sem, val)
        for key, (sem, val) in best.items():
            if self.waited[eng].get(key, 0) >= val:
                continue
            self.waited[eng][key] = val
            waits.append((sem, val))
        return waits

    def _deps(self, reads, writes):
        toks = []
        for r in reads:
            toks.append(r.w)
            if r.excl:
                toks.extend(r.r)
        for w in writes:
            toks.append(w.w)
            toks.extend(w.r)
        return toks

    def _commit(self, tok, reads, writes):
        for r in reads:
            r.r.append(tok)
            if len(r.r) > 24:
                best = {}
                for t in r.r:
                    if best.get(t[2], (None, 0, None))[1] < t[1]:
                        best[t[2]] = t
                r.r = list(best.values())
        for w in writes:
            w.w = tok
            w.r = []

    def op(self, eng, fn, reads=(), writes=()):
        waits = self._need(eng, self._deps(reads, writes))
        tok = self._tok(eng)
        self.ops[eng].append((waits, fn(PROXY), (tok[0], 1)))
        self._commit(tok, reads, writes)
        return tok

    def group(self, eng, fns, reads=(), writes=()):
        waits = self._need(eng, self._deps(reads, writes))
        tok = self._tok(eng)
        n = len(fns)
        for i, fn in enumerate(fns):
            self.ops[eng].append((waits if i == 0 else [], fn(PROXY),
                                  (tok[0], 1) if i == n - 1 else None))
        self._commit(tok, reads, writes)
        return tok

    def dma(self, q, out, in_, reads=(), writes=(), **kw):
        i = self.dcount
        self.dcount += 1
        s = i % NDMASEM
        sem = self.dsem[s]
        prev = self.dlast[s]
        tgt = prev + 16
        if tgt > EPOCH:
            raise RuntimeError("dma sem overflow")
        self.dlast[s] = tgt
        toks = self._deps(reads, writes)
        if prev:
            toks.append((sem, prev, ("d", s)))
        waits = self._need(q, toks)
        tok = (sem, tgt, ("d", s))

        self.ops[q].append((waits, ("dma_start", (), dict(out=out, in_=in_, **kw)), (sem, 16)))
        self._commit(tok, reads, writes)
        return tok

    def barrier(self):
        toks = []
        for e in self.ENGS:
            k = self.count[e]
            if k:
                ep = (k - 1) // EPOCH
                toks.append((self.sems[e][ep], (k - 1) % EPOCH + 1, (e, ep)))
        for s in range(NDMASEM):
            if self.dlast[s]:
                toks.append((self.dsem[s], self.dlast[s], ("d", s)))
        for e in self.ENGS:
            waits = self._need(e, toks)
            if waits:
                self.ops[e].append((waits, None, None))

    def wait_all(self, eng, toks):
        waits = self._need(eng, toks)
        if waits:
            self.ops[eng].append((waits, None, None))

    def emit(self):
        nc = self.nc
        ops = self.ops

        def run(e, lst):
            for waits, fn, inc in lst:
                for sem, val in waits:
                    e.wait_ge(sem, val)
                if fn is None:
                    continue
                name, a, kw = fn
                ins = getattr(e, name)(*a, **kw)
                if inc is not None:
                    ins.then_inc(inc[0], inc[1])

        with nc.Block() as block:
            @block.tensor
            def _(e):
                run(e, ops["pe"])

            @block.scalar
            def _(e):
                run(e, ops["act"])

            @block.vector
            def _(e):
                run(e, ops["dve"])

            @block.gpsimd
            def _(e):
                run(e, ops["pool"])

            @block.sync
            def _(e):
                run(e, ops["sp"])

D = 4096; T = 2048; TC = 256; NT = 2304
NTILE = 18
IN_COLS = 18048
EPS = 1e-6
SCALE = 192 ** -0.5
TB = [(0, 512), (512, 512), (1024, 512), (1536, 512), (2048, 256)]
C_ML, C_MC, C_N1, C_N2, C_CW, C_G = 0, 192, 384, 416, 448, 688


def _consts():
    tri = np.triu(np.ones((128, 128), np.float32))
    i = np.arange(128)
    c = {
        "ident": np.eye(128, dtype=np.float32),
        "triF": tri, "triB": tri.T.copy(),
        "pmF": np.where(i[:, None] > i[None, :], 0.0, 1e4).astype(np.float32),
        "pmB": np.where(i[:, None] < i[None, :], 0.0, 1e4).astype(np.float32),
        "nmF": np.where(i[None, :] >= i[:, None], 0.0, -1e4).astype(np.float32),
        "nmB": np.where(i[None, :] <= i[:, None], 0.0, -1e4).astype(np.float32),
    }
    rows = T // 64
    row = np.repeat(np.arange(rows), 64).astype(np.float32)
    col = np.tile(np.arange(64), rows).astype(np.float32)
    inv = (10000.0 ** (-np.arange(0, 32, 2, dtype=np.float32) / 32)).astype(np.float32)
    ang = np.concatenate([row[:, None] * inv, col[:, None] * inv], axis=-1)
    c["cosT"] = np.cos(ang).T.astype(np.float32).copy()
    c["sinT"] = np.sin(ang).T.astype(np.float32).copy()
    return c


class K:
    pass


IN_SHAPES = {
    "x": [T, D], "ctx": [TC, D], "c2": [2, D], "norm1_g": [D], "norm2_g": [D], "w_mod": [D, 6 * D], "b_mod": [6 * D],
    "w_in": [D, IN_COLS], "q_a_norm_g": [1024], "w_uq": [1024, 3072], "kv_norm_g": [512], "w_ukv": [512, 4096],
    "q_norm_g": [128], "q_rope_norm_g": [64], "k_norm_g": [128], "k_rope_norm_g": [64], "conv_w": [5, 6144],
    "a_log": [32], "dt_bias": [32], "dn_norm_g": [128], "w_oa": [2048, D], "w_ob": [2048, D], "w_out": [D, D],
    "w_rg": [D, 4], "b_rg": [4], "w_re": [D, 32], "b_re": [32], "w1": [32, D, 1024], "w3": [32, D, 1024], "w2": [32, 1024, D],
    "ident": [128, 128], "triF": [128, 128], "triB": [128, 128], "pmF": [128, 128], "pmB": [128, 128],
    "nmF": [128, 128], "nmB": [128, 128], "cosT": [32, T], "sinT": [32, T],
}


class LazyIn(dict):
    def __init__(self, nc):
        super().__init__()
        self.nc = nc

    def __missing__(self, name):
        ap = self.nc.dram_tensor(name, list(IN_SHAPES[name]), F32, kind="ExternalInput").ap()
        self[name] = ap
        return ap


def declare(nc):
    return LazyIn(nc)


def sl(c, n=128):
    return slice(c * n, (c + 1) * n)


def phase0(k):
    S, I, P, ident, colT = k.S, k.I, k.P, k.ident, k.colT
    csrc = S.sb("csrc", [64, 128]); S.dma("sp", csrc[:], I["c2"].rearrange("j (k p) -> (j k) p", p=128), writes=[csrc])
    cT = S.sb("cT", [128, 64])
    S.group("pe", [lambda e: e.transpose(P[1][:, 0:64], csrc[:], ident[0:64, 0:64])], reads=[csrc, ident], writes=[P[1]])
    S.op("act", lambda e: e.activation(out=cT[:], in_=P[1][:, 0:64], func=AF.Silu), reads=[P[1]], writes=[cT])
    wm = [S.sb(f"wm{i}", [128, 32, 512]) for i in range(2)]
    wv = I["w_mod"].rearrange("(k p) n -> p k n", p=128)
    pm_ = P[0]
    for nb in range(48):
        w = wm[nb % 2]
        S.dma("sp", w[:], wv[:, :, sl(nb, 512)], writes=[w])
        for ci in range(4):
            ch = nb * 4 + ci
            S.group("pe", [lambda e, kk=kk, w=w, ci=ci, ch=ch: e.matmul(pm_[:, 2 * ch:2 * ch + 2], lhsT=w[:, kk, sl(ci)], rhs=cT[:, kk:64:32], start=(kk == 0), stop=(kk == 31)) for kk in range(32)],
                    reads=[cT, w], writes=[pm_])
    rows = S.sb("rows", [128, 5, 128])
    S.op("pool", lambda e: e.memset(rows[:], 0.0), writes=[rows])
    bvr = I["b_mod"].rearrange("(r p) -> r p", p=128)
    S.dma("sp", rows[:, 0, :], bvr[0:128, :], reads=[rows], writes=[rows])
    S.dma("sp", rows[0:64, 1, :], bvr[128:192, :], reads=[rows], writes=[rows])

    def rowsrc(name):
        return I[name].rearrange("(r p) -> r p", p=128)
    S.dma("sp", rows[0:32, 2, :], rowsrc("norm1_g"), reads=[rows], writes=[rows])
    S.dma("sp", rows[32:64, 2, :], rowsrc("norm2_g"), reads=[rows], writes=[rows])
    cwv = I["conv_w"].rearrange("j (r p) -> (j r) p", p=128)
    S.dma("sp", rows[0:128, 3, :], cwv[0:128, :], reads=[rows], writes=[rows])
    S.dma("sp", rows[0:112, 4, :], cwv[128:240, :], reads=[rows], writes=[rows])
    S.dma("sp", rows[112:116, 4, :], rowsrc("kv_norm_g"), reads=[rows], writes=[rows])
    S.dma("sp", rows[116:124, 4, :], rowsrc("q_a_norm_g"), reads=[rows], writes=[rows])
    S.dma("sp", rows[124:125, 4, :], rowsrc("q_norm_g"), reads=[rows], writes=[rows])
    S.dma("sp", rows[125:126, 4, :], rowsrc("k_norm_g"), reads=[rows], writes=[rows])
    S.dma("sp", rows[126:127, 4, :], rowsrc("dn_norm_g"), reads=[rows], writes=[rows])
    bcol = S.sb("bcol", [128, 192])
    for i, (dst, off, n) in enumerate([(bcol, 0, 128), (bcol, 128, 64), (colT, 384, 64), (colT, 448, 128), (colT, 576, 128)]):
        pb = P[2 + i % 2]
        S.group("pe", [lambda e, i=i, n=n, pb=pb: e.transpose(pb[:, 0:n], rows[0:n, i, :], ident[0:n, 0:n])], reads=[rows, ident], writes=[pb])
        S.op("dve", lambda e, dst=dst, off=off, n=n, pb=pb: e.tensor_copy(out=dst[:, off:off + n], in_=pb[:, 0:n]), reads=[pb], writes=[dst])
    S.op("dve", lambda e: e.tensor_tensor(out=colT[:, C_ML:C_ML + 192], in0=pm_[:, 0:384:2], in1=bcol[:], op=ALU.add), reads=[pm_, bcol, colT], writes=[colT])
    S.op("dve", lambda e: e.tensor_tensor(out=colT[:, C_MC:C_MC + 192], in0=pm_[:, 1:384:2], in1=bcol[:], op=ALU.add), reads=[pm_, bcol, colT], writes=[colT])


def bcast_row(k, col0, dst, dgs):
    S, P = k.S, k.P
    for g in range(8):
        pb = P[6 + g % 2]
        for c4 in range(4):
            c = 4 * g + c4
            dg = dgs[c % 2]
            S.op("dve", lambda e, dg=dg, c=c: e.tensor_scalar(out=dg[:], in0=k.ident[:], scalar1=k.colT[:, col0 + c:col0 + c + 1], scalar2=None, op0=ALU.mult), reads=[k.ident, k.colT], writes=[dg])
            S.group("pe", [lambda e, dg=dg, c4=c4, pb=pb: e.matmul(pb[:, sl(c4)], lhsT=k.ones_f[:], rhs=dg[:], start=True, stop=True)], reads=[k.ones_f, dg], writes=[pb])
        S.op("act", lambda e, g=g, pb=pb: e.copy(out=dst[:, sl(g, 512)], in_=pb[:]), reads=[pb], writes=[dst])


def derived(k):
    S, colT = k.S, k.colT
    k.A1l = S.sb("A1l", [128, 32]); k.A1c = S.sb("A1c", [128, 32]); k.A2l = S.sb("A2l", [128, 32])
    for A, sc in ((k.A1l, C_ML + 32), (k.A1c, C_MC + 32)):
        S.op("dve", lambda e, A=A, sc=sc: e.scalar_tensor_tensor(out=A[:], in0=colT[:, sc:sc + 32], scalar=1.0, in1=colT[:, C_N1:C_N1 + 32], op0=ALU.add, op1=ALU.mult), reads=[colT], writes=[A])
    S.op("dve", lambda e: e.scalar_tensor_tensor(out=k.A2l[:], in0=colT[:, C_ML + 128:C_ML + 160], scalar=1.0, in1=colT[:, C_N2:C_N2 + 32], op0=ALU.add, op1=ALU.mult), reads=[colT], writes=[k.A2l])


def norm_transpose(k, xt, xs, junk, ss, rs, A, shoff, dstT, f32copy=None):
    S, P, ident, colT = k.S, k.P, k.ident, k.colT
    S.op("act", lambda e: e.activation(out=junk[:], in_=xt[:], func=AF.Square, accum_out=ss[:]), reads=[xt], writes=[junk, ss])
    S.op("act", lambda e: e.activation(out=rs[:], in_=ss[:], func=AF.Sqrt, bias=k.epsc[:], scale=1.0 / D), reads=[ss, k.epsc], writes=[rs])
    S.op("dve", lambda e: e.reciprocal(out=rs[:], in_=rs[:]), reads=[rs], writes=[rs])
    S.op("dve", lambda e: e.tensor_scalar(out=xs[:], in0=xt[:], scalar1=rs[:, 0:1], scalar2=None, op0=ALU.mult), reads=[xt, rs], writes=[xs])
    for g in range(8):
        pb = P[4 + g % 4]
        S.group("pe", [lambda e, c=c, pb=pb, g=g: e.transpose(pb[:, sl(c - 4 * g)], xs[:, sl(c)], ident[:]) for c in range(4 * g, 4 * g + 4)],
                reads=[xs, ident], writes=[pb])
        for c in range(4 * g, 4 * g + 4):
            if c % 2:
                S.op("act", lambda e, c=c, pb=pb, g=g: e.activation(out=dstT[:, c, :], in_=pb[:, sl(c - 4 * g)], func=AF.Identity, scale=A[:, c:c + 1], bias=colT[:, shoff + c:shoff + c + 1]),
                     reads=[pb, A, colT], writes=[dstT])
            else:
                S.op("dve", lambda e, c=c, pb=pb, g=g: e.tensor_scalar(out=dstT[:, c, :], in0=pb[:, sl(c - 4 * g)], scalar1=A[:, c:c + 1], scalar2=colT[:, shoff + c:shoff + c + 1], op0=ALU.mult, op1=ALU.add),
                     reads=[pb, A, colT], writes=[dstT])
            if f32copy is not None:
                S.op("dve", lambda e, c=c, pb=pb, g=g: e.tensor_scalar(out=f32copy[:, c, :], in0=pb[:, sl(c - 4 * g)], scalar1=A[:, c:c + 1], scalar2=colT[:, shoff + c:shoff + c + 1], op0=ALU.mult, op1=ALU.add),
                     reads=[pb, A, colT], writes=[f32copy])


def phase1(k):
    S, I = k.S, k.I
    xts = [S.sb(f"xt{i}", [128, D]) for i in range(2)]
    xss = [S.sb(f"xs{i}", [128, D]) for i in range(2)]
    hts = [S.sb(f"ht{i}", [128, 32, 128], BF16) for i in range(2)]
    junk = S.sb("junk", [128, D], BF16)
    sss = [S.sb(f"ss{i}", [128, 1]) for i in range(2)]
    rss = [S.sb(f"rs{i}", [128, 1]) for i in range(2)]
    for ti in range(NTILE):
        xt = xts[ti % 2]; ht = hts[ti % 2]
        src = I["x"][sl(ti), :] if ti < 16 else I["ctx"][sl(ti - 16), :]
        S.dma("sp", xt[:], src, writes=[xt])
        norm_transpose(k, xt, xss[ti % 2], junk, sss[ti % 2], rss[ti % 2], k.A1l if ti < 16 else k.A1c, C_ML if ti < 16 else C_MC, ht)
        S.dma("sp", k.hT.t.ap()[:, :, sl(ti)].rearrange("c p t -> p c t"), ht[:], reads=[ht], writes=[k.hT])


def phase2(k):
    S, I, P = k.S, k.I, k.P
    wb = [S.sb(f"wb{i}", [128, 32, 1024], BF16) for i in range(2)]
    hb = [S.sb(f"hb{i}", [128, 32, 512], BF16) for i in range(2)]
    ob = [S.sb(f"ob{i}", [128, 512], BF16) for i in range(2)]
    of = [S.sb(f"of{i}", [64, 512]) for i in range(3)]
    wv = I["w_in"].rearrange("(k p) n -> p k n", p=128)
    hv = k.hT.t.ap().rearrange("c p t -> p c t")
    nblk = [(n0, min(1024, IN_COLS - n0)) for n0 in range(0, IN_COLS, 1024)]
    cnt = 0
    hcnt = 0
    for bi, (n0, nw) in enumerate(nblk):
        w = wb[bi % 2]
        S.dma("pool", w[:, :, 0:nw], wv[:, :, n0:n0 + nw], writes=[w])
        for (t0, tw) in TB:
            if t0 >= T and n0 >= 4736:
                continue
            h = hb[hcnt % 2]; hcnt += 1
            S.dma("sp", h[:, :, 0:tw], hv[:, :, t0:t0 + tw], reads=[k.hT], writes=[h])
            for ci in range(nw // 128):
                ch = n0 // 128 + ci
                pb = P[cnt % 4]; o = ob[cnt % 2]; cnt += 1
                S.group("pe", [lambda e, kk=kk, w=w, h=h, pb=pb, ci=ci, tw=tw: e.matmul(pb[:, 0:tw], lhsT=w[:, kk, sl(ci)], rhs=h[:, kk, 0:tw], start=(kk == 0), stop=(kk == 31)) for kk in range(32)],
                        reads=[w, h], writes=[pb])
                if cnt % 2:
                    S.op("act", lambda e, o=o, pb=pb, tw=tw: e.copy(out=o[:, 0:tw], in_=pb[:, 0:tw]), reads=[pb], writes=[o])
                else:
                    S.op("dve", lambda e, o=o, pb=pb, tw=tw: e.tensor_copy(out=o[:, 0:tw], in_=pb[:, 0:tw]), reads=[pb], writes=[o])
                S.dma("sp", k.plT[ch][:, t0:t0 + tw], o[:, 0:tw], reads=[o], writes=[k.plT[ch]])
                if ch == 4:
                    for j, (dst, lsl, m) in enumerate(((k.kreD, slice(512 - n0, 576 - n0, 2), 32), (k.kroD, slice(513 - n0, 576 - n0, 2), 32), (k.abD, slice(576 - n0, 640 - n0), 64))):
                        pb2 = P[4 + j]; o2 = of[j]
                        S.group("pe", [lambda e, kk=kk, w=w, h=h, pb2=pb2, lsl=lsl, m=m, tw=tw: e.matmul(pb2[0:m, 0:tw], lhsT=w[:, kk, lsl], rhs=h[:, kk, 0:tw], start=(kk == 0), stop=(kk == 31)) for kk in range(32)],
                                reads=[w, h], writes=[pb2])
                        S.op("dve", lambda e, o2=o2, pb2=pb2, m=m, tw=tw: e.tensor_copy(out=o2[0:m, 0:tw], in_=pb2[0:m, 0:tw]), reads=[pb2], writes=[o2])
                        S.dma("sp", dst[:, t0:t0 + tw], o2[0:m, 0:tw], reads=[o2], writes=[dst])


def blocks(W):
    return [(t0, min(512, W - t0)) for t0 in range(0, W, 512)]


def phase3(k):
    import os
    lvl = int(os.environ.get("K3", "9"))
    S, I, P, colT = k.S, k.I, k.P, k.colT
    ones_b, ones_f, epsc = k.ones_b, k.ones_f, k.epsc
    wukvs = [S.sb(f"wukv{i}", [128, 4, 256], BF16) for i in range(2)]
    wuqs = [S.sb(f"wuq{i}", [128, 8, 192], BF16) for i in range(2)]
    wukv_v = I["w_ukv"].rearrange("(c p) n -> p c n", p=128); wuq_v = I["w_uq"].rearrange("(c p) n -> p c n", p=128)
    ckvn = S.sb("ckvn", [128, 4, NT], BF16)
    cqn = S.sb("cqn", [128, 8, T], BF16)
    kr0 = S.sb("kr0", [32, NT], BF16); kr1 = S.sb("kr1", [32, NT], BF16)
    rbc = S.sb("rbc", [128, NT])
    cosT = S.sb("cosT", [32, T]); sinT = S.sb("sinT", [32, T])
    S.dma("sp", cosT[:], I["cosT"], writes=[cosT]); S.dma("sp", sinT[:], I["sinT"], writes=[sinT])
    sqs = [S.sb(f"sq{i}", [128, 512], BF16) for i in range(3)]
    rsq = [S.sb(f"rsq{i}", [128, 512]) for i in range(2)]
    grope = S.sb("grope", [32, 4])
    for j, (nm, par) in enumerate((("q_rope_norm_g", 0), ("q_rope_norm_g", 1), ("k_rope_norm_g", 0), ("k_rope_norm_g", 1))):
        S.dma("sp", grope[:, j:j + 1], I[nm].rearrange("(i two) -> i two", two=2)[:, par:par + 1], reads=[grope], writes=[grope], allow_slow_non_contiguous=True)
    grow = S.sb("grow", [1, 384]); gm = S.sb("gm", [1, 8]); negb = S.sb("negb", [128, 1])
    for j, (nm, o, n) in enumerate((("q_norm_g", 0, 128), ("k_norm_g", 128, 128), ("q_rope_norm_g", 256, 64), ("k_rope_norm_g", 320, 64))):
        S.dma("sp", grow[:, o:o + n], I[nm].rearrange("(o n) -> o n", o=1), reads=[grow], writes=[grow])
        S.op("dve", lambda e, j=j, o=o, n=n: e.tensor_reduce(out=gm[:, j:j + 1], in_=grow[:, o:o + n], axis=AX.X, op=ALU.max, apply_absolute_value=True), reads=[grow, gm], writes=[gm])
    S.op("dve", lambda e: e.tensor_tensor(out=gm[:, 4:5], in0=gm[:, 0:1], in1=gm[:, 1:2], op=ALU.mult), reads=[gm], writes=[gm])
    S.op("dve", lambda e: e.tensor_tensor(out=gm[:, 5:6], in0=gm[:, 2:3], in1=gm[:, 3:4], op=ALU.mult), reads=[gm], writes=[gm])
    S.op("dve", lambda e: e.tensor_scalar(out=gm[:, 4:5], in0=gm[:, 4:5], scalar1=-128.0 * SCALE, scalar2=None, op0=ALU.mult), reads=[gm], writes=[gm])
    S.op("dve", lambda e: e.scalar_tensor_tensor(out=gm[:, 6:7], in0=gm[:, 5:6], scalar=-64.0 * SCALE, in1=gm[:, 4:5], op0=ALU.mult, op1=ALU.add), reads=[gm], writes=[gm])
    S.group("pe", [lambda e: e.matmul(P[7][:, 0:1], lhsT=ones_f[0:1, :], rhs=gm[:, 6:7], start=True, stop=True)], reads=[ones_f, gm], writes=[P[7]])
    S.op("dve", lambda e: e.tensor_copy(out=negb[:], in_=P[7][:, 0:1]), reads=[P[7]], writes=[negb])

    if lvl < 1:
        return
    cnt = [0]

    def sumsq_rstd(srcs, parts, nfeat, t0, tw, dst_ap, dst_res):
        pb = P[7]
        n = len(srcs)
        for ci, (res, ap) in enumerate(srcs):
            sq = sqs[cnt[0] % 3]; cnt[0] += 1
            S.op("act", lambda e, sq=sq, ap=ap: e.activation(out=sq[0:parts, 0:tw], in_=ap, func=AF.Square), reads=[res], writes=[sq])
            S.group("pe", [lambda e, sq=sq, ci=ci: e.matmul(pb[0:parts, 0:tw], lhsT=ones_b[0:parts, 0:parts], rhs=sq[0:parts, 0:tw], start=(ci == 0), stop=(ci == n - 1))],
                    reads=[sq, ones_b], writes=[pb])
        S.op("act", lambda e: e.activation(out=dst_ap, in_=pb[0:parts, 0:tw], func=AF.Sqrt, bias=epsc[0:parts, :], scale=1.0 / nfeat), reads=[pb, epsc], writes=[dst_res])
        S.op("dve", lambda e: e.reciprocal(out=dst_ap, in_=dst_ap), reads=[dst_res], writes=[dst_res])

    ld = [S.sb(f"ld{i}", [128, NT], BF16) for i in range(8)]
    for c in range(4):
        S.dma("sp", ld[c][:], k.plT[c][:], reads=[k.plT[c]], writes=[ld[c]])
    for (t0, tw) in blocks(NT):
        sumsq_rstd([(ld[c], ld[c][:, t0:t0 + tw]) for c in range(4)], 128, 512, t0, tw, rbc[:, t0:t0 + tw], rbc)
    for c in range(4):
        S.op("dve", lambda e, c=c: e.scalar_tensor_tensor(out=ckvn[:, c, :], in0=ld[c][:], scalar=colT[:, C_G + c:C_G + c + 1], in1=rbc[:], op0=ALU.mult, op1=ALU.mult), reads=[ld[c], colT, rbc], writes=[ckvn])
    for c in range(8):
        S.dma("sp", ld[c][:, 0:T], k.plT[53 + c][:, 0:T], reads=[k.plT[53 + c]], writes=[ld[c]])
    for (t0, tw) in blocks(T):
        sumsq_rstd([(ld[c], ld[c][:, t0:t0 + tw]) for c in range(8)], 128, 1024, t0, tw, rbc[:, t0:t0 + tw], rbc)
    for c in range(8):
        S.op("dve", lambda e, c=c: e.scalar_tensor_tensor(out=cqn[:, c, :], in0=ld[c][:, 0:T], scalar=colT[:, C_G + 4 + c:C_G + 5 + c], in1=rbc[:, 0:T], op0=ALU.mult, op1=ALU.mult), reads=[ld[c], colT, rbc], writes=[cqn])

    if lvl < 2:
        return
    t1 = S.sb("t1", [32, 512]); t2 = S.sb("t2", [32, 512])

    def rope(ne, no, nres, t0, tw, d0, d1, dres0, dres1, pos0):
        cs = cosT[:, pos0:pos0 + tw]; sn = sinT[:, pos0:pos0 + tw]
        S.op("dve", lambda e: e.tensor_tensor(out=t1[:, 0:tw], in0=ne, in1=cs, op=ALU.mult), reads=[nres, cosT], writes=[t1])
        S.op("pool", lambda e: e.tensor_tensor(out=t2[:, 0:tw], in0=no, in1=sn, op=ALU.mult), reads=[nres, sinT], writes=[t2])
        S.op("dve", lambda e: e.tensor_tensor(out=d0, in0=t1[:, 0:tw], in1=t2[:, 0:tw], op=ALU.subtract), reads=[t1, t2], writes=[dres0])
        S.op("dve", lambda e: e.tensor_tensor(out=t1[:, 0:tw], in0=ne, in1=sn, op=ALU.mult), reads=[nres, sinT], writes=[t1])
        S.op("pool", lambda e: e.tensor_tensor(out=t2[:, 0:tw], in0=no, in1=cs, op=ALU.mult), reads=[nres, cosT], writes=[t2])
        S.op("dve", lambda e: e.tensor_tensor(out=d1, in0=t1[:, 0:tw], in1=t2[:, 0:tw], op=ALU.add), reads=[t1, t2], writes=[dres1])

    kre = S.sb("kre", [32, NT]); kro = S.sb("kro", [32, NT]); nn = S.sb("nn", [32, 2, 512])
    S.dma("sp", kre[:], k.kreD[:], reads=[k.kreD], writes=[kre]); S.dma("sp", kro[:], k.kroD[:], reads=[k.kroD], writes=[kro])
    for (t0, tw) in blocks(NT):
        rs = rsq[0]
        sumsq_rstd([(kre, kre[:, t0:t0 + tw]), (kro, kro[:, t0:t0 + tw])], 32, 64, t0, tw, rs[0:32, 0:tw], rs)
        S.op("dve", lambda e, t0=t0, tw=tw, rs=rs: e.scalar_tensor_tensor(out=nn[:, 0, 0:tw], in0=kre[:, t0:t0 + tw], scalar=grope[:, 2:3], in1=rs[0:32, 0:tw], op0=ALU.mult, op1=ALU.mult), reads=[kre, grope, rs], writes=[nn])
        S.op("dve", lambda e, t0=t0, tw=tw, rs=rs: e.scalar_tensor_tensor(out=nn[:, 1, 0:tw], in0=kro[:, t0:t0 + tw], scalar=grope[:, 3:4], in1=rs[0:32, 0:tw], op0=ALU.mult, op1=ALU.mult), reads=[kro, grope, rs, nn], writes=[nn])
        if t0 < T:
            rope(nn[:, 0, 0:tw], nn[:, 1, 0:tw], nn, t0, tw, kr0[:, t0:t0 + tw], kr1[:, t0:t0 + tw], kr0, kr1, t0)
        else:
            S.op("dve", lambda e, t0=t0, tw=tw: e.tensor_copy(out=kr0[:, t0:t0 + tw], in_=nn[:, 0, 0:tw]), reads=[nn], writes=[kr0])
            S.op("dve", lambda e, t0=t0, tw=tw: e.tensor_copy(out=kr1[:, t0:t0 + tw], in_=nn[:, 1, 0:tw]), reads=[nn], writes=[kr1])

    if lvl < 3:
        return
    knT = S.sb("knT", [128, NT], BF16); vsb = S.sb("vsb", [128, 18, 128], BF16)
    qnT = S.sb("qnT", [128, T], BF16); qr0 = S.sb("qr0", [32, T], BF16); qr1 = S.sb("qr1", [32, T], BF16)
    pTs = [S.sb(f"pT{i}", [128, 512], BF16) for i in range(3)]
    rec = S.sb("rec", [128, 512]); ao = [S.sb(f"ao{i}", [128, 512], BF16) for i in range(2)]
    k.d3 = dict(negb=negb, knT=knT, qnT=qnT, kr0=kr0, kr1=kr1, qr0=qr0, qr1=qr1, vsb=vsb, rec=rec, pT=pTs[0], ao1=ao[1], ao0=ao[0], ckvn=ckvn, cqn=cqn, rbc=rbc, gm=gm)
    pc = 0
    for h in range(16 if lvl >= 9 else 1):
        wukv = wukvs[h % 2]; wuq = wuqs[h % 2]
        S.dma("pool", wukv[:], wukv_v[:, :, h * 256:(h + 1) * 256], writes=[wukv])
        S.dma("pool", wuq[:], wuq_v[:, :, h * 192:(h + 1) * 192], writes=[wuq])
        for (t0, tw) in blocks(NT):
            pa = P[6]
            S.group("pe", [lambda e, c=c, t0=t0, tw=tw, wukv=wukv: e.matmul(pa[:, 0:tw], lhsT=wukv[:, c, 0:128], rhs=ckvn[:, c, t0:t0 + tw], start=(c == 0), stop=(c == 3)) for c in range(4)],
                    reads=[wukv, ckvn], writes=[pa])
            rs = rsq[1]
            sumsq_rstd([(pa, pa[:, 0:tw])], 128, 128, t0, tw, rs[:, 0:tw], rs)
            S.op("dve", lambda e, t0=t0, tw=tw, rs=rs, pa=pa: e.scalar_tensor_tensor(out=knT[:, t0:t0 + tw], in0=pa[:, 0:tw], scalar=colT[:, C_G + 13:C_G + 14], in1=rs[:, 0:tw], op0=ALU.mult, op1=ALU.mult), reads=[pa, colT, rs], writes=[knT])
        for g in range(5):
            pa = P[6]
            tiles = list(range(4 * g, min(4 * g + 4, 18)))
            for ti in tiles:
                S.group("pe", [lambda e, c=c, ti=ti, g=g, wukv=wukv: e.matmul(pa[:, sl(ti - 4 * g)], lhsT=ckvn[:, c, sl(ti)], rhs=wukv[:, c, 128:256], start=(c == 0), stop=(c == 3)) for c in range(4)],
                        reads=[wukv, ckvn], writes=[pa])
            S.op("act", lambda e, g=g, n=len(tiles): e.copy(out=vsb[:, 4 * g:4 * g + n, :], in_=pa[:, 0:n * 128].rearrange("p (a b) -> p a b", b=128)), reads=[pa], writes=[vsb])
        for (t0, tw) in blocks(T):
            pa = P[6]
            S.group("pe", [lambda e, c=c, t0=t0, tw=tw, wuq=wuq: e.matmul(pa[:, 0:tw], lhsT=wuq[:, c, 0:128], rhs=cqn[:, c, t0:t0 + tw], start=(c == 0), stop=(c == 7)) for c in range(8)],
                    reads=[wuq, cqn], writes=[pa])
            rs = rsq[1]
            sumsq_rstd([(pa, pa[:, 0:tw])], 128, 128, t0, tw, rs[:, 0:tw], rs)
            S.op("dve", lambda e, t0=t0, tw=tw, rs=rs, pa=pa: e.scalar_tensor_tensor(out=qnT[:, t0:t0 + tw], in0=pa[:, 0:tw], scalar=colT[:, C_G + 12:C_G + 13], in1=rs[:, 0:tw], op0=ALU.mult, op1=ALU.mult), reads=[pa, colT, rs], writes=[qnT])
            pe_, po_ = P[4], P[5]
            for par, pp in ((0, pe_), (1, po_)):
                S.group("pe", [lambda e, c=c, t0=t0, tw=tw, par=par, pp=pp, wuq=wuq: e.matmul(pp[0:32, 0:tw], lhsT=wuq[:, c, 128 + par:192:2], rhs=cqn[:, c, t0:t0 + tw], start=(c == 0), stop=(c == 7)) for c in range(8)],
                        reads=[wuq, cqn], writes=[pp])
            rs = rsq[0]
            sumsq_rstd([(pe_, pe_[0:32, 0:tw]), (po_, po_[0:32, 0:tw])], 32, 64, t0, tw, rs[0:32, 0:tw], rs)
            S.op("dve", lambda e, tw=tw, rs=rs: e.scalar_tensor_tensor(out=nn[:, 0, 0:tw], in0=pe_[0:32, 0:tw], scalar=grope[:, 0:1], in1=rs[0:32, 0:tw], op0=ALU.mult, op1=ALU.mult), reads=[pe_, grope, rs], writes=[nn])
            S.op("dve", lambda e, tw=tw, rs=rs: e.scalar_tensor_tensor(out=nn[:, 1, 0:tw], in0=po_[0:32, 0:tw], scalar=grope[:, 1:2], in1=rs[0:32, 0:tw], op0=ALU.mult, op1=ALU.mult), reads=[po_, grope, rs, nn], writes=[nn])
            rope(nn[:, 0, 0:tw], nn[:, 1, 0:tw], nn, t0, tw, qr0[:, t0:t0 + tw], qr1[:, t0:t0 + tw], qr0, qr1, t0)
        if lvl < 4:
            break
        for qb in range(4):
            q0 = qb * 512
            po = P[2 + qb % 2]; pd = P[4 + qb % 2]
            prev = None
            for kc in range(19):
                if kc < 18:
                    ps_ = P[kc % 2]
                    S.group("pe", [lambda e, ps_=ps_, kc=kc: e.matmul(ps_[:, :], lhsT=knT[:, sl(kc)], rhs=qnT[:, q0:q0 + 512], start=True, stop=False),
                                   lambda e, ps_=ps_, kc=kc: e.matmul(ps_[:, :], lhsT=kr0[:, sl(kc)], rhs=qr0[:, q0:q0 + 512], start=False, stop=False),
                                   lambda e, ps_=ps_, kc=kc: e.matmul(ps_[:, :], lhsT=kr1[:, sl(kc)], rhs=qr1[:, q0:q0 + 512], start=False, stop=True)],
                            reads=[knT, qnT, kr0, kr1, qr0, qr1], writes=[ps_])
                    pT = pTs[pc % 3]; pc += 1
                    S.op("act", lambda e, pT=pT, ps_=ps_: e.activation(out=pT[:], in_=ps_[:], func=AF.Exp, bias=negb[:, 0:1], scale=SCALE), reads=[ps_, negb], writes=[pT])
                if prev is not None and lvl >= 5:
                    pkc, ppT = prev
                    S.group("pe", [lambda e, pkc=pkc, ppT=ppT: e.matmul(pd[:, :], lhsT=ones_b[:], rhs=ppT[:], start=(pkc == 0), stop=(pkc == 17)),
                                   lambda e, pkc=pkc, ppT=ppT: e.matmul(po[:, :], lhsT=vsb[:, pkc, :], rhs=ppT[:], start=(pkc == 0), stop=(pkc == 17))],
                            reads=[ones_b, vsb, ppT], writes=[pd, po])
                prev = (kc, pT) if kc < 18 else None
            if lvl < 6:
                continue
            S.op("act", lambda e, pd=pd: e.copy(out=rec[:], in_=pd[:]), reads=[pd], writes=[rec])
            S.op("dve", lambda e: e.reciprocal(out=rec[:], in_=rec[:]), reads=[rec], writes=[rec])
            a = ao[qb % 2]
            if lvl < 7:
                continue
            S.op("dve", lambda e, a=a, po=po: e.tensor_tensor(out=a[:], in0=po[:], in1=rec[:], op=ALU.mult), reads=[po, rec], writes=[a])
            if lvl < 8:
                continue
            S.dma("sp", k.attD[h][:, q0:q0 + 512], a[:], reads=[a], writes=[k.attD[h]])
            if os.environ.get("K3BAR"):
                S.barrier()


def phase4(k):
    S, I, P, colT = k.S, k.I, k.P, k.colT
    ident, ones_f, ones_b, epsc = k.ident, k.ones_f, k.ones_b, k.epsc
    NG = 18 * 32
    cons = {}
    for nm in ("triF", "triB", "pmF", "pmB", "nmF", "nmB"):
        cons[nm] = S.sb("c_" + nm, [128, 128]); S.dma("sp", cons[nm][:], I[nm], writes=[cons[nm]])
    tri = (cons["triF"], cons["triB"]); pm = (cons["pmF"], cons["pmB"]); nm_ = (cons["nmF"], cons["nmB"])
    abT = S.sb("abT", [64, NT]); S.dma("sp", abT[:], k.abD[:], reads=[k.abD], writes=[abT])
    ab = S.sb("ab", [128, 18, 64])
    for g in range(5):
        tiles = list(range(4 * g, min(4 * g + 4, 18)))
        pa = P[g % 2]
        S.group("pe", [lambda e, ti=ti, g=g: e.transpose(pa[:, (ti - 4 * g) * 64:(ti - 4 * g + 1) * 64], abT[:, sl(ti)], ident[0:64, 0:64]) for ti in tiles], reads=[abT, ident], writes=[pa])
        S.op("dve", lambda e, g=g, n=len(tiles), pa=pa: e.tensor_copy(out=ab[:, 4 * g:4 * g + n, :], in_=pa[:, 0:n * 64].rearrange("p (a b) -> p a b", b=64)), reads=[pa], writes=[ab])
    dtb = S.sb("dtb", [128, 32]); negA = S.sb("negA", [128, 32])
    S.dma("sp", dtb[:], I["dt_bias"].partition_broadcast(128), writes=[dtb])
    S.dma("sp", negA[:], I["a_log"].partition_broadcast(128), writes=[negA])
    S.op("act", lambda e: e.activation(out=negA[:], in_=negA[:], func=AF.Exp), reads=[negA], writes=[negA])
    S.op("dve", lambda e: e.tensor_scalar(out=negA[:], in0=negA[:], scalar1=-1.0, scalar2=None, op0=ALU.mult), reads=[negA], writes=[negA])
    names = ("la", "lb", "beta", "g", "glast", "a", "eg", "c1", "kts", "egl")
    G_ = {n: S.sb("g_" + n, [128, 18, 32]) for n in names}
    la, lb, beta, gg, glast, aa, eg, c1, kts, egl = (G_[n] for n in names)
    one_c = ones_f[:, 0:1]
    bc = lambda t: t[:].unsqueeze(1).to_broadcast([128, 18, 32])
    S.op("dve", lambda e: e.tensor_tensor(out=la[:], in0=ab[:, :, 0:32], in1=bc(dtb), op=ALU.add), reads=[ab, dtb], writes=[la])
    S.op("act", lambda e: e.activation(out=la[:], in_=la[:], func=AF.Exp), reads=[la], writes=[la])
    S.op("act", lambda e: e.activation(out=la[:], in_=la[:], func=AF.Ln, bias=one_c), reads=[la, ones_f], writes=[la])
    S.op("dve", lambda e: e.tensor_tensor(out=la[:], in0=la[:], in1=bc(negA), op=ALU.mult), reads=[la, negA], writes=[la])
    S.op("act", lambda e: e.activation(out=lb[:], in_=ab[:, :, 32:64], func=AF.Exp, scale=-1.0), reads=[ab], writes=[lb])
    S.op("act", lambda e: e.activation(out=lb[:], in_=lb[:], func=AF.Ln, bias=one_c), reads=[lb, ones_f], writes=[lb])
    S.op("act", lambda e: e.activation(out=beta[:], in_=lb[:], func=AF.Exp, scale=-1.0), reads=[lb], writes=[beta])
    S.op("dve", lambda e: e.tensor_scalar(out=lb[:], in0=lb[:], scalar1=-1.0, scalar2=None, op0=ALU.mult), reads=[lb], writes=[lb])
    for half in range(2):
        tiles = list(range(9 * half, 9 * half + 9))
        pa, pb = P[2], P[3]
        for ti in tiles:
            j = ti - 9 * half
            S.group("pe", [lambda e, ti=ti, j=j: e.matmul(pa[:, j * 32:j * 32 + 16], lhsT=tri[0][:], rhs=la[:, ti, 0:16], start=True, stop=True),
                           lambda e, ti=ti, j=j: e.matmul(pa[:, j * 32 + 16:j * 32 + 32], lhsT=tri[1][:], rhs=la[:, ti, 16:32], start=True, stop=True),
                           lambda e, ti=ti, j=j: e.matmul(pb[:, j * 32:j * 32 + 32], lhsT=ones_f[:], rhs=la[:, ti, :], start=True, stop=True)],
                    reads=[tri[0], tri[1], la, ones_f], writes=[pa, pb])
        S.op("dve", lambda e, half=half, pa=pa: e.tensor_copy(out=gg[:, 9 * half:9 * half + 9, :], in_=pa[:, 0:288].rearrange("p (a b) -> p a b", b=32)), reads=[pa], writes=[gg])
        S.op("dve", lambda e, half=half, pb=pb: e.tensor_copy(out=glast[:, 9 * half:9 * half + 9, :], in_=pb[:, 0:288].rearrange("p (a b) -> p a b", b=32)), reads=[pb], writes=[glast])
    S.op("dve", lambda e: e.tensor_tensor(out=aa[:], in0=gg[:], in1=lb[:], op=ALU.add), reads=[gg, lb], writes=[aa])
    S.op("act", lambda e: e.activation(out=eg[:], in_=gg[:], func=AF.Exp), reads=[gg], writes=[eg])
    S.op("dve", lambda e: e.scalar_tensor_tensor(out=c1[:], in0=beta[:], scalar=-1.0, in1=eg[:], op0=ALU.mult, op1=ALU.mult), reads=[beta, eg], writes=[c1])
    S.op("dve", lambda e: e.tensor_tensor(out=kts[:], in0=glast[:], in1=gg[:], op=ALU.subtract), reads=[glast, gg], writes=[kts])
    S.op("act", lambda e: e.activation(out=kts[:], in_=kts[:], func=AF.Exp), reads=[kts], writes=[kts])
    S.op("act", lambda e: e.activation(out=egl[:], in_=glast[:], func=AF.Exp), reads=[glast], writes=[egl])

    import os
    lvl4 = int(os.environ.get("K4", "9"))
    if lvl4 < 1:
        return
    xin = S.sb("xin", [128, NT], BF16)
    kTf = S.sb("kTf", [128, NT]); vTf = S.sb("vTf", [128, NT]); qTf = S.sb("qTf", [128, T])
    kTb = S.sb("kTb", [128, NT], BF16); qTb = S.sb("qTb", [128, T], BF16)
    ktok = S.sb("ktok", [128, 18, 128]); vtok = S.sb("vtok", [128, 18, 128])
    Qall = S.sb("Qall", [128, 36, 128]); QKall = S.sb("QKall", [128, 32, 128], BF16)
    oall = S.sb("oall", [128, 16, 128])
    sqb = S.sb("sqb", [128, 512], BF16); rsb = S.sb("rsb", [128, 512])
    zs = S.sb("zs", [128, T]); odo = S.sb("odo", [128, T], BF16)
    tmp = {}
    for c4 in range(4):
        for nm in ("LT", "u", "F", "A", "B", "A2", "B2", "Q", "v2"):
            tmp[(nm, c4)] = S.sb(f"t_{nm}{c4}", [128, 128])
    Gs2 = [S.sb(f"Gs2_{i}", [128, 128]) for i in range(2)]; GqT2 = [S.sb(f"GqT2_{i}", [128, 128]) for i in range(2)]
    for d in range(2):
        for nm in ("vb", "X"):
            tmp[(nm, d)] = S.sb(f"t_{nm}{d}", [128, 128])
        for nm in ("ktl", "vn", "Sb"):
            tmp[(nm, d)] = S.sb(f"t_{nm}{d}", [128, 128], BF16)
        tmp[("Sf", d)] = S.sb(f"t_Sf{d}", [128, 128])
    ssn = S.sb("ssn", [128, 16]); jk = S.sb("jk", [128, 128], BF16)

    def conv_silu(src_ch, conv_ch, dst, W):
        S.dma("sp", xin[:, 0:W], k.plT[src_ch][:, 0:W], reads=[k.plT[src_ch]], writes=[xin])
        segs = [(0, T)] + ([(T, TC)] if W > T else [])
        wcol = lambda j: colT[:, C_CW + j * 48 + conv_ch:C_CW + j * 48 + conv_ch + 1]
        for si, (s0, L) in enumerate(segs):
            eng = "dve"
            S.op(eng, lambda e, s0=s0, L=L: e.tensor_scalar(out=dst[:, s0:s0 + L], in0=xin[:, s0:s0 + L], scalar1=wcol(2), scalar2=None, op0=ALU.mult), reads=[xin, colT], writes=[dst])
            for j in (0, 1, 3, 4):
                sft = j - 2
                a0 = max(0, -sft); a1 = L - max(0, sft)
                S.op(eng, lambda e, s0=s0, a0=a0, a1=a1, sft=sft, j=j: e.scalar_tensor_tensor(out=dst[:, s0 + a0:s0 + a1], in0=xin[:, s0 + a0 + sft:s0 + a1 + sft], scalar=wcol(j), in1=dst[:, s0 + a0:s0 + a1], op0=ALU.mult, op1=ALU.add),
                     reads=[xin, colT, dst], writes=[dst])
        S.op("act", lambda e: e.activation(out=dst[:, 0:W], in_=dst[:, 0:W], func=AF.Silu), reads=[dst], writes=[dst])

    def l2n(src, W, dstb, dstf, scale):
        for (t0, tw) in blocks(W):
            pa = P[7]
            S.op("act", lambda e, t0=t0, tw=tw: e.activation(out=sqb[:, 0:tw], in_=src[:, t0:t0 + tw], func=AF.Square), reads=[src], writes=[sqb])
            S.group("pe", [lambda e, tw=tw: e.matmul(pa[:, 0:tw], lhsT=ones_b[:], rhs=sqb[:, 0:tw], start=True, stop=True)], reads=[sqb, ones_b], writes=[pa])
            S.op("act", lambda e, tw=tw: e.activation(out=rsb[:, 0:tw], in_=pa[:, 0:tw], func=AF.Sqrt, bias=epsc[:], scale=1.0), reads=[pa, epsc], writes=[rsb])
            S.op("dve", lambda e, tw=tw: e.reciprocal(out=rsb[:, 0:tw], in_=rsb[:, 0:tw]), reads=[rsb], writes=[rsb])
            S.op("dve", lambda e, t0=t0, tw=tw: e.scalar_tensor_tensor(out=dstb[:, t0:t0 + tw], in0=src[:, t0:t0 + tw], scalar=scale, in1=rsb[:, 0:tw], op0=ALU.mult, op1=ALU.mult), reads=[src, rsb], writes=[dstb])
            if dstf is not None:
                S.op("pool", lambda e, t0=t0, tw=tw: e.tensor_tensor(out=dstf[:, t0:t0 + tw], in0=src[:, t0:t0 + tw], in1=rsb[:, 0:tw], op=ALU.mult), reads=[src, rsb], writes=[dstf])

    import os
    k.d4 = dict(oall=oall, Qall=Qall, QKall=QKall, kTb=kTb, qTb=qTb, vtok=vtok, ktok=ktok, gg=gg, la=la, beta=beta, lb=lb, glast=glast)
    for h in range(int(os.environ.get("K4H", "16"))):
        conv_silu(5 + h, h, kTf, NT)
        conv_silu(21 + h, 16 + h, vTf, NT)
        conv_silu(37 + h, 32 + h, qTf, T)
        l2n(kTf, NT, kTb, kTf, 1.0)
        l2n(qTf, T, qTb, None, 128 ** -0.5)
        for src, dst in ((kTf, ktok), (vTf, vtok)):
            for g in range(5):
                tiles = list(range(4 * g, min(4 * g + 4, 18)))
                pa = P[g % 2]
                S.group("pe", [lambda e, ti=ti, g=g, src=src, pa=pa: e.transpose(pa[:, sl(ti - 4 * g)], src[:, sl(ti)], ident[:]) for ti in tiles], reads=[src, ident], writes=[pa])
                S.op("act", lambda e, g=g, n=len(tiles), dst=dst, pa=pa: e.copy(out=dst[:, 4 * g:4 * g + n, :], in_=pa[:, 0:n * 128].rearrange("p (a b) -> p a b", b=128)), reads=[pa], writes=[dst])
        if lvl4 < 2:
            continue
        X = lambda nm, c: tmp[(nm, c)]
        PO = [(16, 17), (0, 1), (14, 15), (2, 3), (12, 13), (4, 5), (10, 11), (6, 7), (8, 9)]
        order = ([16, 17] + list(range(16)), [17, 16] + list(range(15, -1, -1)))
        ptr = [0, 0]; done = set()
        S.op("pool", lambda e: e.memset(oall[:], 0.0), writes=[oall])
        for d in range(2):
            S.op("pool", lambda e, d=d: e.memset(tmp[("Sf", d)][:], 0.0), writes=[tmp[("Sf", d)]])
            S.op("pool", lambda e, d=d: e.memset(tmp[("Sb", d)][:], 0.0), writes=[tmp[("Sb", d)]])
        for tp in range(9):
            pair = PO[tp]
            chains = [(sub * 2 + d, pair[sub], d) for sub in range(2) for d in range(2)]
            for sub in range(2):
                ti = pair[sub]
                lat = ti < 16
                pg = P[4 * sub]
                S.group("pe", [lambda e, ti=ti, pg=pg: e.matmul(pg[:, 256:384], lhsT=kTb[:, sl(ti)], rhs=kTb[:, sl(ti)], start=True, stop=True)], reads=[kTb], writes=[pg])
                S.op("act", lambda e, sub=sub, pg=pg: e.copy(out=Gs2[sub][:], in_=pg[:, 256:384]), reads=[pg], writes=[Gs2[sub]])
                if lat:
                    S.group("pe", [lambda e, ti=ti, pg=pg: e.matmul(pg[:, 384:512], lhsT=kTb[:, sl(ti)], rhs=qTb[:, sl(ti)], start=True, stop=True)], reads=[kTb, qTb], writes=[pg])
                    S.op("act", lambda e, sub=sub, pg=pg: e.copy(out=GqT2[sub][:], in_=pg[:, 384:512]), reads=[pg], writes=[GqT2[sub]])
            for (c, ti, d) in chains:
                lat = ti < 16; sub = c // 2
                cd = d * 16 + h
                px = P[2 * c]
                S.op("pool", lambda e, c=c, d=d, cd=cd, ti=ti: e.tensor_scalar(out=X("LT", c)[:], in0=tri[d][:], scalar1=la[:, ti, cd:cd + 1], scalar2=None, op0=ALU.mult), reads=[tri[d], la], writes=[X("LT", c)])
                S.group("pe", [lambda e, c=c, px=px: e.matmul(px[:, 0:128], lhsT=ones_f[:], rhs=X("LT", c)[:], start=True, stop=True)], reads=[ones_f, X("LT", c)], writes=[px])
                S.op("dve", lambda e, c=c, d=d, cd=cd, ti=ti, px=px: e.scalar_tensor_tensor(out=X("u", c)[:], in0=px[:, 0:128], scalar=aa[:, ti, cd:cd + 1], in1=pm[d][:], op0=ALU.subtract, op1=ALU.max), reads=[px, aa, pm[d]], writes=[X("u", c)])
                if lat:
                    S.op("dve", lambda e, c=c, d=d, cd=cd, ti=ti, px=px: e.scalar_tensor_tensor(out=X("v2", c)[:], in0=px[:, 0:128], scalar=gg[:, ti, cd:cd + 1], in1=nm_[d][:], op0=ALU.subtract, op1=ALU.min), reads=[px, gg, nm_[d]], writes=[X("v2", c)])
                S.op("act", lambda e, c=c: e.activation(out=X("F", c)[:], in_=X("u", c)[:], func=AF.Exp, scale=-1.0), reads=[X("u", c)], writes=[X("F", c)])
                S.op("dve", lambda e, c=c, sub=sub: e.scalar_tensor_tensor(out=X("A", c)[:], in0=Gs2[sub][:], scalar=-1.0, in1=X("F", c)[:], op0=ALU.mult, op1=ALU.mult), reads=[Gs2[sub], X("F", c)], writes=[X("A", c)])
                if lat:
                    S.op("act", lambda e, c=c: e.activation(out=X("v2", c)[:], in_=X("v2", c)[:], func=AF.Exp), reads=[X("v2", c)], writes=[X("v2", c)])
                    S.op("pool", lambda e, c=c, d=d, ti=ti, sub=sub: e.tensor_tensor(out=QKall[:, d * 16 + ti, :], in0=GqT2[sub][:], in1=X("v2", c)[:], op=ALU.mult), reads=[GqT2[sub], X("v2", c)], writes=[QKall])
            for (c, ti, d) in chains:
                px = P[2 * c]
                S.group("pe", [lambda e, c=c, px=px: e.transpose(px[:, 128:256], X("A", c)[:], ident[:])], reads=[X("A", c), ident], writes=[px])
                S.op("act", lambda e, c=c, px=px: e.copy(out=X("B", c)[:], in_=px[:, 128:256]), reads=[px], writes=[X("B", c)])
                S.op("dve", lambda e, c=c, px=px: e.tensor_tensor(out=X("Q", c)[:], in0=px[:, 128:256], in1=ident[:], op=ALU.add), reads=[px, ident], writes=[X("Q", c)])
            cur = {c: ("A", "B") for c in range(4)}
            for lvl in range(6):
                last = lvl == 5
                for (c, ti, d) in chains:
                    An, Bn = cur[c]
                    A2n, B2n = ("A2", "B2") if An == "A" else ("A", "B")
                    px, py = P[2 * c], P[2 * c + 1]
                    S.group("pe", [lambda e, c=c, An=An, Bn=Bn, px=px: e.matmul(px[:, 0:128], lhsT=X(Bn, c)[:], rhs=X(An, c)[:], start=True, stop=True)], reads=[X(An, c), X(Bn, c)], writes=[px])
                    S.op("act", lambda e, c=c, A2n=A2n, px=px: e.copy(out=X(A2n, c)[:], in_=px[:, 0:128]), reads=[px], writes=[X(A2n, c)])
                    if not last:
                        S.group("pe", [lambda e, c=c, An=An, Bn=Bn, py=py: e.matmul(py[:, 0:128], lhsT=X(An, c)[:], rhs=X(Bn, c)[:], start=True, stop=True)], reads=[X(An, c), X(Bn, c)], writes=[py])
                        S.op("act", lambda e, c=c, B2n=B2n, py=py: e.copy(out=X(B2n, c)[:], in_=py[:, 0:128]), reads=[py], writes=[X(B2n, c)])
                    cur[c] = (A2n, B2n)
                for (c, ti, d) in chains:
                    A2n = cur[c][0]
                    py = P[2 * c + 1]
                    S.group("pe", [lambda e, c=c, A2n=A2n, py=py: e.matmul(py[:, 256:384], lhsT=X(A2n, c)[:], rhs=X("Q", c)[:], start=True, stop=True)], reads=[X(A2n, c), X("Q", c)], writes=[py])
                    dstQ = Qall[:, d * 18 + ti, :] if last else X("Q", c)[:]
                    dres = Qall if last else X("Q", c)
                    S.op("dve", lambda e, c=c, py=py, dstQ=dstQ: e.tensor_tensor(out=dstQ, in0=py[:, 256:384], in1=X("Q", c)[:], op=ALU.add), reads=[py, X("Q", c)], writes=[dres])
            done.update(pair)
            progressed = True
            while progressed:
                progressed = False
                for d in range(2):
                    if ptr[d] < 18 and order[d][ptr[d]] in done:
                        ti = order[d][ptr[d]]; ptr[d] += 1; progressed = True
                        lat = ti < 16; cd = d * 16 + h
                        Sf, Sb, vb, Xx, vn, ktl = (tmp[(n, d)] for n in ("Sf", "Sb", "vb", "X", "vn", "ktl"))
                        pks, pvn, pqs, po2, pds = P[0 + d], P[2 + d], P[4 + d], P[6 + d], P[6 + d]
                        S.op("pool", lambda e, ti=ti, cd=cd, vb=vb: e.tensor_scalar(out=vb[:], in0=vtok[:, ti, :], scalar1=beta[:, ti, cd:cd + 1], scalar2=None, op0=ALU.mult), reads=[vtok, beta], writes=[vb])
                        S.op("pool", lambda e, ti=ti, cd=cd, ktl=ktl: e.tensor_scalar(out=ktl[:], in0=ktok[:, ti, :], scalar1=kts[:, ti, cd:cd + 1], scalar2=None, op0=ALU.mult), reads=[ktok, kts], writes=[ktl])
                        S.group("pe", [lambda e, ti=ti, Sb=Sb, pks=pks: e.matmul(pks[:, 0:128], lhsT=kTb[:, sl(ti)], rhs=Sb[:], start=True, stop=True)], reads=[kTb, Sb], writes=[pks])
                        if lat:
                            S.group("pe", [lambda e, ti=ti, Sb=Sb, pqs=pqs: e.matmul(pqs[:, 0:128], lhsT=qTb[:, sl(ti)], rhs=Sb[:], start=True, stop=True)], reads=[qTb, Sb], writes=[pqs])
                        S.op("dve", lambda e, ti=ti, cd=cd, pks=pks, vb=vb, Xx=Xx: e.scalar_tensor_tensor(out=Xx[:], in0=pks[:, 0:128], scalar=c1[:, ti, cd:cd + 1], in1=vb[:], op0=ALU.mult, op1=ALU.add), reads=[pks, c1, vb], writes=[Xx])
                        S.group("pe", [lambda e, ti=ti, d=d, Xx=Xx, pvn=pvn: e.matmul(pvn[:, 0:128], lhsT=Qall[:, d * 18 + ti, :], rhs=Xx[:], start=True, stop=True)], reads=[Qall, Xx], writes=[pvn])
                        S.op("act", lambda e, vn=vn, pvn=pvn: e.copy(out=vn[:], in_=pvn[:, 0:128]), reads=[pvn], writes=[vn])
                        if lat:
                            S.group("pe", [lambda e, ti=ti, d=d, vn=vn, po2=po2: e.matmul(po2[:, 128:256], lhsT=QKall[:, d * 16 + ti, :], rhs=vn[:], start=True, stop=True)], reads=[QKall, vn], writes=[po2])
                            S.op("dve", lambda e, ti=ti, cd=cd, pqs=pqs: e.scalar_tensor_tensor(out=oall[:, ti, :], in0=pqs[:, 0:128], scalar=eg[:, ti, cd:cd + 1], in1=oall[:, ti, :], op0=ALU.mult, op1=ALU.add), reads=[pqs, eg, oall], writes=[oall])
                            S.op("dve", lambda e, ti=ti, po2=po2: e.tensor_tensor(out=oall[:, ti, :], in0=po2[:, 128:256], in1=oall[:, ti, :], op=ALU.add), reads=[po2, oall], writes=[oall])
                        S.group("pe", [lambda e, ktl=ktl, vn=vn, pds=pds: e.matmul(pds[:, 0:128], lhsT=ktl[:], rhs=vn[:], start=True, stop=True)], reads=[ktl, vn], writes=[pds])
                        S.op("dve", lambda e, ti=ti, cd=cd, Sf=Sf, pds=pds: e.scalar_tensor_tensor(out=Sf[:], in0=Sf[:], scalar=egl[:, ti, cd:cd + 1], in1=pds[:, 0:128], op0=ALU.mult, op1=ALU.add), reads=[Sf, egl, pds], writes=[Sf])
                        S.op("act", lambda e, Sf=Sf, Sb=Sb: e.copy(out=Sb[:], in_=Sf[:]), reads=[Sf], writes=[Sb])
        if lvl4 < 4:
            continue
        for ti in range(16):
            S.op("act", lambda e, ti=ti: e.activation(out=jk[:], in_=oall[:, ti, :], func=AF.Square, accum_out=ssn[:, ti:ti + 1]), reads=[oall], writes=[jk, ssn])
        S.op("act", lambda e: e.activation(out=ssn[:], in_=ssn[:], func=AF.Sqrt, bias=epsc[:], scale=1.0 / 128), reads=[ssn, epsc], writes=[ssn])
        S.op("dve", lambda e: e.reciprocal(out=ssn[:], in_=ssn[:]), reads=[ssn], writes=[ssn])
        S.op("dve", lambda e: e.tensor_tensor(out=oall[:], in0=oall[:], in1=ssn[:].unsqueeze(2).to_broadcast([128, 16, 128]), op=ALU.mult), reads=[oall, ssn], writes=[oall])
        S.dma("sp", xin[:, 0:T], k.plT[61 + h][:, 0:T], reads=[k.plT[61 + h]], writes=[xin])
        S.op("act", lambda e: e.activation(out=zs[:], in_=xin[:, 0:T], func=AF.Silu), reads=[xin], writes=[zs])
        for g in range(4):
            pa = P[g % 2]
            S.group("pe", [lambda e, ti=ti, g=g, pa=pa: e.transpose(pa[:, sl(ti - 4 * g)], oall[:, ti, :], ident[:]) for ti in range(4 * g, 4 * g + 4)], reads=[oall, ident], writes=[pa])
            S.op("dve", lambda e, g=g, pa=pa: e.scalar_tensor_tensor(out=odo[:, sl(g, 512)], in0=pa[:], scalar=colT[:, C_G + 14:C_G + 15], in1=zs[:, sl(g, 512)], op0=ALU.mult, op1=ALU.mult), reads=[pa, colT, zs], writes=[odo])
        S.dma("sp", k.odD[h][:], odo[:], reads=[odo], writes=[k.odD[h]])


def phase5(k):
    S, I, P, colT = k.S, k.I, k.P, k.colT
    g1bc = S.sb("g1bc", [128, D])
    dgs5 = [S.sb(f"dg5_{i}", [128, 128]) for i in range(2)]
    bcast_row(k, C_ML + 64, g1bc, dgs5)
    wr = S.sb("wr", [128, 32, 36])
    S.dma("sp", wr[:, :, 0:4], I["w_rg"].rearrange("(c p) n -> p c n", p=128), writes=[wr])
    S.dma("sp", wr[:, :, 4:36], I["w_re"].rearrange("(c p) n -> p c n", p=128), reads=[wr], writes=[wr])
    brb = S.sb("brb", [128, 36])
    S.dma("sp", brb[:, 0:4], I["b_rg"].partition_broadcast(128), writes=[brb])
    S.dma("sp", brb[:, 4:36], I["b_re"].partition_broadcast(128), reads=[brb], writes=[brb])
    minT = S.sb("minT", [128, 32, 512], BF16)
    big = S.sb("big5", [128, 16384])
    bv = big.t
    attb = Res(bv[:, 0:4096].bitcast(BF16).rearrange("p (h t) -> p h t", t=512), "attb")
    odb = Res(bv[:, 4096:8192].bitcast(BF16).rearrange("p (h t) -> p h t", t=512), "odb")
    wab = [Res(bv[:, 8192 + 2048 * i:10240 + 2048 * i].bitcast(BF16).rearrange("p (h t) -> p h t", t=256), f"wab{i}") for i in range(2)]
    rest5 = Res(bv[:, 12288:16384], "rest5")
    xs4 = bv[:].rearrange("p (a b) -> p a b", b=D)
    allbig = [attb, odb, wab[0], wab[1], rest5]
    gsb = [S.sb(f"gsb{i}", [128, 512], BF16) for i in range(2)]
    gsg = [S.sb(f"gsg{i}", [128, 512]) for i in range(2)]
    tmpa = S.sb("tmpa", [128, 512])
    wo = [S.sb(f"wo{i}", [128, 32, 128], BF16) for i in range(2)]
    xt = S.sb("xt5", [128, D]); xs = S.sb("xs5", [128, D]); junk = xs
    ss = S.sb("ss5", [128, 1]); rs = S.sb("rs5", [128, 1])
    ht = S.sb("ht5", [128, 32, 128], BF16); hf = S.sb("hf5", [128, 32, 128])
    lg = S.sb("lg", [128, 36]); sm = S.sb("sm", [128, 64])
    woav = I["w_oa"].rearrange("(h p) n -> p h n", p=128); wobv = I["w_ob"].rearrange("(h p) n -> p h n", p=128)
    woutv = I["w_out"].rearrange("(c p) n -> p c n", p=128)
    wi = 0
    for tb in range(4):
        t0 = tb * 512
        for h in range(16):
            S.dma("sp", attb[:, h, :], k.attD[h][:, t0:t0 + 512], reads=[k.attD[h], attb], writes=[attb])
            S.dma("sp", odb[:, h, :], k.odD[h][:, t0:t0 + 512], reads=[k.odD[h], odb], writes=[odb])
        for nb in range(16):
            wa = wab[0]; wbb = wab[1]
            S.dma("pool", wa[:], woav[:, :, sl(nb, 256)], writes=[wa])
            S.dma("pool", wbb[:], wobv[:, :, sl(nb, 256)], writes=[wbb])
            for ci in range(2):
                ch = nb * 2 + ci
                pa, pb = P[0], P[1]
                S.group("pe", [lambda e, h=h, ci=ci: e.matmul(pa[:, :], lhsT=wa[:, h, sl(ci)], rhs=attb[:, h, :], start=(h == 0), stop=(h == 15)) for h in range(16)], reads=[wa, attb], writes=[pa])
                S.group("pe", [lambda e, h=h, ci=ci: e.matmul(pb[:, :], lhsT=wbb[:, h, sl(ci)], rhs=odb[:, h, :], start=(h == 0), stop=(h == 15)) for h in range(16)], reads=[wbb, odb], writes=[pb])
                for j, pp in ((0, pa), (1, pb)):
                    gch = 77 + j * 32 + ch
                    S.dma("sp", gsb[j][:], k.plT[gch][:, t0:t0 + 512], reads=[k.plT[gch]], writes=[gsb[j]])
                    S.op("act", lambda e, j=j: e.activation(out=gsg[j][:], in_=gsb[j][:], func=AF.Sigmoid), reads=[gsb[j]], writes=[gsg[j]])
                S.op("dve", lambda e: e.tensor_tensor(out=tmpa[:], in0=pa[:], in1=gsg[0][:], op=ALU.mult), reads=[pa, gsg[0]], writes=[tmpa])
                S.op("dve", lambda e: e.tensor_tensor(out=gsg[1][:], in0=pb[:], in1=gsg[1][:], op=ALU.mult), reads=[pb, gsg[1]], writes=[gsg[1]])
                S.op("dve", lambda e, ch=ch: e.tensor_tensor(out=minT[:, ch, :], in0=tmpa[:], in1=gsg[1][:], op=ALU.add), reads=[tmpa, gsg[1]], writes=[minT])
        for nb in range(32):
            w = wo[wi % 2]; wi += 1
            S.dma("pool", w[:], woutv[:, :, sl(nb, 128)], writes=[w])
            pp = P[2 + nb % 2]
            for tt in range(4):
                S.group("pe", [lambda e, c=c, w=w, pp=pp, tt=tt: e.matmul(pp[:, sl(tt)], lhsT=minT[:, c, sl(tt)], rhs=w[:, c, :], start=(c == 0), stop=(c == 31)) for c in range(32)], reads=[minT, w], writes=[pp])
            S.op("dve", lambda e, pp=pp, nb=nb: e.tensor_tensor(out=xs4[:, :, sl(nb, 128)], in0=pp[:, :].rearrange("p (a b) -> p a b", b=128), in1=g1bc[:, sl(nb, 128)].unsqueeze(1).to_broadcast([128, 4, 128]), op=ALU.mult), reads=[pp, g1bc] + allbig, writes=allbig)
        for tt in range(4):
            ti = tb * 4 + tt
            S.dma("sp", xt[:], I["x"][sl(ti), :], writes=[xt])
            S.op("pool", lambda e, tt=tt: e.tensor_tensor(out=xt[:], in0=xt[:], in1=xs4[:, tt, :], op=ALU.add), reads=[xt] + allbig, writes=[xt])
            S.dma("sp", k.xl1D[sl(ti), :], xt[:], reads=[xt], writes=[k.xl1D])
            norm_transpose(k, xt, xs, junk, ss, rs, k.A2l, C_ML + 96, ht, f32copy=hf)
            S.dma("sp", k.hl2D.t.ap()[:, :, sl(ti)].rearrange("c p t -> p c t"), ht[:], reads=[ht], writes=[k.hl2D])
            pr = P[4]
            S.group("pe", [lambda e, c=c: e.matmul(pr[:, 0:36], lhsT=hf[:, c, :], rhs=wr[:, c, :], start=(c == 0), stop=(c == 31)) for c in range(32)], reads=[hf, wr], writes=[pr])
            S.op("dve", lambda e: e.tensor_tensor(out=lg[:], in0=pr[:, 0:36], in1=brb[:], op=ALU.add), reads=[pr, brb], writes=[lg])
            router(k, lg, sm, ti)


def router(k, lg, sm, ti):
    S = k.S
    g = k.gates

    def dv(fn):
        S.op("dve", fn, reads=[lg, sm, g], writes=[sm, g])
    dv(lambda e: e.tensor_reduce(out=sm[:, 0:1], in_=lg[:, 0:4], axis=AX.X, op=ALU.max))
    dv(lambda e: e.tensor_scalar(out=sm[:, 4:8], in0=lg[:, 0:4], scalar1=sm[:, 0:1], scalar2=None, op0=ALU.subtract))
    S.op("act", lambda e: e.activation(out=sm[:, 8:12], in_=sm[:, 4:8], func=AF.Exp, accum_out=sm[:, 1:2]), reads=[sm], writes=[sm])
    dv(lambda e: e.reciprocal(out=sm[:, 2:3], in_=sm[:, 1:2]))
    dv(lambda e: e.tensor_scalar(out=sm[:, 12:16], in0=lg[:, 0:4], scalar1=sm[:, 0:1], scalar2=None, op0=ALU.is_equal))
    dv(lambda e: e.tensor_scalar(out=sm[:, 12:16], in0=sm[:, 12:16], scalar1=1e4, scalar2=-1e4, op0=ALU.mult, op1=ALU.add))
    dv(lambda e: e.tensor_tensor(out=g[:, ti, :].rearrange("p (a b) -> p a b", b=8), in0=lg[:, 4:36].rearrange("p (a b) -> p a b", b=8),
                                  in1=sm[:, 12:16].unsqueeze(2).to_broadcast([128, 4, 8]), op=ALU.add))
    dv(lambda e: e.tensor_reduce(out=sm[:, 16:17], in_=g[:, ti, :], axis=AX.X, op=ALU.max))
    dv(lambda e: e.tensor_scalar(out=sm[:, 32:64], in0=g[:, ti, :], scalar1=sm[:, 16:17], scalar2=None, op0=ALU.is_equal))
    dv(lambda e: e.scalar_tensor_tensor(out=sm[:, 20:21].to_broadcast([128, 32]) if False else g[:, ti, :], in0=sm[:, 32:64], scalar=-1e4, in1=g[:, ti, :], op0=ALU.mult, op1=ALU.add))
    dv(lambda e: e.tensor_reduce(out=sm[:, 17:18], in_=g[:, ti, :], axis=AX.X, op=ALU.max))
    dv(lambda e: e.tensor_tensor(out=sm[:, 18:19], in0=sm[:, 17:18], in1=sm[:, 16:17], op=ALU.subtract))
    S.op("act", lambda e: e.activation(out=sm[:, 19:20], in_=sm[:, 18:19], func=AF.Exp), reads=[sm], writes=[sm])
    dv(lambda e: e.tensor_scalar(out=sm[:, 20:21], in0=sm[:, 19:20], scalar1=1.0, scalar2=None, op0=ALU.add))
    dv(lambda e: e.reciprocal(out=sm[:, 20:21], in_=sm[:, 20:21]))
    dv(lambda e: e.tensor_tensor(out=sm[:, 20:21], in0=sm[:, 20:21], in1=sm[:, 2:3], op=ALU.mult))
    dv(lambda e: e.tensor_tensor(out=sm[:, 21:22], in0=sm[:, 20:21], in1=sm[:, 19:20], op=ALU.mult))
    dv(lambda e: e.tensor_scalar(out=g[:, ti, :], in0=g[:, ti, :], scalar1=sm[:, 17:18], scalar2=sm[:, 21:22], op0=ALU.is_equal, op1=ALU.mult))
    dv(lambda e: e.scalar_tensor_tensor(out=g[:, ti, :], in0=sm[:, 32:64], scalar=sm[:, 20:21], in1=g[:, ti, :], op0=ALU.mult, op1=ALU.add))


def phase6(k):
    S, I, P = k.S, k.I, k.P
    g2bc = S.sb("g2bc", [128, D])
    dgs6 = [S.sb(f"dg6_{i}", [128, 128]) for i in range(2)]
    bcast_row(k, C_ML + 160, g2bc, dgs6)
    hb = S.sb("hb6", [128, 32, 512], BF16)
    yacc = S.sb("yacc", [128, 4, D])
    hid = S.sb("hid", [128, 8, 512], BF16)
    wq = [S.sb(f"wq{i}", [128, 8192], BF16) for i in range(4)]
    h1s = S.sb("h1s", [128, 512])
    xo = S.sb("xo", [128, 2048])
    hv = k.hl2D.t.ap().rearrange("c p t -> p c t")
    wc = 0
    for tb in range(4):
        t0 = tb * 512
        S.dma("sp", hb[:], hv[:, :, t0:t0 + 512], reads=[k.hl2D], writes=[hb])
        S.op("pool", lambda e: e.memset(yacc[:], 0.0), writes=[yacc])
        for ex in range(32):
            w1v = I["w1"][ex].rearrange("(c p) n -> p c n", p=128); w3v = I["w3"][ex].rearrange("(c p) n -> p c n", p=128)
            w2v = I["w2"][ex].rearrange("(c p) n -> p c n", p=128)
            for hq in range(4):
                wa = wq[wc % 4]; wc += 1
                wbq = wq[wc % 4]; wc += 1
                wa3 = wa[:].rearrange("p (c n) -> p c n", n=256); wb3 = wbq[:].rearrange("p (c n) -> p c n", n=256)
                S.dma("pool", wa3, w1v[:, :, sl(hq, 256)], writes=[wa])
                S.dma("pool", wb3, w3v[:, :, sl(hq, 256)], writes=[wbq])
                for hc2 in range(2):
                    hc = hq * 2 + hc2
                    p1, p3 = P[0 + hc % 2], P[2 + hc % 2]
                    S.group("pe", [lambda e, c=c, wa3=wa3, p1=p1, hc2=hc2: e.matmul(p1[:, :], lhsT=wa3[:, c, sl(hc2)], rhs=hb[:, c, :], start=(c == 0), stop=(c == 31)) for c in range(32)], reads=[wa, hb], writes=[p1])
                    S.group("pe", [lambda e, c=c, wb3=wb3, p3=p3, hc2=hc2: e.matmul(p3[:, :], lhsT=wb3[:, c, sl(hc2)], rhs=hb[:, c, :], start=(c == 0), stop=(c == 31)) for c in range(32)], reads=[wbq, hb], writes=[p3])
                    S.op("act", lambda e, p1=p1: e.activation(out=h1s[:], in_=p1[:], func=AF.Silu), reads=[p1], writes=[h1s])
                    S.op("dve", lambda e, p3=p3, hc=hc: e.tensor_tensor(out=hid[:, hc, :], in0=p3[:], in1=h1s[:], op=ALU.mult), reads=[p3, h1s], writes=[hid])
            for nq in range(4):
                w2 = wq[wc % 4]; wc += 1
                w23 = w2[:].rearrange("p (c n) -> p c n", n=1024)
                S.dma("pool", w23, w2v[:, :, sl(nq, 1024)], writes=[w2])
                for tt in range(4):
                    for nh in range(2):
                        nb = nq * 2 + nh
                        py = P[4 + (tt * 2 + nh) % 4]
                        S.group("pe", [lambda e, c=c, w23=w23, py=py, tt=tt, nh=nh: e.matmul(py[:, :], lhsT=hid[:, c, sl(tt)], rhs=w23[:, c, sl(nh, 512)], start=(c == 0), stop=(c == 7)) for c in range(8)], reads=[hid, w2], writes=[py])
                        S.op("dve", lambda e, py=py, tt=tt, nb=nb, ex=ex, tb=tb: e.scalar_tensor_tensor(out=yacc[:, tt, sl(nb, 512)], in0=py[:], scalar=k.gates[:, tb * 4 + tt, ex:ex + 1], in1=yacc[:, tt, sl(nb, 512)], op0=ALU.mult, op1=ALU.add),
                             reads=[py, k.gates, yacc], writes=[yacc])
        for tt in range(4):
            ti = tb * 4 + tt
            S.op("dve", lambda e, tt=tt: e.tensor_tensor(out=yacc[:, tt, :], in0=yacc[:, tt, :], in1=g2bc[:], op=ALU.mult), reads=[yacc, g2bc], writes=[yacc])
            for hf_ in range(2):
                S.dma("sp", xo[:], k.xl1D[sl(ti), sl(hf_, 2048)], reads=[k.xl1D], writes=[xo])
                S.op("pool", lambda e, tt=tt, hf_=hf_: e.tensor_tensor(out=xo[:], in0=xo[:], in1=yacc[:, tt, sl(hf_, 2048)], op=ALU.add), reads=[xo, yacc], writes=[xo])
                S.dma("sp", k.out[sl(ti), sl(hf_, 2048)], xo[:], reads=[xo], writes=[])


def build(stop_after=99, dbg=None, only=None, iso=()):
    nc = bass.Bass("TRN2", target_bir_lowering=False)
    I = declare(nc)
    out = nc.dram_tensor("out", [T, D], F32, kind="ExternalOutput").ap()
    dbg_out = None
    if dbg and not isinstance(dbg, list):
        dbg = [("dbg",) + tuple(dbg)]
    dbg_outs = []
    for (nm, fn, shp, dt_) in (dbg or []):
        dbg_outs.append(nc.dram_tensor(nm, list(shp), dt_, kind="ExternalOutput").ap())
    with contextlib.ExitStack() as gst:
        S = Sched(nc, gst)
        k = K(); k.S = S; k.I = I; k.nc = nc; k.out = out

        def scr(name, shape, dt):
            if name in iso:
                return Res(nc.dram_tensor("d_" + name, list(shape), dt, kind="ExternalInput"), name)
            return S.dram(name, shape, dt)
        k.hT = scr("hT", [32, 128, NT], BF16)
        k.plT = [scr(f"plT{c}", [128, NT], BF16) for c in range(141)]
        k.kreD = scr("kreD", [32, NT], F32); k.kroD = scr("kroD", [32, NT], F32); k.abD = scr("abD", [64, NT], F32)
        k.modrow = scr("modrow", [2, 6 * D], F32)
        k.attD = [scr(f"attD{h}", [128, T], BF16) for h in range(16)]
        k.odD = [scr(f"odD{h}", [128, T], BF16) for h in range(16)]
        k.xl1D = scr("xl1D", [T, D], F32)
        k.hl2D = scr("hl2D", [32, 128, T], BF16)
        S.tstack = gst
        k.ident = S.sb("ident", [128, 128]); S.dma("sp", k.ident[:], I["ident"], writes=[k.ident])
        k.ones_f = S.sb("ones_f", [128, 128]); S.op("pool", lambda e: e.memset(k.ones_f[:], 1.0), writes=[k.ones_f])
        k.ones_b = S.sb("ones_b", [128, 128], BF16); S.op("pool", lambda e: e.memset(k.ones_b[:], 1.0), writes=[k.ones_b])
        k.epsc = S.sb("epsc", [128, 1]); S.op("pool", lambda e: e.memset(k.epsc[:], EPS), writes=[k.epsc])
        k.colT = S.sb("colT", [128, 704])
        k.gates = S.sb("gates", [128, 16, 32])
        k.P = [S.ps(f"P{i}", [128, 512]) for i in range(8)]
        if "colT" in iso:
            cin = nc.dram_tensor("d_colT", [128, 704], F32, kind="ExternalInput").ap()
            S.dma("sp", k.colT[:], cin, writes=[k.colT])
            derived(k)
        if "gates" in iso:
            gin = nc.dram_tensor("d_gates", [128, 16, 32], F32, kind="ExternalInput").ap()
            S.dma("sp", k.gates[:], gin, writes=[k.gates])
        phases = [phase0, phase1, phase2, phase3, phase4, phase5, phase6]
        for pi, ph in enumerate(phases):
            if pi > stop_after:
                break
            if only is not None and pi not in only:
                continue
            st = contextlib.ExitStack()
            S.tstack = st
            ph(k)
            S.barrier()
            st.close()
            S.tstack = gst
            if pi == 0:
                derived(k)
        for (nm, fn, shp, dt_), do in zip(dbg or [], dbg_outs):
            S.dma("sp", do, fn(k), reads=[], writes=[])
        S.barrier()
        S.emit()
    LAST["inputs"] = set(I.keys())
    return nc


def make_inputs(inputs):
    sq = {n: np.ascontiguousarray(np.asarray(inputs[n])[0]) for n in
          ("norm1_g", "norm2_g", "w_mod", "b_mod", "w_in", "q_a_norm_g", "w_uq", "kv_norm_g", "w_ukv", "q_norm_g",
           "q_rope_norm_g", "k_norm_g", "k_rope_norm_g", "conv_w", "dn_norm_g", "w_oa", "w_ob", "w_out", "w_rg",
           "b_rg", "w_re", "b_re", "w1", "w3", "w2")}
    sq["a_log"] = np.ascontiguousarray(np.asarray(inputs["a_log"])[0].reshape(32))
    sq["dt_bias"] = np.ascontiguousarray(np.asarray(inputs["dt_bias"])[0].reshape(32))
    sq.update(_consts())
    x = np.asarray(inputs["x"]); ctx = np.asarray(inputs["ctx"]); c = np.asarray(inputs["c"]); cc = np.asarray(inputs["c_ctx"])
    maps = []
    for b in range(8):
        m = dict(sq)
        m["x"] = np.ascontiguousarray(x[b]); m["ctx"] = np.ascontiguousarray(ctx[b])
        m["c2"] = np.ascontiguousarray(np.stack([c[b], cc], axis=0))
        maps.append(m)
    return maps


LAST = {}


def nc_inputs(nc, m):
    return [n for n in m if n in LAST["inputs"]]


def kernel(**inputs):
    from concourse.bass_utils import run_bass_kernel_spmd
    nc = build()
    maps = make_inputs(inputs)
    maps = [{n: m[n] for n in nc_inputs(nc, m)} for m in maps]
    res = run_bass_kernel_spmd(nc, maps, core_ids=list(range(8)))
    return np.stack([np.asarray(r["out"]) for r in res.results], axis=0).astype(np.float32)
```

```python
import contextlib
import numpy as np
import concourse.bass as bass
import concourse.mybir as mybir

F32 = mybir.dt.float32
BF16 = mybir.dt.bfloat16
I32 = mybir.dt.int32
AF = mybir.ActivationFunctionType
ALU = mybir.AluOpType
AX = mybir.AxisListType

EPOCH = 30000
NDMASEM = 48


class Res:
    __slots__ = ("t", "w", "r", "name", "excl")

    def __init__(self, t, name="", excl=False):
        self.excl = excl
        self.t = t
        self.w = None
        self.r = []
        self.name = name

    def __getitem__(self, idx):
        return self.t[idx]


class _Proxy:
    def __getattr__(self, name):
        def f(*a, **kw):
            return (name, a, kw)
        return f


PROXY = _Proxy()


class Sched:
    ENGS = ("pe", "act", "dve", "pool", "sp")

    def __init__(self, nc, stack):
        self.nc = nc
        self.stack = stack
        self.ops = {e: [] for e in self.ENGS}
        self.count = {e: 0 for e in self.ENGS}
        self.sems = {e: [] for e in self.ENGS}
        self.waited = {e: {} for e in self.ENGS}
        self.dsem = [stack.enter_context(nc.semaphore(f"dma{i}")) for i in range(NDMASEM)]
        self.dcount = 0
        self.dlast = [0] * NDMASEM
        self.nsb = 0

    def sb(self, name, shape, dt=F32):
        t = self.tstack.enter_context(self.nc.sbuf_tensor("sb_" + name, list(shape), dt))
        return Res(t, name)

    def ps(self, name, shape, dt=F32):
        t = self.tstack.enter_context(self.nc.psum_tensor("ps_" + name, list(shape), dt))
        return Res(t, name, excl=True)

    def dram(self, name, shape, dt=F32):
        t = self.nc.dram_tensor("d_" + name, list(shape), dt, kind="Internal")
        return Res(t, name)

    def _tok(self, eng):
        self.count[eng] += 1
        k = self.count[eng]
        ep = (k - 1) // EPOCH
        while len(self.sems[eng]) <= ep:
            self.sems[eng].append(self.stack.enter_context(
                self.nc.semaphore(f"s_{eng}_{len(self.sems[eng])}")))
        return (self.sems[eng][ep], (k - 1) % EPOCH + 1, (eng, ep))

    def _need(self, eng, toks):
        waits = []
        best = {}
        for tk in toks:
            if tk is None:
                continue
            sem, val, key = tk
            if eng == "pe" and key[0] == "pe":
                continue
            if best.get(key, (None, 0))[1] < val:
                best[key] = (sem, val)
        for key, (sem, val) in best.items():
            if self.waited[eng].get(key, 0) >= val:
                continue
            self.waited[eng][key] = val
            waits.append((sem, val))
        return waits

    def _deps(self, reads, writes):
        toks = []
        for r in reads:
            toks.append(r.w)
            if r.excl:
                toks.extend(r.r)
        for w in writes:
            toks.append(w.w)
            toks.extend(w.r)
        return toks

    def _commit(self, tok, reads, writes):
        for r in reads:
            r.r.append(tok)
            if len(r.r) > 24:
                best = {}
                for t in r.r:
                    if best.get(t[2], (None, 0, None))[1] < t[1]:
                        best[t[2]] = t
                r.r = list(best.values())
        for w in writes:
            w.w = tok
            w.r = []

    def op(self, eng, fn, reads=(), writes=()):
        waits = self._need(eng, self._deps(reads, writes))
        tok = self._tok(eng)
        self.ops[eng].append((waits, fn(PROXY), (tok[0], 1)))
        self._commit(tok, reads, writes)
        return tok

    def group(self, eng, fns, reads=(), writes=()):
        waits = self._need(eng, self._deps(reads, writes))
        tok = self._tok(eng)
        n = len(fns)
        for i, fn in enumerate(fns):
            self.ops[eng].append((waits if i == 0 else [], fn(PROXY),
                                  (tok[0], 1) if i == n - 1 else None))
        self._commit(tok, reads, writes)
        return tok

    def dma(self, q, out, in_, reads=(), writes=(), **kw):
        i = self.dcount
        self.dcount += 1
        s = i % NDMASEM
        sem = self.dsem[s]
        prev = self.dlast[s]
        tgt = prev + 16
        if tgt > EPOCH:
            raise RuntimeError("dma sem overflow")
        self.dlast[s] = tgt
        toks = self._deps(reads, writes)
        if prev:
            toks.append((sem, prev, ("d", s)))
        waits = self._need(q, toks)
        tok = (sem, tgt, ("d", s))

        self.ops[q].append((waits, ("dma_start", (), dict(out=out, in_=in_, **kw)), (sem, 16)))
        self._commit(tok, reads, writes)
        return tok

    def barrier(self):
        toks = []
        for e in self.ENGS:
            k = self.count[e]
            if k:
                ep = (k - 1) // EPOCH
                toks.append((self.sems[e][ep], (k - 1) % EPOCH + 1, (e, ep)))
        for s in range(NDMASEM):
            if self.dlast[s]:
                toks.append((self.dsem[s], self.dlast[s], ("d", s)))
        for e in self.ENGS:
            waits = self._need(e, toks)
            if waits:
                self.ops[e].append((waits, None, None))

    def wait_all(self, eng, toks):
        waits = self._need(eng, toks)
        if waits:
            self.ops[eng].append((waits, None, None))

    def emit(self):
        nc = self.nc
        ops = self.ops

        def run(e, lst):
            for waits, fn, inc in lst:
                for sem, val in waits:
                    e.wait_ge(sem, val)
                if fn is None:
                    continue
                name, a, kw = fn
                ins = getattr(e, name)(*a, **kw)
                if inc is not None:
                    ins.then_inc(inc[0], inc[1])

        with nc.Block() as block:
            @block.tensor
            def _(e):
                run(e, ops["pe"])

            @block.scalar
            def _(e):
                run(e, ops["act"])

            @block.vector
            def _(e):
                run(e, ops["dve"])

            @block.gpsimd
            def _(e):
                run(e, ops["pool"])

            @block.sync
            def _(e):
                run(e, ops["sp"])

D = 4096; T = 2048; TC = 256; NT = 2304
NTILE = 18
IN_COLS = 18048
EPS = 1e-6
SCALE = 192 ** -0.5
TB = [(0, 512), (512, 512), (1024, 512), (1536, 512), (2048, 256)]
C_ML, C_MC, C_N1, C_N2, C_CW, C_G = 0, 192, 384, 416, 448, 688


def _consts():
    tri = np.triu(np.ones((128, 128), np.float32))
    i = np.arange(128)
    c = {
        "ident": np.eye(128, dtype=np.float32),
        "triF": tri, "triB": tri.T.copy(),
        "pmF": np.where(i[:, None] > i[None, :], 0.0, 1e4).astype(np.float32),
        "pmB": np.where(i[:, None] < i[None, :], 0.0, 1e4).astype(np.float32),
        "nmF": np.where(i[None, :] >= i[:, None], 0.0, -1e4).astype(np.float32),
        "nmB": np.where(i[None, :] <= i[:, None], 0.0, -1e4).astype(np.float32),
    }
    rows = T // 64
    row = np.repeat(np.arange(rows), 64).astype(np.float32)
    col = np.tile(np.arange(64), rows).astype(np.float32)
    inv = (10000.0 ** (-np.arange(0, 32, 2, dtype=np.float32) / 32)).astype(np.float32)
    ang = np.concatenate([row[:, None] * inv, col[:, None] * inv], axis=-1)
    c["cosT"] = np.cos(ang).T.astype(np.float32).copy()
    c["sinT"] = np.sin(ang).T.astype(np.float32).copy()
    return c


class K:
    pass


IN_SHAPES = {
    "x": [T, D], "ctx": [TC, D], "c2": [2, D], "norm1_g": [D], "norm2_g": [D], "w_mod": [D, 6 * D], "b_mod": [6 * D],
    "w_in": [D, IN_COLS], "q_a_norm_g": [1024], "w_uq": [1024, 3072], "kv_norm_g": [512], "w_ukv": [512, 4096],
    "q_norm_g": [128], "q_rope_norm_g": [64], "k_norm_g": [128], "k_rope_norm_g": [64], "conv_w": [5, 6144],
    "a_log": [32], "dt_bias": [32], "dn_norm_g": [128], "w_oa": [2048, D], "w_ob": [2048, D], "w_out": [D, D],
    "w_rg": [D, 4], "b_rg": [4], "w_re": [D, 32], "b_re": [32], "w1": [32, D, 1024], "w3": [32, D, 1024], "w2": [32, 1024, D],
    "ident": [128, 128], "triF": [128, 128], "triB": [128, 128], "pmF": [128, 128], "pmB": [128, 128],
    "nmF": [128, 128], "nmB": [128, 128], "cosT": [32, T], "sinT": [32, T],
}


class LazyIn(dict):
    def __init__(self, nc):
        super().__init__()
        self.nc = nc

    def __missing__(self, name):
        ap = self.nc.dram_tensor(name, list(IN_SHAPES[name]), F32, kind="ExternalInput").ap()
        self[name] = ap
        return ap


def declare(nc):
    return LazyIn(nc)


def sl(c, n=128):
    return slice(c * n, (c + 1) * n)


def phase0(k):
    S, I, P, ident, colT = k.S, k.I, k.P, k.ident, k.colT
    csrc = S.sb("csrc", [64, 128]); S.dma("sp", csrc[:], I["c2"].rearrange("j (k p) -> (j k) p", p=128), writes=[csrc])
    cT = S.sb("cT", [128, 64])
    S.group("pe", [lambda e: e.transpose(P[1][:, 0:64], csrc[:], ident[0:64, 0:64])], reads=[csrc, ident], writes=[P[1]])
    S.op("act", lambda e: e.activation(out=cT[:], in_=P[1][:, 0:64], func=AF.Silu), reads=[P[1]], writes=[cT])
    wm = [S.sb(f"wm{i}", [128, 32, 512]) for i in range(2)]
    wv = I["w_mod"].rearrange("(k p) n -> p k n", p=128)
    pm_ = P[0]
    for nb in range(48):
        w = wm[nb % 2]
        S.dma("sp", w[:], wv[:, :, sl(nb, 512)], writes=[w])
        for ci in range(4):
            ch = nb * 4 + ci
            S.group("pe", [lambda e, kk=kk, w=w, ci=ci, ch=ch: e.matmul(pm_[:, 2 * ch:2 * ch + 2], lhsT=w[:, kk, sl(ci)], rhs=cT[:, kk:64:32], start=(kk == 0), stop=(kk == 31)) for kk in range(32)],
                    reads=[cT, w], writes=[pm_])
    rows = S.sb("rows", [128, 5, 128])
    S.op("pool", lambda e: e.memset(rows[:], 0.0), writes=[rows])
    bvr = I["b_mod"].rearrange("(r p) -> r p", p=128)
    S.dma("sp", rows[:, 0, :], bvr[0:128, :], reads=[rows], writes=[rows])
    S.dma("sp", rows[0:64, 1, :], bvr[128:192, :], reads=[rows], writes=[rows])

    def rowsrc(name):
        return I[name].rearrange("(r p) -> r p", p=128)
    S.dma("sp", rows[0:32, 2, :], rowsrc("norm1_g"), reads=[rows], writes=[rows])
    S.dma("sp", rows[32:64, 2, :], rowsrc("norm2_g"), reads=[rows], writes=[rows])
    cwv = I["conv_w"].rearrange("j (r p) -> (j r) p", p=128)
    S.dma("sp", rows[0:128, 3, :], cwv[0:128, :], reads=[rows], writes=[rows])
    S.dma("sp", rows[0:112, 4, :], cwv[128:240, :], reads=[rows], writes=[rows])
    S.dma("sp", rows[112:116, 4, :], rowsrc("kv_norm_g"), reads=[rows], writes=[rows])
    S.dma("sp", rows[116:124, 4, :], rowsrc("q_a_norm_g"), reads=[rows], writes=[rows])
    S.dma("sp", rows[124:125, 4, :], rowsrc("q_norm_g"), reads=[rows], writes=[rows])
    S.dma("sp", rows[125:126, 4, :], rowsrc("k_norm_g"), reads=[rows], writes=[rows])
    S.dma("sp", rows[126:127, 4, :], rowsrc("dn_norm_g"), reads=[rows], writes=[rows])
    bcol = S.sb("bcol", [128, 192])
    for i, (dst, off, n) in enumerate([(bcol, 0, 128), (bcol, 128, 64), (colT, 384, 64), (colT, 448, 128), (colT, 576, 128)]):
        pb = P[2 + i % 2]
        S.group("pe", [lambda e, i=i, n=n, pb=pb: e.transpose(pb[:, 0:n], rows[0:n, i, :], ident[0:n, 0:n])], reads=[rows, ident], writes=[pb])
        S.op("dve", lambda e, dst=dst, off=off, n=n, pb=pb: e.tensor_copy(out=dst[:, off:off + n], in_=pb[:, 0:n]), reads=[pb], writes=[dst])
    S.op("dve", lambda e: e.tensor_tensor(out=colT[:, C_ML:C_ML + 192], in0=pm_[:, 0:384:2], in1=bcol[:], op=ALU.add), reads=[pm_, bcol, colT], writes=[colT])
    S.op("dve", lambda e: e.tensor_tensor(out=colT[:, C_MC:C_MC + 192], in0=pm_[:, 1:384:2], in1=bcol[:], op=ALU.add), reads=[pm_, bcol, colT], writes=[colT])


def bcast_row(k, col0, dst, dgs):
    S, P = k.S, k.P
    for g in range(8):
        pb = P[6 + g % 2]
        for c4 in range(4):
            c = 4 * g + c4
            dg = dgs[c % 2]
            S.op("dve", lambda e, dg=dg, c=c: e.tensor_scalar(out=dg[:], in0=k.ident[:], scalar1=k.colT[:, col0 + c:col0 + c + 1], scalar2=None, op0=ALU.mult), reads=[k.ident, k.colT], writes=[dg])
            S.group("pe", [lambda e, dg=dg, c4=c4, pb=pb: e.matmul(pb[:, sl(c4)], lhsT=k.ones_f[:], rhs=dg[:], start=True, stop=True)], reads=[k.ones_f, dg], writes=[pb])
        S.op("act", lambda e, g=g, pb=pb: e.copy(out=dst[:, sl(g, 512)], in_=pb[:]), reads=[pb], writes=[dst])


def derived(k):
    S, colT = k.S, k.colT
    k.A1l = S.sb("A1l", [128, 32]); k.A1c = S.sb("A1c", [128, 32]); k.A2l = S.sb("A2l", [128, 32])
    for A, sc in ((k.A1l, C_ML + 32), (k.A1c, C_MC + 32)):
        S.op("dve", lambda e, A=A, sc=sc: e.scalar_tensor_tensor(out=A[:], in0=colT[:, sc:sc + 32], scalar=1.0, in1=colT[:, C_N1:C_N1 + 32], op0=ALU.add, op1=ALU.mult), reads=[colT], writes=[A])
    S.op("dve", lambda e: e.scalar_tensor_tensor(out=k.A2l[:], in0=colT[:, C_ML + 128:C_ML + 160], scalar=1.0, in1=colT[:, C_N2:C_N2 + 32], op0=ALU.add, op1=ALU.mult), reads=[colT], writes=[k.A2l])


def norm_transpose(k, xt, xs, junk, ss, rs, A, shoff, dstT, f32copy=None):
    S, P, ident, colT = k.S, k.P, k.ident, k.colT
    S.op("act", lambda e: e.activation(out=junk[:], in_=xt[:], func=AF.Square, accum_out=ss[:]), reads=[xt], writes=[junk, ss])
    S.op("act", lambda e: e.activation(out=rs[:], in_=ss[:], func=AF.Sqrt, bias=k.epsc[:], scale=1.0 / D), reads=[ss, k.epsc], writes=[rs])
    S.op("dve", lambda e: e.reciprocal(out=rs[:], in_=rs[:]), reads=[rs], writes=[rs])
    S.op("dve", lambda e: e.tensor_scalar(out=xs[:], in0=xt[:], scalar1=rs[:, 0:1], scalar2=None, op0=ALU.mult), reads=[xt, rs], writes=[xs])
    for g in range(8):
        pb = P[4 + g % 4]
        S.group("pe", [lambda e, c=c, pb=pb, g=g: e.transpose(pb[:, sl(c - 4 * g)], xs[:, sl(c)], ident[:]) for c in range(4 * g, 4 * g + 4)],
                reads=[xs, ident], writes=[pb])
        for c in range(4 * g, 4 * g + 4):
            if c % 2:
                S.op("act", lambda e, c=c, pb=pb, g=g: e.activation(out=dstT[:, c, :], in_=pb[:, sl(c - 4 * g)], func=AF.Identity, scale=A[:, c:c + 1], bias=colT[:, shoff + c:shoff + c + 1]),
                     reads=[pb, A, colT], writes=[dstT])
            else:
                S.op("dve", lambda e, c=c, pb=pb, g=g: e.tensor_scalar(out=dstT[:, c, :], in0=pb[:, sl(c - 4 * g)], scalar1=A[:, c:c + 1], scalar2=colT[:, shoff + c:shoff + c + 1], op0=ALU.mult, op1=ALU.add),
                     reads=[pb, A, colT], writes=[dstT])
            if f32copy is not None:
                S.op("dve", lambda e, c=c, pb=pb, g=g: e.tensor_scalar(out=f32copy[:, c, :], in0=pb[:, sl(c - 4 * g)], scalar1=A[:, c:c + 1], scalar2=colT[:, shoff + c:shoff + c + 1], op0=ALU.mult, op1=ALU.add),
                     reads=[pb, A, colT], writes=[f32copy])


def phase1(k):
    S, I = k.S, k.I
    xts = [S.sb(f"xt{i}", [128, D]) for i in range(2)]
    xss = [S.sb(f"xs{i}", [128, D]) for i in range(2)]
    hts = [S.sb(f"ht{i}", [128, 32, 128], BF16) for i in range(2)]
    junk = S.sb("junk", [128, D], BF16)
    sss = [S.sb(f"ss{i}", [128, 1]) for i in range(2)]
    rss = [S.sb(f"rs{i}", [128, 1]) for i in range(2)]
    for ti in range(NTILE):
        xt = xts[ti % 2]; ht = hts[ti % 2]
        src = I["x"][sl(ti), :] if ti < 16 else I["ctx"][sl(ti - 16), :]
        S.dma("sp", xt[:], src, writes=[xt])
        norm_transpose(k, xt, xss[ti % 2], junk, sss[ti % 2], rss[ti % 2], k.A1l if ti < 16 else k.A1c, C_ML if ti < 16 else C_MC, ht)
        S.dma("sp", k.hT.t.ap()[:, :, sl(ti)].rearrange("c p t -> p c t"), ht[:], reads=[ht], writes=[k.hT])


def phase2(k):
    S, I, P = k.S, k.I, k.P
    wb = [S.sb(f"wb{i}", [128, 32, 1024], BF16) for i in range(2)]
    hb = [S.sb(f"hb{i}", [128, 32, 512], BF16) for i in range(2)]
    ob = [S.sb(f"ob{i}", [128, 512], BF16) for i in range(2)]
    of = [S.sb(f"of{i}", [64, 512]) for i in range(3)]
    wv = I["w_in"].rearrange("(k p) n -> p k n", p=128)
    hv = k.hT.t.ap().rearrange("c p t -> p c t")
    nblk = [(n0, min(1024, IN_COLS - n0)) for n0 in range(0, IN_COLS, 1024)]
    cnt = 0
    hcnt = 0
    for bi, (n0, nw) in enumerate(nblk):
        w = wb[bi % 2]
        S.dma("pool", w[:, :, 0:nw], wv[:, :, n0:n0 + nw], writes=[w])
        for (t0, tw) in TB:
            if t0 >= T and n0 >= 4736:
                continue
            h = hb[hcnt % 2]; hcnt += 1
            S.dma("sp", h[:, :, 0:tw], hv[:, :, t0:t0 + tw], reads=[k.hT], writes=[h])
            for ci in range(nw // 128):
                ch = n0 // 128 + ci
                pb = P[cnt % 4]; o = ob[cnt % 2]; cnt += 1
                S.group("pe", [lambda e, kk=kk, w=w, h=h, pb=pb, ci=ci, tw=tw: e.matmul(pb[:, 0:tw], lhsT=w[:, kk, sl(ci)], rhs=h[:, kk, 0:tw], start=(kk == 0), stop=(kk == 31)) for kk in range(32)],
                        reads=[w, h], writes=[pb])
                if cnt % 2:
                    S.op("act", lambda e, o=o, pb=pb, tw=tw: e.copy(out=o[:, 0:tw], in_=pb[:, 0:tw]), reads=[pb], writes=[o])
                else:
                    S.op("dve", lambda e, o=o, pb=pb, tw=tw: e.tensor_copy(out=o[:, 0:tw], in_=pb[:, 0:tw]), reads=[pb], writes=[o])
                S.dma("sp", k.plT[ch][:, t0:t0 + tw], o[:, 0:tw], reads=[o], writes=[k.plT[ch]])
                if ch == 4:
                    for j, (dst, lsl, m) in enumerate(((k.kreD, slice(512 - n0, 576 - n0, 2), 32), (k.kroD, slice(513 - n0, 576 - n0, 2), 32), (k.abD, slice(576 - n0, 640 - n0), 64))):
                        pb2 = P[4 + j]; o2 = of[j]
                        S.group("pe", [lambda e, kk=kk, w=w, h=h, pb2=pb2, lsl=lsl, m=m, tw=tw: e.matmul(pb2[0:m, 0:tw], lhsT=w[:, kk, lsl], rhs=h[:, kk, 0:tw], start=(kk == 0), stop=(kk == 31)) for kk in range(32)],
                                reads=[w, h], writes=[pb2])
                        S.op("dve", lambda e, o2=o2, pb2=pb2, m=m, tw=tw: e.tensor_copy(out=o2[0:m, 0:tw], in_=pb2[0:m, 0:tw]), reads=[pb2], writes=[o2])
                        S.dma("sp", dst[:, t0:t0 + tw], o2[0:m, 0:tw], reads=[o2], writes=[dst])


def blocks(W):
    return [(t0, min(512, W - t0)) for t0 in range(0, W, 512)]


def phase3(k):
    import os
    lvl = int(os.environ.get("K3", "9"))
    S, I, P, colT = k.S, k.I, k.P, k.colT
    ones_b, ones_f, epsc = k.ones_b, k.ones_f, k.epsc
    wukvs = [S.sb(f"wukv{i}", [128, 4, 256], BF16) for i in range(2)]
    wuqs = [S.sb(f"wuq{i}", [128, 8, 192], BF16) for i in range(2)]
    wukv_v = I["w_ukv"].rearrange("(c p) n -> p c n", p=128); wuq_v = I["w_uq"].rearrange("(c p) n -> p c n", p=128)
    ckvn = S.sb("ckvn", [128, 4, NT], BF16)
    cqn = S.sb("cqn", [128, 8, T], BF16)
    kr0 = S.sb("kr0", [32, NT], BF16); kr1 = S.sb("kr1", [32, NT], BF16)
    rbc = S.sb("rbc", [128, NT])
    cosT = S.sb("cosT", [32, T]); sinT = S.sb("sinT", [32, T])
    S.dma("sp", cosT[:], I["cosT"], writes=[cosT]); S.dma("sp", sinT[:], I["sinT"], writes=[sinT])
    sqs = [S.sb(f"sq{i}", [128, 512], BF16) for i in range(3)]
    rsq = [S.sb(f"rsq{i}", [128, 512]) for i in range(2)]
    grope = S.sb("grope", [32, 4])
    for j, (nm, par) in enumerate((("q_rope_norm_g", 0), ("q_rope_norm_g", 1), ("k_rope_norm_g", 0), ("k_rope_norm_g", 1))):
        S.dma("sp", grope[:, j:j + 1], I[nm].rearrange("(i two) -> i two", two=2)[:, par:par + 1], reads=[grope], writes=[grope], allow_slow_non_contiguous=True)
    grow = S.sb("grow", [1, 384]); gm = S.sb("gm", [1, 8]); negb = S.sb("negb", [128, 1])
    for j, (nm, o, n) in enumerate((("q_norm_g", 0, 128), ("k_norm_g", 128, 128), ("q_rope_norm_g", 256, 64), ("k_rope_norm_g", 320, 64))):
        S.dma("sp", grow[:, o:o + n], I[nm].rearrange("(o n) -> o n", o=1), reads=[grow], writes=[grow])
        S.op("dve", lambda e, j=j, o=o, n=n: e.tensor_reduce(out=gm[:, j:j + 1], in_=grow[:, o:o + n], axis=AX.X, op=ALU.max, apply_absolute_value=True), reads=[grow, gm], writes=[gm])
    S.op("dve", lambda e: e.tensor_tensor(out=gm[:, 4:5], in0=gm[:, 0:1], in1=gm[:, 1:2], op=ALU.mult), reads=[gm], writes=[gm])
    S.op("dve", lambda e: e.tensor_tensor(out=gm[:, 5:6], in0=gm[:, 2:3], in1=gm[:, 3:4], op=ALU.mult), reads=[gm], writes=[gm])
    S.op("dve", lambda e: e.tensor_scalar(out=gm[:, 4:5], in0=gm[:, 4:5], scalar1=-128.0 * SCALE, scalar2=None, op0=ALU.mult), reads=[gm], writes=[gm])
    S.op("dve", lambda e: e.scalar_tensor_tensor(out=gm[:, 6:7], in0=gm[:, 5:6], scalar=-64.0 * SCALE, in1=gm[:, 4:5], op0=ALU.mult, op1=ALU.add), reads=[gm], writes=[gm])
    S.group("pe", [lambda e: e.matmul(P[7][:, 0:1], lhsT=ones_f[0:1, :], rhs=gm[:, 6:7], start=True, stop=True)], reads=[ones_f, gm], writes=[P[7]])
    S.op("dve", lambda e: e.tensor_copy(out=negb[:], in_=P[7][:, 0:1]), reads=[P[7]], writes=[negb])

    if lvl < 1:
        return
    cnt = [0]

    def sumsq_rstd(srcs, parts, nfeat, t0, tw, dst_ap, dst_res):
        pb = P[7]
        n = len(srcs)
        for ci, (res, ap) in enumerate(srcs):
            sq = sqs[cnt[0] % 3]; cnt[0] += 1
            S.op("act", lambda e, sq=sq, ap=ap: e.activation(out=sq[0:parts, 0:tw], in_=ap, func=AF.Square), reads=[res], writes=[sq])
            S.group("pe", [lambda e, sq=sq, ci=ci: e.matmul(pb[0:parts, 0:tw], lhsT=ones_b[0:parts, 0:parts], rhs=sq[0:parts, 0:tw], start=(ci == 0), stop=(ci == n - 1))],
                    reads=[sq, ones_b], writes=[pb])
        S.op("act", lambda e: e.activation(out=dst_ap, in_=pb[0:parts, 0:tw], func=AF.Sqrt, bias=epsc[0:parts, :], scale=1.0 / nfeat), reads=[pb, epsc], writes=[dst_res])
        S.op("dve", lambda e: e.reciprocal(out=dst_ap, in_=dst_ap), reads=[dst_res], writes=[dst_res])

    ld = [S.sb(f"ld{i}", [128, NT], BF16) for i in range(8)]
    for c in range(4):
        S.dma("sp", ld[c][:], k.plT[c][:], reads=[k.plT[c]], writes=[ld[c]])
    for (t0, tw) in blocks(NT):
        sumsq_rstd([(ld[c], ld[c][:, t0:t0 + tw]) for c in range(4)], 128, 512, t0, tw, rbc[:, t0:t0 + tw], rbc)
    for c in range(4):
        S.op("dve", lambda e, c=c: e.scalar_tensor_tensor(out=ckvn[:, c, :], in0=ld[c][:], scalar=colT[:, C_G + c:C_G + c + 1], in1=rbc[:], op0=ALU.mult, op1=ALU.mult), reads=[ld[c], colT, rbc], writes=[ckvn])
    for c in range(8):
        S.dma("sp", ld[c][:, 0:T], k.plT[53 + c][:, 0:T], reads=[k.plT[53 + c]], writes=[ld[c]])
    for (t0, tw) in blocks(T):
        sumsq_rstd([(ld[c], ld[c][:, t0:t0 + tw]) for c in range(8)], 128, 1024, t0, tw, rbc[:, t0:t0 + tw], rbc)
    for c in range(8):
        S.op("dve", lambda e, c=c: e.scalar_tensor_tensor(out=cqn[:, c, :], in0=ld[c][:, 0:T], scalar=colT[:, C_G + 4 + c:C_G + 5 + c], in1=rbc[:, 0:T], op0=ALU.mult, op1=ALU.mult), reads=[ld[c], colT, rbc], writes=[cqn])

    if lvl < 2:
        return
    t1 = S.sb("t1", [32, 512]); t2 = S.sb("t2", [32, 512])

    def rope(ne, no, nres, t0, tw, d0, d1, dres0, dres1, pos0):
        cs = cosT[:, pos0:pos0 + tw]; sn = sinT[:, pos0:pos0 + tw]
        S.op("dve", lambda e: e.tensor_tensor(out=t1[:, 0:tw], in0=ne, in1=cs, op=ALU.mult), reads=[nres, cosT], writes=[t1])
        S.op("pool", lambda e: e.tensor_tensor(out=t2[:, 0:tw], in0=no, in1=sn, op=ALU.mult), reads=[nres, sinT], writes=[t2])
        S.op("dve", lambda e: e.tensor_tensor(out=d0, in0=t1[:, 0:tw], in1=t2[:, 0:tw], op=ALU.subtract), reads=[t1, t2], writes=[dres0])
        S.op("dve", lambda e: e.tensor_tensor(out=t1[:, 0:tw], in0=ne, in1=sn, op=ALU.mult), reads=[nres, sinT], writes=[t1])
        S.op("pool", lambda e: e.tensor_tensor(out=t2[:, 0:tw], in0=no, in1=cs, op=ALU.mult), reads=[nres, cosT], writes=[t2])
        S.op("dve", lambda e: e.tensor_tensor(out=d1, in0=t1[:, 0:tw], in1=t2[:, 0:tw], op=ALU.add), reads=[t1, t2], writes=[dres1])

    kre = S.sb("kre", [32, NT]); kro = S.sb("kro", [32, NT]); nn = S.sb("nn", [32, 2, 512])
    S.dma("sp", kre[:], k.kreD[:], reads=[k.kreD], writes=[kre]); S.dma("sp", kro[:], k.kroD[:], reads=[k.kroD], writes=[kro])
    for (t0, tw) in blocks(NT):
        rs = rsq[0]
        sumsq_rstd([(kre, kre[:, t0:t0 + tw]), (kro, kro[:, t0:t0 + tw])], 32, 64, t0, tw, rs[0:32, 0:tw], rs)
        S.op("dve", lambda e, t0=t0, tw=tw, rs=rs: e.scalar_tensor_tensor(out=nn[:, 0, 0:tw], in0=kre[:, t0:t0 + tw], scalar=grope[:, 2:3], in1=rs[0:32, 0:tw], op0=ALU.mult, op1=ALU.mult), reads=[kre, grope, rs], writes=[nn])
        S.op("dve", lambda e, t0=t0, tw=tw, rs=rs: e.scalar_tensor_tensor(out=nn[:, 1, 0:tw], in0=kro[:, t0:t0 + tw], scalar=grope[:, 3:4], in1=rs[0:32, 0:tw], op0=ALU.mult, op1=ALU.mult), reads=[kro, grope, rs, nn], writes=[nn])
        if t0 < T:
            rope(nn[:, 0, 0:tw], nn[:, 1, 0:tw], nn, t0, tw, kr0[:, t0:t0 + tw], kr1[:, t0:t0 + tw], kr0, kr1, t0)
        else:
            S.op("dve", lambda e, t0=t0, tw=tw: e.tensor_copy(out=kr0[:, t0:t0 + tw], in_=nn[:, 0, 0:tw]), reads=[nn], writes=[kr0])
            S.op("dve", lambda e, t0=t0, tw=tw: e.tensor_copy(out=kr1[:, t0:t0 + tw], in_=nn[:, 1, 0:tw]), reads=[nn], writes=[kr1])

    if lvl < 3:
        return
    knT = S.sb("knT", [128, NT], BF16); vsb = S.sb("vsb", [128, 18, 128], BF16)
    qnT = S.sb("qnT", [128, T], BF16); qr0 = S.sb("qr0", [32, T], BF16); qr1 = S.sb("qr1", [32, T], BF16)
    pTs = [S.sb(f"pT{i}", [128, 512], BF16) for i in range(3)]
    rec = S.sb("rec", [128, 512]); ao = [S.sb(f"ao{i}", [128, 512], BF16) for i in range(2)]
    k.d3 = dict(negb=negb, knT=knT, qnT=qnT, kr0=kr0, kr1=kr1, qr0=qr0, qr1=qr1, vsb=vsb, rec=rec, pT=pTs[0], ao1=ao[1], ao0=ao[0], ckvn=ckvn, cqn=cqn, rbc=rbc, gm=gm)
    pc = 0
    for h in range(16 if lvl >= 9 else 1):
        wukv = wukvs[h % 2]; wuq = wuqs[h % 2]
        S.dma("pool", wukv[:], wukv_v[:, :, h * 256:(h + 1) * 256], writes=[wukv])
        S.dma("pool", wuq[:], wuq_v[:, :, h * 192:(h + 1) * 192], writes=[wuq])
        for (t0, tw) in blocks(NT):
            pa = P[6]
            S.group("pe", [lambda e, c=c, t0=t0, tw=tw, wukv=wukv: e.matmul(pa[:, 0:tw], lhsT=wukv[:, c, 0:128], rhs=ckvn[:, c, t0:t0 + tw], start=(c == 0), stop=(c == 3)) for c in range(4)],
                    reads=[wukv, ckvn], writes=[pa])
            rs = rsq[1]
            sumsq_rstd([(pa, pa[:, 0:tw])], 128, 128, t0, tw, rs[:, 0:tw], rs)
            S.op("dve", lambda e, t0=t0, tw=tw, rs=rs, pa=pa: e.scalar_tensor_tensor(out=knT[:, t0:t0 + tw], in0=pa[:, 0:tw], scalar=colT[:, C_G + 13:C_G + 14], in1=rs[:, 0:tw], op0=ALU.mult, op1=ALU.mult), reads=[pa, colT, rs], writes=[knT])
        for g in range(5):
            pa = P[6]
            tiles = list(range(4 * g, min(4 * g + 4, 18)))
            for ti in tiles:
                S.group("pe", [lambda e, c=c, ti=ti, g=g, wukv=wukv: e.matmul(pa[:, sl(ti - 4 * g)], lhsT=ckvn[:, c, sl(ti)], rhs=wukv[:, c, 128:256], start=(c == 0), stop=(c == 3)) for c in range(4)],
                        reads=[wukv, ckvn], writes=[pa])
            S.op("act", lambda e, g=g, n=len(tiles): e.copy(out=vsb[:, 4 * g:4 * g + n, :], in_=pa[:, 0:n * 128].rearrange("p (a b) -> p a b", b=128)), reads=[pa], writes=[vsb])
        for (t0, tw) in blocks(T):
            pa = P[6]
            S.group("pe", [lambda e, c=c, t0=t0, tw=tw, wuq=wuq: e.matmul(pa[:, 0:tw], lhsT=wuq[:, c, 0:128], rhs=cqn[:, c, t0:t0 + tw], start=(c == 0), stop=(c == 7)) for c in range(8)],
                    reads=[wuq, cqn], writes=[pa])
            rs = rsq[1]
            sumsq_rstd([(pa, pa[:, 0:tw])], 128, 128, t0, tw, rs[:, 0:tw], rs)
            S.op("dve", lambda e, t0=t0, tw=tw, rs=rs, pa=pa: e.scalar_tensor_tensor(out=qnT[:, t0:t0 + tw], in0=pa[:, 0:tw], scalar=colT[:, C_G + 12:C_G + 13], in1=rs[:, 0:tw], op0=ALU.mult, op1=ALU.mult), reads=[pa, colT, rs], writes=[qnT])
            pe_, po_ = P[4], P[5]
            for par, pp in ((0, pe_), (1, po_)):
                S.group("pe", [lambda e, c=c, t0=t0, tw=tw, par=par, pp=pp, wuq=wuq: e.matmul(pp[0:32, 0:tw], lhsT=wuq[:, c, 128 + par:192:2], rhs=cqn[:, c, t0:t0 + tw], start=(c == 0), stop=(c == 7)) for c in range(8)],
                        reads=[wuq, cqn], writes=[pp])
            rs = rsq[0]
            sumsq_rstd([(pe_, pe_[0:32, 0:tw]), (po_, po_[0:32, 0:tw])], 32, 64, t0, tw, rs[0:32, 0:tw], rs)
            S.op("dve", lambda e, tw=tw, rs=rs: e.scalar_tensor_tensor(out=nn[:, 0, 0:tw], in0=pe_[0:32, 0:tw], scalar=grope[:, 0:1], in1=rs[0:32, 0:tw], op0=ALU.mult, op1=ALU.mult), reads=[pe_, grope, rs], writes=[nn])
            S.op("dve", lambda e, tw=tw, rs=rs: e.scalar_tensor_tensor(out=nn[:, 1, 0:tw], in0=po_[0:32, 0:tw], scalar=grope[:, 1:2], in1=rs[0:32, 0:tw], op0=ALU.mult, op1=ALU.mult), reads=[po_, grope, rs, nn], writes=[nn])
            rope(nn[:, 0, 0:tw], nn[:, 1, 0:tw], nn, t0, tw, qr0[:, t0:t0 + tw], qr1[:, t0:t0 + tw], qr0, qr1, t0)
        if lvl < 4:
            break
        for qb in range(4):
            q0 = qb * 512
            po = P[2 + qb % 2]; pd = P[4 + qb % 2]
            prev = None
            for kc in range(19):
                if kc < 18:
                    ps_ = P[kc % 2]
                    S.group("pe", [lambda e, ps_=ps_, kc=kc: e.matmul(ps_[:, :], lhsT=knT[:, sl(kc)], rhs=qnT[:, q0:q0 + 512], start=True, stop=False),
                                   lambda e, ps_=ps_, kc=kc: e.matmul(ps_[:, :], lhsT=kr0[:, sl(kc)], rhs=qr0[:, q0:q0 + 512], start=False, stop=False),
                                   lambda e, ps_=ps_, kc=kc: e.matmul(ps_[:, :], lhsT=kr1[:, sl(kc)], rhs=qr1[:, q0:q0 + 512], start=False, stop=True)],
                            reads=[knT, qnT, kr0, kr1, qr0, qr1], writes=[ps_])
                    pT = pTs[pc % 3]; pc += 1
                    S.op("act", lambda e, pT=pT, ps_=ps_: e.activation(out=pT[:], in_=ps_[:], func=AF.Exp, bias=negb[:, 0:1], scale=SCALE), reads=[ps_, negb], writes=[pT])
                if prev is not None and lvl >= 5:
                    pkc, ppT = prev
                    S.group("pe", [lambda e, pkc=pkc, ppT=ppT: e.matmul(pd[:, :], lhsT=ones_b[:], rhs=ppT[:], start=(pkc == 0), stop=(pkc == 17)),
                                   lambda e, pkc=pkc, ppT=ppT: e.matmul(po[:, :], lhsT=vsb[:, pkc, :], rhs=ppT[:], start=(pkc == 0), stop=(pkc == 17))],
                            reads=[ones_b, vsb, ppT], writes=[pd, po])
                prev = (kc, pT) if kc < 18 else None
            if lvl < 6:
                continue
            S.op("act", lambda e, pd=pd: e.copy(out=rec[:], in_=pd[:]), reads=[pd], writes=[rec])
            S.op("dve", lambda e: e.reciprocal(out=rec[:], in_=rec[:]), reads=[rec], writes=[rec])
            a = ao[qb % 2]
            if lvl < 7:
                continue
            S.op("dve", lambda e, a=a, po=po: e.tensor_tensor(out=a[:], in0=po[:], in1=rec[:], op=ALU.mult), reads=[po, rec], writes=[a])
            if lvl < 8:
                continue
            S.dma("sp", k.attD[h][:, q0:q0 + 512], a[:], reads=[a], writes=[k.attD[h]])
            if os.environ.get("K3BAR"):
                S.barrier()


def phase4(k):
    S, I, P, colT = k.S, k.I, k.P, k.colT
    ident, ones_f, ones_b, epsc = k.ident, k.ones_f, k.ones_b, k.epsc
    NG = 18 * 32
    cons = {}
    for nm in ("triF", "triB", "pmF", "pmB", "nmF", "nmB"):
        cons[nm] = S.sb("c_" + nm, [128, 128]); S.dma("sp", cons[nm][:], I[nm], writes=[cons[nm]])
    tri = (cons["triF"], cons["triB"]); pm = (cons["pmF"], cons["pmB"]); nm_ = (cons["nmF"], cons["nmB"])
    abT = S.sb("abT", [64, NT]); S.dma("sp", abT[:], k.abD[:], reads=[k.abD], writes=[abT])
    ab = S.sb("ab", [128, 18, 64])
    for g in range(5):
        tiles = list(range(4 * g, min(4 * g + 4, 18)))
        pa = P[g % 2]
        S.group("pe", [lambda e, ti=ti, g=g: e.transpose(pa[:, (ti - 4 * g) * 64:(ti - 4 * g + 1) * 64], abT[:, sl(ti)], ident[0:64, 0:64]) for ti in tiles], reads=[abT, ident], writes=[pa])
        S.op("dve", lambda e, g=g, n=len(tiles), pa=pa: e.tensor_copy(out=ab[:, 4 * g:4 * g + n, :], in_=pa[:, 0:n * 64].rearrange("p (a b) -> p a b", b=64)), reads=[pa], writes=[ab])
    dtb = S.sb("dtb", [128, 32]); negA = S.sb("negA", [128, 32])
    S.dma("sp", dtb[:], I["dt_bias"].partition_broadcast(128), writes=[dtb])
    S.dma("sp", negA[:], I["a_log"].partition_broadcast(128), writes=[negA])
    S.op("act", lambda e: e.activation(out=negA[:], in_=negA[:], func=AF.Exp), reads=[negA], writes=[negA])
    S.op("dve", lambda e: e.tensor_scalar(out=negA[:], in0=negA[:], scalar1=-1.0, scalar2=None, op0=ALU.mult), reads=[negA], writes=[negA])
    names = ("la", "lb", "beta", "g", "glast", "a", "eg", "c1", "kts", "egl")
    G_ = {n: S.sb("g_" + n, [128, 18, 32]) for n in names}
    la, lb, beta, gg, glast, aa, eg, c1, kts, egl = (G_[n] for n in names)
    one_c = ones_f[:, 0:1]
    bc = lambda t: t[:].unsqueeze(1).to_broadcast([128, 18, 32])
    S.op("dve", lambda e: e.tensor_tensor(out=la[:], in0=ab[:, :, 0:32], in1=bc(dtb), op=ALU.add), reads=[ab, dtb], writes=[la])
    S.op("act", lambda e: e.activation(out=la[:], in_=la[:], func=AF.Exp), reads=[la], writes=[la])
    S.op("act", lambda e: e.activation(out=la[:], in_=la[:], func=AF.Ln, bias=one_c), reads=[la, ones_f], writes=[la])
    S.op("dve", lambda e: e.tensor_tensor(out=la[:], in0=la[:], in1=bc(negA), op=ALU.mult), reads=[la, negA], writes=[la])
    S.op("act", lambda e: e.activation(out=lb[:], in_=ab[:, :, 32:64], func=AF.Exp, scale=-1.0), reads=[ab], writes=[lb])
    S.op("act", lambda e: e.activation(out=lb[:], in_=lb[:], func=AF.Ln, bias=one_c), reads=[lb, ones_f], writes=[lb])
    S.op("act", lambda e: e.activation(out=beta[:], in_=lb[:], func=AF.Exp, scale=-1.0), reads=[lb], writes=[beta])
    S.op("dve", lambda e: e.tensor_scalar(out=lb[:], in0=lb[:], scalar1=-1.0, scalar2=None, op0=ALU.mult), reads=[lb], writes=[lb])
    for half in range(2):
        tiles = list(range(9 * half, 9 * half + 9))
        pa, pb = P[2], P[3]
        for ti in tiles:
            j = ti - 9 * half
            S.group("pe", [lambda e, ti=ti, j=j: e.matmul(pa[:, j * 32:j * 32 + 16], lhsT=tri[0][:], rhs=la[:, ti, 0:16], start=True, stop=True),
                           lambda e, ti=ti, j=j: e.matmul(pa[:, j * 32 + 16:j * 32 + 32], lhsT=tri[1][:], rhs=la[:, ti, 16:32], start=True, stop=True),
                           lambda e, ti=ti, j=j: e.matmul(pb[:, j * 32:j * 32 + 32], lhsT=ones_f[:], rhs=la[:, ti, :], start=True, stop=True)],
                    reads=[tri[0], tri[1], la, ones_f], writes=[pa, pb])
        S.op("dve", lambda e, half=half, pa=pa: e.tensor_copy(out=gg[:, 9 * half:9 * half + 9, :], in_=pa[:, 0:288].rearrange("p (a b) -> p a b", b=32)), reads=[pa], writes=[gg])
        S.op("dve", lambda e, half=half, pb=pb: e.tensor_copy(out=glast[:, 9 * half:9 * half + 9, :], in_=pb[:, 0:288].rearrange("p (a b) -> p a b", b=32)), reads=[pb], writes=[glast])
    S.op("dve", lambda e: e.tensor_tensor(out=aa[:], in0=gg[:], in1=lb[:], op=ALU.add), reads=[gg, lb], writes=[aa])
    S.op("act", lambda e: e.activation(out=eg[:], in_=gg[:], func=AF.Exp), reads=[gg], writes=[eg])
    S.op("dve", lambda e: e.scalar_tensor_tensor(out=c1[:], in0=beta[:], scalar=-1.0, in1=eg[:], op0=ALU.mult, op1=ALU.mult), reads=[beta, eg], writes=[c1])
    S.op("dve", lambda e: e.tensor_tensor(out=kts[:], in0=glast[:], in1=gg[:], op=ALU.subtract), reads=[glast, gg], writes=[kts])
    S.op("act", lambda e: e.activation(out=kts[:], in_=kts[:], func=AF.Exp), reads=[kts], writes=[kts])
    S.op("act", lambda e: e.activation(out=egl[:], in_=glast[:], func=AF.Exp), reads=[glast], writes=[egl])

    import os
    lvl4 = int(os.environ.get("K4", "9"))
    if lvl4 < 1:
        return
    xin = S.sb("xin", [128, NT], BF16)
    kTf = S.sb("kTf", [128, NT]); vTf = S.sb("vTf", [128, NT]); qTf = S.sb("qTf", [128, T])
    kTb = S.sb("kTb", [128, NT], BF16); qTb = S.sb("qTb", [128, T], BF16)
    ktok = S.sb("ktok", [128, 18, 128]); vtok = S.sb("vtok", [128, 18, 128])
    Qall = S.sb("Qall", [128, 36, 128]); QKall = S.sb("QKall", [128, 32, 128], BF16)
    oall = S.sb("oall", [128, 16, 128])
    sqb = S.sb("sqb", [128, 512], BF16); rsb = S.sb("rsb", [128, 512])
    zs = S.sb("zs", [128, T]); odo = S.sb("odo", [128, T], BF16)
    tmp = {}
    for c4 in range(4):
        for nm in ("LT", "u", "F", "A", "B", "A2", "B2", "Q", "v2"):
            tmp[(nm, c4)] = S.sb(f"t_{nm}{c4}", [128, 128])
    Gs2 = [S.sb(f"Gs2_{i}", [128, 128]) for i in range(2)]; GqT2 = [S.sb(f"GqT2_{i}", [128, 128]) for i in range(2)]
    for d in range(2):
        for nm in ("vb", "X"):
            tmp[(nm, d)] = S.sb(f"t_{nm}{d}", [128, 128])
        for nm in ("ktl", "vn", "Sb"):
            tmp[(nm, d)] = S.sb(f"t_{nm}{d}", [128, 128], BF16)
        tmp[("Sf", d)] = S.sb(f"t_Sf{d}", [128, 128])
    ssn = S.sb("ssn", [128, 16]); jk = S.sb("jk", [128, 128], BF16)

    def conv_silu(src_ch, conv_ch, dst, W):
        S.dma("sp", xin[:, 0:W], k.plT[src_ch][:, 0:W], reads=[k.plT[src_ch]], writes=[xin])
        segs = [(0, T)] + ([(T, TC)] if W > T else [])
        wcol = lambda j: colT[:, C_CW + j * 48 + conv_ch:C_CW + j * 48 + conv_ch + 1]
        for si, (s0, L) in enumerate(segs):
            eng = "dve"
            S.op(eng, lambda e, s0=s0, L=L: e.tensor_scalar(out=dst[:, s0:s0 + L], in0=xin[:, s0:s0 + L], scalar1=wcol(2), scalar2=None, op0=ALU.mult), reads=[xin, colT], writes=[dst])
            for j in (0, 1, 3, 4):
                sft = j - 2
                a0 = max(0, -sft); a1 = L - max(0, sft)
                S.op(eng, lambda e, s0=s0, a0=a0, a1=a1, sft=sft, j=j: e.scalar_tensor_tensor(out=dst[:, s0 + a0:s0 + a1], in0=xin[:, s0 + a0 + sft:s0 + a1 + sft], scalar=wcol(j), in1=dst[:, s0 + a0:s0 + a1], op0=ALU.mult, op1=ALU.add),
                     reads=[xin, colT, dst], writes=[dst])
        S.op("act", lambda e: e.activation(out=dst[:, 0:W], in_=dst[:, 0:W], func=AF.Silu), reads=[dst], writes=[dst])

    def l2n(src, W, dstb, dstf, scale):
        for (t0, tw) in blocks(W):
            pa = P[7]
            S.op("act", lambda e, t0=t0, tw=tw: e.activation(out=sqb[:, 0:tw], in_=src[:, t0:t0 + tw], func=AF.Square), reads=[src], writes=[sqb])
            S.group("pe", [lambda e, tw=tw: e.matmul(pa[:, 0:tw], lhsT=ones_b[:], rhs=sqb[:, 0:tw], start=True, stop=True)], reads=[sqb, ones_b], writes=[pa])
            S.op("act", lambda e, tw=tw: e.activation(out=rsb[:, 0:tw], in_=pa[:, 0:tw], func=AF.Sqrt, bias=epsc[:], scale=1.0), reads=[pa, epsc], writes=[rsb])
            S.op("dve", lambda e, tw=tw: e.reciprocal(out=rsb[:, 0:tw], in_=rsb[:, 0:tw]), reads=[rsb], writes=[rsb])
            S.op("dve", lambda e, t0=t0, tw=tw: e.scalar_tensor_tensor(out=dstb[:, t0:t0 + tw], in0=src[:, t0:t0 + tw], scalar=scale, in1=rsb[:, 0:tw], op0=ALU.mult, op1=ALU.mult), reads=[src, rsb], writes=[dstb])
            if dstf is not None:
                S.op("pool", lambda e, t0=t0, tw=tw: e.tensor_tensor(out=dstf[:, t0:t0 + tw], in0=src[:, t0:t0 + tw], in1=rsb[:, 0:tw], op=ALU.mult), reads=[src, rsb], writes=[dstf])

    import os
    k.d4 = dict(oall=oall, Qall=Qall, QKall=QKall, kTb=kTb, qTb=qTb, vtok=vtok, ktok=ktok, gg=gg, la=la, beta=beta, lb=lb, glast=glast)
    for h in range(int(os.environ.get("K4H", "16"))):
        conv_silu(5 + h, h, kTf, NT)
        conv_silu(21 + h, 16 + h, vTf, NT)
        conv_silu(37 + h, 32 + h, qTf, T)
        l2n(kTf, NT, kTb, kTf, 1.0)
        l2n(qTf, T, qTb, None, 128 ** -0.5)
        for src, dst in ((kTf, ktok), (vTf, vtok)):
            for g in range(5):
                tiles = list(range(4 * g, min(4 * g + 4, 18)))
                pa = P[g % 2]
                S.group("pe", [lambda e, ti=ti, g=g, src=src, pa=pa: e.transpose(pa[:, sl(ti - 4 * g)], src[:, sl(ti)], ident[:]) for ti in tiles], reads=[src, ident], writes=[pa])
                S.op("act", lambda e, g=g, n=len(tiles), dst=dst, pa=pa: e.copy(out=dst[:, 4 * g:4 * g + n, :], in_=pa[:, 0:n * 128].rearrange("p (a b) -> p a b", b=128)), reads=[pa], writes=[dst])
        if lvl4 < 2:
            continue
        X = lambda nm, c: tmp[(nm, c)]
        PO = [(16, 17), (0, 1), (14, 15), (2, 3), (12, 13), (4, 5), (10, 11), (6, 7), (8, 9)]
        order = ([16, 17] + list(range(16)), [17, 16] + list(range(15, -1, -1)))
        ptr = [0, 0]; done = set()
        S.op("pool", lambda e: e.memset(oall[:], 0.0), writes=[oall])
        for d in range(2):
            S.op("pool", lambda e, d=d: e.memset(tmp[("Sf", d)][:], 0.0), writes=[tmp[("Sf", d)]])
            S.op("pool", lambda e, d=d: e.memset(tmp[("Sb", d)][:], 0.0), writes=[tmp[("Sb", d)]])
        for tp in range(9):
            pair = PO[tp]
            chains = [(sub * 2 + d, pair[sub], d) for sub in range(2) for d in range(2)]
            for sub in range(2):
                ti = pair[sub]
                lat = ti < 16
                pg = P[4 * sub]
                S.group("pe", [lambda e, ti=ti, pg=pg: e.matmul(pg[:, 256:384], lhsT=kTb[:, sl(ti)], rhs=kTb[:, sl(ti)], start=True, stop=True)], reads=[kTb], writes=[pg])
                S.op("act", lambda e, sub=sub, pg=pg: e.copy(out=Gs2[sub][:], in_=pg[:, 256:384]), reads=[pg], writes=[Gs2[sub]])
                if lat:
                    S.group("pe", [lambda e, ti=ti, pg=pg: e.matmul(pg[:, 384:512], lhsT=kTb[:, sl(ti)], rhs=qTb[:, sl(ti)], start=True, stop=True)], reads=[kTb, qTb], writes=[pg])
                    S.op("act", lambda e, sub=sub, pg=pg: e.copy(out=GqT2[sub][:], in_=pg[:, 384:512]), reads=[pg], writes=[GqT2[sub]])
            for (c, ti, d) in chains:
                lat = ti < 16; sub = c // 2
                cd = d * 16 + h
                px = P[2 * c]
                S.op("dve", lambda e, c=c, d=d, cd=cd, ti=ti: e.tensor_scalar(out=X("LT", c)[:], in0=tri[d][:], scalar1=la[:, ti, cd:cd + 1], scalar2=None, op0=ALU.mult), reads=[tri[d], la], writes=[X("LT", c)])
                S.group("pe", [lambda e, c=c, px=px: e.matmul(px[:, 0:128], lhsT=ones_f[:], rhs=X("LT", c)[:], start=True, stop=True)], reads=[ones_f, X("LT", c)], writes=[px])
                S.op("dve", lambda e, c=c, d=d, cd=cd, ti=ti, px=px: e.scalar_tensor_tensor(out=X("u", c)[:], in0=px[:, 0:128], scalar=aa[:, ti, cd:cd + 1], in1=pm[d][:], op0=ALU.subtract, op1=ALU.max), reads=[px, aa, pm[d]], writes=[X("u", c)])
                if lat:
                    S.op("dve", lambda e, c=c, d=d, cd=cd, ti=ti, px=px: e.scalar_tensor_tensor(out=X("v2", c)[:], in0=px[:, 0:128], scalar=gg[:, ti, cd:cd + 1], in1=nm_[d][:], op0=ALU.subtract, op1=ALU.min), reads=[px, gg, nm_[d]], writes=[X("v2", c)])
                S.op("act", lambda e, c=c: e.activation(out=X("F", c)[:], in_=X("u", c)[:], func=AF.Exp, scale=-1.0), reads=[X("u", c)], writes=[X("F", c)])
                S.op("dve", lambda e, c=c, sub=sub: e.scalar_tensor_tensor(out=X("A", c)[:], in0=Gs2[sub][:], scalar=-1.0, in1=X("F", c)[:], op0=ALU.mult, op1=ALU.mult), reads=[Gs2[sub], X("F", c)], writes=[X("A", c)])
                if lat:
                    S.op("act", lambda e, c=c: e.activation(out=X("v2", c)[:], in_=X("v2", c)[:], func=AF.Exp), reads=[X("v2", c)], writes=[X("v2", c)])
                    S.op("pool", lambda e, c=c, d=d, ti=ti, sub=sub: e.tensor_tensor(out=QKall[:, d * 16 + ti, :], in0=GqT2[sub][:], in1=X("v2", c)[:], op=ALU.mult), reads=[GqT2[sub], X("v2", c)], writes=[QKall])
            for (c, ti, d) in chains:
                px = P[2 * c]
                S.group("pe", [lambda e, c=c, px=px: e.transpose(px[:, 128:256], X("A", c)[:], ident[:])], reads=[X("A", c), ident], writes=[px])
                S.op("act", lambda e, c=c, px=px: e.copy(out=X("B", c)[:], in_=px[:, 128:256]), reads=[px], writes=[X("B", c)])
                S.op("dve", lambda e, c=c, px=px: e.tensor_tensor(out=X("Q", c)[:], in0=px[:, 128:256], in1=ident[:], op=ALU.add), reads=[px, ident], writes=[X("Q", c)])
            cur = {c: ("A", "B") for c in range(4)}
            for lvl in range(6):
                last = lvl == 5
                for (c, ti, d) in chains:
                    An, Bn = cur[c]
                    A2n, B2n = ("A2", "B2") if An == "A" else ("A", "B")
                    px, py = P[2 * c], P[2 * c + 1]
                    S.group("pe", [lambda e, c=c, An=An, Bn=Bn, px=px: e.matmul(px[:, 0:128], lhsT=X(Bn, c)[:], rhs=X(An, c)[:], start=True, stop=True)], reads=[X(An, c), X(Bn, c)], writes=[px])
                    S.op("act", lambda e, c=c, A2n=A2n, px=px: e.copy(out=X(A2n, c)[:], in_=px[:, 0:128]), reads=[px], writes=[X(A2n, c)])
                    if not last:
                        S.group("pe", [lambda e, c=c, An=An, Bn=Bn, py=py: e.matmul(py[:, 0:128], lhsT=X(An, c)[:], rhs=X(Bn, c)[:], start=True, stop=True)], reads=[X(An, c), X(Bn, c)], writes=[py])
                        S.op("dve", lambda e, c=c, B2n=B2n, py=py: e.tensor_copy(out=X(B2n, c)[:], in_=py[:, 0:128]), reads=[py], writes=[X(B2n, c)])
                    cur[c] = (A2n, B2n)
                for (c, ti, d) in chains:
                    A2n = cur[c][0]
                    py = P[2 * c + 1]
                    S.group("pe", [lambda e, c=c, A2n=A2n, py=py: e.matmul(py[:, 256:384], lhsT=X(A2n, c)[:], rhs=X("Q", c)[:], start=True, stop=True)], reads=[X(A2n, c), X("Q", c)], writes=[py])
                    dstQ = Qall[:, d * 18 + ti, :] if last else X("Q", c)[:]
                    dres = Qall if last else X("Q", c)
                    S.op("dve", lambda e, c=c, py=py, dstQ=dstQ: e.tensor_tensor(out=dstQ, in0=py[:, 256:384], in1=X("Q", c)[:], op=ALU.add), reads=[py, X("Q", c)], writes=[dres])
            done.update(pair)
            progressed = True
            while progressed:
                progressed = False
                for d in range(2):
                    if ptr[d] < 18 and order[d][ptr[d]] in done:
                        ti = order[d][ptr[d]]; ptr[d] += 1; progressed = True
                        lat = ti < 16; cd = d * 16 + h
                        Sf, Sb, vb, Xx, vn, ktl = (tmp[(n, d)] for n in ("Sf", "Sb", "vb", "X", "vn", "ktl"))
                        pks, pvn, pqs, po2, pds = P[0 + d], P[2 + d], P[4 + d], P[6 + d], P[6 + d]
                        S.op("pool", lambda e, ti=ti, cd=cd, vb=vb: e.tensor_scalar(out=vb[:], in0=vtok[:, ti, :], scalar1=beta[:, ti, cd:cd + 1], scalar2=None, op0=ALU.mult), reads=[vtok, beta], writes=[vb])
                        S.op("pool", lambda e, ti=ti, cd=cd, ktl=ktl: e.tensor_scalar(out=ktl[:], in0=ktok[:, ti, :], scalar1=kts[:, ti, cd:cd + 1], scalar2=None, op0=ALU.mult), reads=[ktok, kts], writes=[ktl])
                        S.group("pe", [lambda e, ti=ti, Sb=Sb, pks=pks: e.matmul(pks[:, 0:128], lhsT=kTb[:, sl(ti)], rhs=Sb[:], start=True, stop=True)], reads=[kTb, Sb], writes=[pks])
                        if lat:
                            S.group("pe", [lambda e, ti=ti, Sb=Sb, pqs=pqs: e.matmul(pqs[:, 0:128], lhsT=qTb[:, sl(ti)], rhs=Sb[:], start=True, stop=True)], reads=[qTb, Sb], writes=[pqs])
                        S.op("dve", lambda e, ti=ti, cd=cd, pks=pks, vb=vb, Xx=Xx: e.scalar_tensor_tensor(out=Xx[:], in0=pks[:, 0:128], scalar=c1[:, ti, cd:cd + 1], in1=vb[:], op0=ALU.mult, op1=ALU.add), reads=[pks, c1, vb], writes=[Xx])
                        S.group("pe", [lambda e, ti=ti, d=d, Xx=Xx, pvn=pvn: e.matmul(pvn[:, 0:128], lhsT=Qall[:, d * 18 + ti, :], rhs=Xx[:], start=True, stop=True)], reads=[Qall, Xx], writes=[pvn])
                        S.op("act", lambda e, vn=vn, pvn=pvn: e.copy(out=vn[:], in_=pvn[:, 0:128]), reads=[pvn], writes=[vn])
                        if lat:
                            S.group("pe", [lambda e, ti=ti, d=d, vn=vn, po2=po2: e.matmul(po2[:, 128:256], lhsT=QKall[:, d * 16 + ti, :], rhs=vn[:], start=True, stop=True)], reads=[QKall, vn], writes=[po2])
                            S.op("dve", lambda e, ti=ti, cd=cd, pqs=pqs: e.scalar_tensor_tensor(out=oall[:, ti, :], in0=pqs[:, 0:128], scalar=eg[:, ti, cd:cd + 1], in1=oall[:, ti, :], op0=ALU.mult, op1=ALU.add), reads=[pqs, eg, oall], writes=[oall])
                            S.op("dve", lambda e, ti=ti, po2=po2: e.tensor_tensor(out=oall[:, ti, :], in0=po2[:, 128:256], in1=oall[:, ti, :], op=ALU.add), reads=[po2, oall], writes=[oall])
                        S.group("pe", [lambda e, ktl=ktl, vn=vn, pds=pds: e.matmul(pds[:, 0:128], lhsT=ktl[:], rhs=vn[:], start=True, stop=True)], reads=[ktl, vn], writes=[pds])
                        S.op("dve", lambda e, ti=ti, cd=cd, Sf=Sf, pds=pds: e.scalar_tensor_tensor(out=Sf[:], in0=Sf[:], scalar=egl[:, ti, cd:cd + 1], in1=pds[:, 0:128], op0=ALU.mult, op1=ALU.add), reads=[Sf, egl, pds], writes=[Sf])
                        S.op("act", lambda e, Sf=Sf, Sb=Sb: e.copy(out=Sb[:], in_=Sf[:]), reads=[Sf], writes=[Sb])
        if lvl4 < 4:
            continue
        for ti in range(16):
            S.op("act", lambda e, ti=ti: e.activation(out=jk[:], in_=oall[:, ti, :], func=AF.Square, accum_out=ssn[:, ti:ti + 1]), reads=[oall], writes=[jk, ssn])
        S.op("act", lambda e: e.activation(out=ssn[:], in_=ssn[:], func=AF.Sqrt, bias=epsc[:], scale=1.0 / 128), reads=[ssn, epsc], writes=[ssn])
        S.op("dve", lambda e: e.reciprocal(out=ssn[:], in_=ssn[:]), reads=[ssn], writes=[ssn])
        S.op("dve", lambda e: e.tensor_tensor(out=oall[:], in0=oall[:], in1=ssn[:].unsqueeze(2).to_broadcast([128, 16, 128]), op=ALU.mult), reads=[oall, ssn], writes=[oall])
        S.dma("sp", xin[:, 0:T], k.plT[61 + h][:, 0:T], reads=[k.plT[61 + h]], writes=[xin])
        S.op("act", lambda e: e.activation(out=zs[:], in_=xin[:, 0:T], func=AF.Silu), reads=[xin], writes=[zs])
        for g in range(4):
            pa = P[g % 2]
            S.group("pe", [lambda e, ti=ti, g=g, pa=pa: e.transpose(pa[:, sl(ti - 4 * g)], oall[:, ti, :], ident[:]) for ti in range(4 * g, 4 * g + 4)], reads=[oall, ident], writes=[pa])
            S.op("dve", lambda e, g=g, pa=pa: e.scalar_tensor_tensor(out=odo[:, sl(g, 512)], in0=pa[:], scalar=colT[:, C_G + 14:C_G + 15], in1=zs[:, sl(g, 512)], op0=ALU.mult, op1=ALU.mult), reads=[pa, colT, zs], writes=[odo])
        S.dma("sp", k.odD[h][:], odo[:], reads=[odo], writes=[k.odD[h]])


def phase5(k):
    S, I, P, colT = k.S, k.I, k.P, k.colT
    g1bc = S.sb("g1bc", [128, D])
    dgs5 = [S.sb(f"dg5_{i}", [128, 128]) for i in range(2)]
    bcast_row(k, C_ML + 64, g1bc, dgs5)
    wr = S.sb("wr", [128, 32, 36])
    S.dma("sp", wr[:, :, 0:4], I["w_rg"].rearrange("(c p) n -> p c n", p=128), writes=[wr])
    S.dma("sp", wr[:, :, 4:36], I["w_re"].rearrange("(c p) n -> p c n", p=128), reads=[wr], writes=[wr])
    brb = S.sb("brb", [128, 36])
    S.dma("sp", brb[:, 0:4], I["b_rg"].partition_broadcast(128), writes=[brb])
    S.dma("sp", brb[:, 4:36], I["b_re"].partition_broadcast(128), reads=[brb], writes=[brb])
    minT = S.sb("minT", [128, 32, 512], BF16)
    big = S.sb("big5", [128, 16384])
    bv = big.t
    attb = Res(bv[:, 0:4096].bitcast(BF16).rearrange("p (h t) -> p h t", t=512), "attb")
    odb = Res(bv[:, 4096:8192].bitcast(BF16).rearrange("p (h t) -> p h t", t=512), "odb")
    wab = [Res(bv[:, 8192 + 2048 * i:10240 + 2048 * i].bitcast(BF16).rearrange("p (h t) -> p h t", t=256), f"wab{i}") for i in range(2)]
    rest5 = Res(bv[:, 12288:16384], "rest5")
    xs4 = bv[:].rearrange("p (a b) -> p a b", b=D)
    allbig = [attb, odb, wab[0], wab[1], rest5]
    gsb = [S.sb(f"gsb{i}", [128, 512], BF16) for i in range(2)]
    gsg = [S.sb(f"gsg{i}", [128, 512]) for i in range(2)]
    tmpa = S.sb("tmpa", [128, 512])
    wo = [S.sb(f"wo{i}", [128, 32, 128], BF16) for i in range(2)]
    xt = S.sb("xt5", [128, D]); xs = S.sb("xs5", [128, D]); junk = xs
    ss = S.sb("ss5", [128, 1]); rs = S.sb("rs5", [128, 1])
    ht = S.sb("ht5", [128, 32, 128], BF16); hf = S.sb("hf5", [128, 32, 128])
    lg = S.sb("lg", [128, 36]); sm = S.sb("sm", [128, 64])
    woav = I["w_oa"].rearrange("(h p) n -> p h n", p=128); wobv = I["w_ob"].rearrange("(h p) n -> p h n", p=128)
    woutv = I["w_out"].rearrange("(c p) n -> p c n", p=128)
    wi = 0
    for tb in range(4):
        t0 = tb * 512
        for h in range(16):
            S.dma("sp", attb[:, h, :], k.attD[h][:, t0:t0 + 512], reads=[k.attD[h], attb], writes=[attb])
            S.dma("sp", odb[:, h, :], k.odD[h][:, t0:t0 + 512], reads=[k.odD[h], odb], writes=[odb])
        for nb in range(16):
            wa = wab[0]; wbb = wab[1]
            S.dma("pool", wa[:], woav[:, :, sl(nb, 256)], writes=[wa])
            S.dma("pool", wbb[:], wobv[:, :, sl(nb, 256)], writes=[wbb])
            for ci in range(2):
                ch = nb * 2 + ci
                pa, pb = P[0], P[1]
                S.group("pe", [lambda e, h=h, ci=ci: e.matmul(pa[:, :], lhsT=wa[:, h, sl(ci)], rhs=attb[:, h, :], start=(h == 0), stop=(h == 15)) for h in range(16)], reads=[wa, attb], writes=[pa])
                S.group("pe", [lambda e, h=h, ci=ci: e.matmul(pb[:, :], lhsT=wbb[:, h, sl(ci)], rhs=odb[:, h, :], start=(h == 0), stop=(h == 15)) for h in range(16)], reads=[wbb, odb], writes=[pb])
                for j, pp in ((0, pa), (1, pb)):
                    gch = 77 + j * 32 + ch
                    S.dma("sp", gsb[j][:], k.plT[gch][:, t0:t0 + 512], reads=[k.plT[gch]], writes=[gsb[j]])
                    S.op("act", lambda e, j=j: e.activation(out=gsg[j][:], in_=gsb[j][:], func=AF.Sigmoid), reads=[gsb[j]], writes=[gsg[j]])
                S.op("dve", lambda e: e.tensor_tensor(out=tmpa[:], in0=pa[:], in1=gsg[0][:], op=ALU.mult), reads=[pa, gsg[0]], writes=[tmpa])
                S.op("dve", lambda e: e.tensor_tensor(out=gsg[1][:], in0=pb[:], in1=gsg[1][:], op=ALU.mult), reads=[pb, gsg[1]], writes=[gsg[1]])
                S.op("dve", lambda e, ch=ch: e.tensor_tensor(out=minT[:, ch, :], in0=tmpa[:], in1=gsg[1][:], op=ALU.add), reads=[tmpa, gsg[1]], writes=[minT])
        for nb in range(32):
            w = wo[wi % 2]; wi += 1
            S.dma("pool", w[:], woutv[:, :, sl(nb, 128)], writes=[w])
            pp = P[2 + nb % 2]
            for tt in range(4):
                S.group("pe", [lambda e, c=c, w=w, pp=pp, tt=tt: e.matmul(pp[:, sl(tt)], lhsT=minT[:, c, sl(tt)], rhs=w[:, c, :], start=(c == 0), stop=(c == 31)) for c in range(32)], reads=[minT, w], writes=[pp])
            S.op("dve", lambda e, pp=pp, nb=nb: e.tensor_tensor(out=xs4[:, :, sl(nb, 128)], in0=pp[:, :].rearrange("p (a b) -> p a b", b=128), in1=g1bc[:, sl(nb, 128)].unsqueeze(1).to_broadcast([128, 4, 128]), op=ALU.mult), reads=[pp, g1bc] + allbig, writes=allbig)
        for tt in range(4):
            ti = tb * 4 + tt
            S.dma("sp", xt[:], I["x"][sl(ti), :], writes=[xt])
            S.op("pool", lambda e, tt=tt: e.tensor_tensor(out=xt[:], in0=xt[:], in1=xs4[:, tt, :], op=ALU.add), reads=[xt] + allbig, writes=[xt])
            S.dma("sp", k.xl1D[sl(ti), :], xt[:], reads=[xt], writes=[k.xl1D])
            norm_transpose(k, xt, xs, junk, ss, rs, k.A2l, C_ML + 96, ht, f32copy=hf)
            S.dma("sp", k.hl2D.t.ap()[:, :, sl(ti)].rearrange("c p t -> p c t"), ht[:], reads=[ht], writes=[k.hl2D])
            pr = P[4]
            S.group("pe", [lambda e, c=c: e.matmul(pr[:, 0:36], lhsT=hf[:, c, :], rhs=wr[:, c, :], start=(c == 0), stop=(c == 31)) for c in range(32)], reads=[hf, wr], writes=[pr])
            S.op("dve", lambda e: e.tensor_tensor(out=lg[:], in0=pr[:, 0:36], in1=brb[:], op=ALU.add), reads=[pr, brb], writes=[lg])
            router(k, lg, sm, ti)


def router(k, lg, sm, ti):
    S = k.S
    g = k.gates

    def dv(fn):
        S.op("dve", fn, reads=[lg, sm, g], writes=[sm, g])
    dv(lambda e: e.tensor_reduce(out=sm[:, 0:1], in_=lg[:, 0:4], axis=AX.X, op=ALU.max))
    dv(lambda e: e.tensor_scalar(out=sm[:, 4:8], in0=lg[:, 0:4], scalar1=sm[:, 0:1], scalar2=None, op0=ALU.subtract))
    S.op("act", lambda e: e.activation(out=sm[:, 8:12], in_=sm[:, 4:8], func=AF.Exp, accum_out=sm[:, 1:2]), reads=[sm], writes=[sm])
    dv(lambda e: e.reciprocal(out=sm[:, 2:3], in_=sm[:, 1:2]))
    dv(lambda e: e.tensor_scalar(out=sm[:, 12:16], in0=lg[:, 0:4], scalar1=sm[:, 0:1], scalar2=None, op0=ALU.is_equal))
    dv(lambda e: e.tensor_scalar(out=sm[:, 12:16], in0=sm[:, 12:16], scalar1=1e4, scalar2=-1e4, op0=ALU.mult, op1=ALU.add))
    dv(lambda e: e.tensor_tensor(out=g[:, ti, :].rearrange("p (a b) -> p a b", b=8), in0=lg[:, 4:36].rearrange("p (a b) -> p a b", b=8),
                                  in1=sm[:, 12:16].unsqueeze(2).to_broadcast([128, 4, 8]), op=ALU.add))
    dv(lambda e: e.tensor_reduce(out=sm[:, 16:17], in_=g[:, ti, :], axis=AX.X, op=ALU.max))
    dv(lambda e: e.tensor_scalar(out=sm[:, 32:64], in0=g[:, ti, :], scalar1=sm[:, 16:17], scalar2=None, op0=ALU.is_equal))
    dv(lambda e: e.scalar_tensor_tensor(out=sm[:, 20:21].to_broadcast([128, 32]) if False else g[:, ti, :], in0=sm[:, 32:64], scalar=-1e4, in1=g[:, ti, :], op0=ALU.mult, op1=ALU.add))
    dv(lambda e: e.tensor_reduce(out=sm[:, 17:18], in_=g[:, ti, :], axis=AX.X, op=ALU.max))
    dv(lambda e: e.tensor_tensor(out=sm[:, 18:19], in0=sm[:, 17:18], in1=sm[:, 16:17], op=ALU.subtract))
    S.op("act", lambda e: e.activation(out=sm[:, 19:20], in_=sm[:, 18:19], func=AF.Exp), reads=[sm], writes=[sm])
    dv(lambda e: e.tensor_scalar(out=sm[:, 20:21], in0=sm[:, 19:20], scalar1=1.0, scalar2=None, op0=ALU.add))
    dv(lambda e: e.reciprocal(out=sm[:, 20:21], in_=sm[:, 20:21]))
    dv(lambda e: e.tensor_tensor(out=sm[:, 20:21], in0=sm[:, 20:21], in1=sm[:, 2:3], op=ALU.mult))
    dv(lambda e: e.tensor_tensor(out=sm[:, 21:22], in0=sm[:, 20:21], in1=sm[:, 19:20], op=ALU.mult))
    dv(lambda e: e.tensor_scalar(out=g[:, ti, :], in0=g[:, ti, :], scalar1=sm[:, 17:18], scalar2=sm[:, 21:22], op0=ALU.is_equal, op1=ALU.mult))
    dv(lambda e: e.scalar_tensor_tensor(out=g[:, ti, :], in0=sm[:, 32:64], scalar=sm[:, 20:21], in1=g[:, ti, :], op0=ALU.mult, op1=ALU.add))


def phase6(k):
    S, I, P = k.S, k.I, k.P
    g2bc = S.sb("g2bc", [128, D])
    dgs6 = [S.sb(f"dg6_{i}", [128, 128]) for i in range(2)]
    bcast_row(k, C_ML + 160, g2bc, dgs6)
    hb = S.sb("hb6", [128, 32, 512], BF16)
    yacc = S.sb("yacc", [128, 4, D])
    hid = S.sb("hid", [128, 8, 512], BF16)
    wq = [S.sb(f"wq{i}", [128, 8192], BF16) for i in range(4)]
    h1s = S.sb("h1s", [128, 512])
    xo = S.sb("xo", [128, 2048])
    hv = k.hl2D.t.ap().rearrange("c p t -> p c t")
    wc = 0
    for tb in range(4):
        t0 = tb * 512
        S.dma("sp", hb[:], hv[:, :, t0:t0 + 512], reads=[k.hl2D], writes=[hb])
        S.op("pool", lambda e: e.memset(yacc[:], 0.0), writes=[yacc])
        for ex in range(32):
            w1v = I["w1"][ex].rearrange("(c p) n -> p c n", p=128); w3v = I["w3"][ex].rearrange("(c p) n -> p c n", p=128)
            w2v = I["w2"][ex].rearrange("(c p) n -> p c n", p=128)
            for hq in range(4):
                wa = wq[wc % 4]; wc += 1
                wbq = wq[wc % 4]; wc += 1
                wa3 = wa[:].rearrange("p (c n) -> p c n", n=256); wb3 = wbq[:].rearrange("p (c n) -> p c n", n=256)
                S.dma("pool", wa3, w1v[:, :, sl(hq, 256)], writes=[wa])
                S.dma("pool", wb3, w3v[:, :, sl(hq, 256)], writes=[wbq])
                for hc2 in range(2):
                    hc = hq * 2 + hc2
                    p1, p3 = P[0 + hc % 2], P[2 + hc % 2]
                    S.group("pe", [lambda e, c=c, wa3=wa3, p1=p1, hc2=hc2: e.matmul(p1[:, :], lhsT=wa3[:, c, sl(hc2)], rhs=hb[:, c, :], start=(c == 0), stop=(c == 31)) for c in range(32)], reads=[wa, hb], writes=[p1])
                    S.group("pe", [lambda e, c=c, wb3=wb3, p3=p3, hc2=hc2: e.matmul(p3[:, :], lhsT=wb3[:, c, sl(hc2)], rhs=hb[:, c, :], start=(c == 0), stop=(c == 31)) for c in range(32)], reads=[wbq, hb], writes=[p3])
                    S.op("act", lambda e, p1=p1: e.activation(out=h1s[:], in_=p1[:], func=AF.Silu), reads=[p1], writes=[h1s])
                    S.op("dve", lambda e, p3=p3, hc=hc: e.tensor_tensor(out=hid[:, hc, :], in0=p3[:], in1=h1s[:], op=ALU.mult), reads=[p3, h1s], writes=[hid])
            for nq in range(4):
                w2 = wq[wc % 4]; wc += 1
                w23 = w2[:].rearrange("p (c n) -> p c n", n=1024)
                S.dma("pool", w23, w2v[:, :, sl(nq, 1024)], writes=[w2])
                for tt in range(4):
                    for nh in range(2):
                        nb = nq * 2 + nh
                        py = P[4 + (tt * 2 + nh) % 4]
                        S.group("pe", [lambda e, c=c, w23=w23, py=py, tt=tt, nh=nh: e.matmul(py[:, :], lhsT=hid[:, c, sl(tt)], rhs=w23[:, c, sl(nh, 512)], start=(c == 0), stop=(c == 7)) for c in range(8)], reads=[hid, w2], writes=[py])
                        S.op("dve", lambda e, py=py, tt=tt, nb=nb, ex=ex, tb=tb: e.scalar_tensor_tensor(out=yacc[:, tt, sl(nb, 512)], in0=py[:], scalar=k.gates[:, tb * 4 + tt, ex:ex + 1], in1=yacc[:, tt, sl(nb, 512)], op0=ALU.mult, op1=ALU.add),
                             reads=[py, k.gates, yacc], writes=[yacc])
        for tt in range(4):
            ti = tb * 4 + tt
            S.op("dve", lambda e, tt=tt: e.tensor_tensor(out=yacc[:, tt, :], in0=yacc[:, tt, :], in1=g2bc[:], op=ALU.mult), reads=[yacc, g2bc], writes=[yacc])
            for hf_ in range(2):
                S.dma("sp", xo[:], k.xl1D[sl(ti), sl(hf_, 2048)], reads=[k.xl1D], writes=[xo])
                S.op("pool", lambda e, tt=tt, hf_=hf_: e.tensor_tensor(out=xo[:], in0=xo[:], in1=yacc[:, tt, sl(hf_, 2048)], op=ALU.add), reads=[xo, yacc], writes=[xo])
                S.dma("sp", k.out[sl(ti), sl(hf_, 2048)], xo[:], reads=[xo], writes=[])


def build(stop_after=99, dbg=None, only=None, iso=()):
    nc = bass.Bass("TRN2", target_bir_lowering=False)
    I = declare(nc)
    out = nc.dram_tensor("out", [T, D], F32, kind="ExternalOutput").ap()
    dbg_out = None
    if dbg and not isinstance(dbg, list):
        dbg = [("dbg",) + tuple(dbg)]
    dbg_outs = []
    for (nm, fn, shp, dt_) in (dbg or []):
        dbg_outs.append(nc.dram_tensor(nm, list(shp), dt_, kind="ExternalOutput").ap())
    with contextlib.ExitStack() as gst:
        S = Sched(nc, gst)
        k = K(); k.S = S; k.I = I; k.nc = nc; k.out = out

        def scr(name, shape, dt):
            if name in iso:
                return Res(nc.dram_tensor("d_" + name, list(shape), dt, kind="ExternalInput"), name)
            return S.dram(name, shape, dt)
        k.hT = scr("hT", [32, 128, NT], BF16)
        k.plT = [scr(f"plT{c}", [128, NT], BF16) for c in range(141)]
        k.kreD = scr("kreD", [32, NT], F32); k.kroD = scr("kroD", [32, NT], F32); k.abD = scr("abD", [64, NT], F32)
        k.modrow = scr("modrow", [2, 6 * D], F32)
        k.attD = [scr(f"attD{h}", [128, T], BF16) for h in range(16)]
        k.odD = [scr(f"odD{h}", [128, T], BF16) for h in range(16)]
        k.xl1D = scr("xl1D", [T, D], F32)
        k.hl2D = scr("hl2D", [32, 128, T], BF16)
        S.tstack = gst
        k.ident = S.sb("ident", [128, 128]); S.dma("sp", k.ident[:], I["ident"], writes=[k.ident])
        k.ones_f = S.sb("ones_f", [128, 128]); S.op("pool", lambda e: e.memset(k.ones_f[:], 1.0), writes=[k.ones_f])
        k.ones_b = S.sb("ones_b", [128, 128], BF16); S.op("pool", lambda e: e.memset(k.ones_b[:], 1.0), writes=[k.ones_b])
        k.epsc = S.sb("epsc", [128, 1]); S.op("pool", lambda e: e.memset(k.epsc[:], EPS), writes=[k.epsc])
        k.colT = S.sb("colT", [128, 704])
        k.gates = S.sb("gates", [128, 16, 32])
        k.P = [S.ps(f"P{i}", [128, 512]) for i in range(8)]
        if "colT" in iso:
            cin = nc.dram_tensor("d_colT", [128, 704], F32, kind="ExternalInput").ap()
            S.dma("sp", k.colT[:], cin, writes=[k.colT])
            derived(k)
        if "gates" in iso:
            gin = nc.dram_tensor("d_gates", [128, 16, 32], F32, kind="ExternalInput").ap()
            S.dma("sp", k.gates[:], gin, writes=[k.gates])
        phases = [phase0, phase1, phase2, phase3, phase4, phase5, phase6]
        for pi, ph in enumerate(phases):
            if pi > stop_after:
                break
            if only is not None and pi not in only:
                continue
            st = contextlib.ExitStack()
            S.tstack = st
            ph(k)
            S.barrier()
            st.close()
            S.tstack = gst
            if pi == 0:
                derived(k)
        for (nm, fn, shp, dt_), do in zip(dbg or [], dbg_outs):
            S.dma("sp", do, fn(k), reads=[], writes=[])
        S.barrier()
        S.emit()
    LAST["inputs"] = set(I.keys())
    return nc


def make_inputs(inputs):
    sq = {n: np.ascontiguousarray(np.asarray(inputs[n])[0]) for n in
          ("norm1_g", "norm2_g", "w_mod", "b_mod", "w_in", "q_a_norm_g", "w_uq", "kv_norm_g", "w_ukv", "q_norm_g",
           "q_rope_norm_g", "k_norm_g", "k_rope_norm_g", "conv_w", "dn_norm_g", "w_oa", "w_ob", "w_out", "w_rg",
           "b_rg", "w_re", "b_re", "w1", "w3", "w2")}
    sq["a_log"] = np.ascontiguousarray(np.asarray(inputs["a_log"])[0].reshape(32))
    sq["dt_bias"] = np.ascontiguousarray(np.asarray(inputs["dt_bias"])[0].reshape(32))
    sq.update(_consts())
    x = np.asarray(inputs["x"]); ctx = np.asarray(inputs["ctx"]); c = np.asarray(inputs["c"]); cc = np.asarray(inputs["c_ctx"])
    maps = []
    for b in range(8):
        m = dict(sq)
        m["x"] = np.ascontiguousarray(x[b]); m["ctx"] = np.ascontiguousarray(ctx[b])
        m["c2"] = np.ascontiguousarray(np.stack([c[b], cc], axis=0))
        maps.append(m)
    return maps


LAST = {}


def nc_inputs(nc, m):
    return [n for n in m if n in LAST["inputs"]]


def kernel(**inputs):
    from concourse.bass_utils import run_bass_kernel_spmd
    nc = build()
    maps = make_inputs(inputs)
    maps = [{n: m[n] for n in nc_inputs(nc, m)} for m in maps]
    res = run_bass_kernel_spmd(nc, maps, core_ids=list(range(8)))
    return np.stack([np.asarray(r["out"]) for r in res.results], axis=0).astype(np.float32)
```
